# Optimizing a Trainium2 kernel written in Bass

```python
import jax, jax.numpy as jnp
from jax import lax
import numpy as np

D_MODEL = 2048
BATCH = 1
SEQ = 8192
DEPTH = 1
DEC_BATCH = 32
DEC_SEQ = 1
PAST_LEN = 8192
PAGE_SIZE = 128

PLE_DIM = 256
HEAD_DIM = 64
ATTN_WIDTH = D_MODEL // 2
N_ATTN_HEADS = ATTN_WIDTH // HEAD_DIM
DILATED_PATTERNS = ((128, 1), (512, 4), (2048, 16))
N_PATTERNS = len(DILATED_PATTERNS)
BAND = 128
POOL_WINDOWS = (2, 4, 8, 16)
POOL_WIDTH = D_MODEL - ATTN_WIDTH
POOL_GROUP = POOL_WIDTH // len(POOL_WINDOWS)
POOL_STATE = max(POOL_WINDOWS) - 1
ROT_DIM = HEAD_DIM // 4
ROPE_THETA = 500000.0
QKV_WIDTH = N_PATTERNS * 3 * ATTN_WIDTH
IN_WIDTH = QKV_WIDTH + POOL_WIDTH
N_GROUPS = 4
EXPERTS_PER_GROUP = 8
N_EXPERTS = N_GROUPS * EXPERTS_PER_GROUP
TOP_K_IN_GROUP = 2
D_FF_EXPERT = 256
ALPHA = (2.0 * DEPTH) ** 0.25
BETA = (8.0 * DEPTH) ** -0.25
LN_EPS = 1e-5
NEG_INF = -1e30

kernel_name = "hymba_pool_dilated_hmoe_decoder_step"


def layer_norm(x, g, b):
    xf = x.astype(jnp.float32)
    mu = jnp.mean(xf, -1, keepdims=True)
    var = jnp.mean(jnp.square(xf - mu), -1, keepdims=True)
    return ((xf - mu) * lax.rsqrt(var + LN_EPS) * g + b).astype(x.dtype)


def partial_rope(x, pos):
    half = ROT_DIM // 2
    inv_freq = ROPE_THETA ** (-jnp.arange(0, ROT_DIM, 2, dtype=jnp.float32) / ROT_DIM)
    ang = pos.astype(jnp.float32)[:, None] * inv_freq[None, :]
    cos = jnp.cos(ang)[None, :, None, :]
    sin = jnp.sin(ang)[None, :, None, :]
    xf = x.astype(jnp.float32)
    x1, x2, rest = xf[..., :half], xf[..., half:ROT_DIM], xf[..., ROT_DIM:]
    out = jnp.concatenate([x1 * cos - x2 * sin, x2 * cos + x1 * sin, rest], -1)
    return out.astype(x.dtype)


def in_proj(x, w_in):
    b, t, _ = x.shape
    z = jnp.einsum('btd,de->bte', x, w_in)
    qkv = z[..., :QKV_WIDTH].reshape(b, t, N_PATTERNS, 3, N_ATTN_HEADS, HEAD_DIM)
    return qkv, z[..., QKV_WIDTH:]


def dilated_attn_prompt(q, k, v, window, dil):
    b, s, h, dh = q.shape
    r_max = window // dil
    span = dil * BAND
    s_pad = -(-s // span) * span
    nb = s_pad // span

    def to_blocks(t):
        t = jnp.pad(t.astype(jnp.float32), ((0, 0), (0, s_pad - s), (0, 0), (0, 0)))
        t = t.reshape(b, s_pad // dil, dil, h, dh).transpose(0, 2, 1, 3, 4)
        return t.reshape(b, dil, nb, BAND, h, dh)

    def with_prev(t):
        prev = jnp.pad(t[:, :, :-1], ((0, 0), (0, 0), (1, 0), (0, 0), (0, 0), (0, 0)))
        return jnp.concatenate([prev, t], axis=3)

    qb = to_blocks(q)
    kk = with_prev(to_blocks(k))
    vv = with_prev(to_blocks(v))
    scores = jnp.einsum('brnqhd,brnkhd->brnhqk', qb, kk) * (HEAD_DIM ** -0.5)
    qi = jnp.arange(BAND)[:, None]
    ki = jnp.arange(2 * BAND)[None, :]
    dist = BAND + qi - ki
    band = (dist >= 0) & (dist <= r_max)
    blk = jnp.arange(nb)[:, None, None]
    valid = band[None] & ((blk > 0) | (ki >= BAND)[None])
    scores = jnp.where(valid[None, None, :, None], scores, NEG_INF)
    m = jnp.max(scores, -1, keepdims=True)
    e = jnp.exp(scores - m)
    den = jnp.sum(e, -1, keepdims=True)
    o = jnp.einsum('brnhqk,brnkhd->brnhqd', e, vv) / den
    lse = (m + jnp.log(den))[..., 0]
    o = o.transpose(0, 2, 4, 1, 3, 5).reshape(b, s_pad, h, dh)[:, :s]
    lse = lse.transpose(0, 2, 4, 1, 3).reshape(b, s_pad, h)[:, :s]
    return o, lse


def dilated_attn_sample(q, kv_all, window, dil):
    t = q.shape[1]
    L = kv_all.shape[1] - t
    r_max = window // dil
    idx = (L + jnp.arange(t))[:, None] - dil * jnp.arange(r_max + 1)[None, :]
    valid = idx >= 0
    g = kv_all[:, jnp.maximum(idx, 0)].astype(jnp.float32)
    kg, vg = g[:, :, :, 0], g[:, :, :, 1]
    scores = jnp.einsum('bqhd,bqkhd->bhqk', q.astype(jnp.float32), kg) * (HEAD_DIM ** -0.5)
    scores = jnp.where(valid[None, None], scores, NEG_INF)
    m = jnp.max(scores, -1, keepdims=True)
    e = jnp.exp(scores - m)
    den = jnp.sum(e, -1, keepdims=True)
    o = jnp.einsum('bhqk,bqkhd->bqhd', e, vg) / den[..., 0].transpose(0, 2, 1)[..., None]
    lse = (m + jnp.log(den))[..., 0].transpose(0, 2, 1)
    return o, lse


def merge_patterns(outs, lses):
    o = jnp.stack(outs, 0)
    w = jax.nn.softmax(jnp.stack(lses, 0), axis=0)
    return jnp.sum(o * w[..., None], 0)


def pool_mix(u_ext, pos, w_pool, pool_scale):
    uf = u_ext.astype(jnp.float32)
    outs = []
    for gi, win in enumerate(POOL_WINDOWS):
        ug = uf[..., gi * POOL_GROUP:(gi + 1) * POOL_GROUP]
        c = jnp.cumsum(ug, axis=1)
        c_shift = jnp.pad(c[:, :-win], ((0, 0), (win, 0), (0, 0)))
        cnt = jnp.minimum(pos + 1, win).astype(jnp.float32)
        mean = (c - c_shift) / cnt[None, :, None]
        outs.append(jnp.einsum('btc,cd->btd', mean - ug, w_pool[gi].astype(jnp.float32)))
    return (jnp.concatenate(outs, -1) * pool_scale).astype(u_ext.dtype)


def prompt_mixer(x, w_in_i, w_pool_i, pool_scale_i):
    b, s, _ = x.shape
    pos = jnp.arange(s, dtype=jnp.int32)
    qkv, u = in_proj(x, w_in_i)
    outs, lses, kv_states = [], [], []
    for pi, (window, dil) in enumerate(DILATED_PATTERNS):
        q = partial_rope(qkv[:, :, pi, 0], pos)
        k = partial_rope(qkv[:, :, pi, 1], pos)
        v = qkv[:, :, pi, 2]
        o, l = dilated_attn_prompt(q, k, v, window, dil)
        outs.append(o)
        lses.append(l)
        keep = min(window, s)
        kv_states.append(jnp.stack([k[:, s - keep:], v[:, s - keep:]], axis=2))
    attn = merge_patterns(outs, lses).reshape(b, s, ATTN_WIDTH).astype(x.dtype)
    pool = pool_mix(u, pos, w_pool_i, pool_scale_i)
    return jnp.concatenate([attn, pool], -1), kv_states, u[:, s - POOL_STATE:]


def sample_mixer(x, kv_bufs, pool_buf, w_in_i, w_pool_i, pool_scale_i):
    b, t, _ = x.shape
    pos = PAST_LEN + jnp.arange(t, dtype=jnp.int32)
    qkv, u = in_proj(x, w_in_i)
    outs, lses, kv_states = [], [], []
    for pi, (window, dil) in enumerate(DILATED_PATTERNS):
        q = partial_rope(qkv[:, :, pi, 0], pos)
        k = partial_rope(qkv[:, :, pi, 1], pos)
        v = qkv[:, :, pi, 2]
        kv_all = jnp.concatenate([kv_bufs[pi], jnp.stack([k, v], axis=2).astype(kv_bufs[pi].dtype)], axis=1)
        o, l = dilated_attn_sample(q, kv_all, window, dil)
        outs.append(o)
        lses.append(l)
        keep = min(window, PAST_LEN + t)
        kv_states.append(kv_all[:, kv_all.shape[1] - keep:])
    attn = merge_patterns(outs, lses).reshape(b, t, ATTN_WIDTH).astype(x.dtype)
    u_ext = jnp.concatenate([pool_buf.astype(u.dtype), u], axis=1)
    pos_ext = PAST_LEN - POOL_STATE + jnp.arange(POOL_STATE + t, dtype=jnp.int32)
    pool = pool_mix(u_ext, pos_ext, w_pool_i, pool_scale_i)[:, POOL_STATE:]
    return jnp.concatenate([attn, pool], -1), kv_states, u_ext[:, t:]


def hier_moe(x, w_group_router, b_group_router, w_expert_router, b_expert_router, w_gate, w_up, w_down):
    b, t, d = x.shape
    xf = x.reshape(b * t, d)
    g_logits = jnp.einsum('nd,dg->ng', xf, w_group_router).astype(jnp.float32) + b_group_router
    g_sel = jnp.argmax(g_logits, -1)
    g_gate = jnp.take_along_axis(jax.nn.softmax(g_logits, -1), g_sel[:, None], -1)
    e_logits = (jnp.einsum('nd,de->ne', xf, w_expert_router).astype(jnp.float32) + b_expert_router)
    e_logits = e_logits.reshape(-1, N_GROUPS, EXPERTS_PER_GROUP)
    e_in = jnp.take_along_axis(e_logits, g_sel[:, None, None], 1)[:, 0]
    top_v, top_i = lax.top_k(e_in, TOP_K_IN_GROUP)
    top_w = jax.nn.softmax(top_v, -1) * g_gate
    e_idx = g_sel[:, None] * EXPERTS_PER_GROUP + top_i
    combine = jnp.sum(jax.nn.one_hot(e_idx, N_EXPERTS, dtype=jnp.float32) * top_w[..., None], 1)
    h = jax.nn.silu(jnp.einsum('nd,edf->nef', xf, w_gate)) * jnp.einsum('nd,edf->nef', xf, w_up)
    y = jnp.einsum('nef,efd->nd', h * combine[..., None].astype(h.dtype), w_down)
    return y.reshape(b, t, d)


def finish_layer(x, mix, p, w_out, ln1_g, ln1_b, w_group_router, b_group_router, w_expert_router,
                 b_expert_router, w_gate, w_up, w_down, ln2_g, ln2_b, w_ple, w_ple_gate):
    h = jnp.einsum('bte,ed->btd', mix, w_out)
    x1 = layer_norm(ALPHA * x + h, ln1_g, ln1_b)
    y = hier_moe(x1, w_group_router, b_group_router, w_expert_router, b_expert_router, w_gate, w_up, w_down)
    x2 = layer_norm(ALPHA * x1 + y, ln2_g, ln2_b)
    gate = jax.nn.sigmoid(jnp.einsum('btd,de->bte', x2, w_ple_gate))
    return x2 + gate * jnp.einsum('btp,pd->btd', p, w_ple)


def setup_inputs(seed: int = 0) -> dict:
    key = jax.random.key(seed)
    ks = jax.random.split(key, 32)
    f32 = jnp.float32

    def nrm(k, shape, scale):
        return jax.random.normal(k, shape, f32) * scale

    x_prompt = nrm(ks[0], (BATCH, SEQ, D_MODEL), 1.0)
    x_sample = nrm(ks[1], (DEC_BATCH, DEC_SEQ, D_MODEL), 1.0)
    caches = []
    for i, (window, dil) in enumerate(DILATED_PATTERNS):
        L = min(window, PAST_LEN)
        caches.append(nrm(ks[2 + i], (DEPTH, DEC_BATCH, L, 2, N_ATTN_HEADS, HEAD_DIM), 1.0))
    state_pool = nrm(ks[5], (DEPTH, DEC_BATCH, POOL_STATE, POOL_WIDTH), 1.0)
    p_prompt = nrm(ks[6], (DEPTH, BATCH, SEQ, PLE_DIM), 1.0)
    p_sample = nrm(ks[7], (DEPTH, DEC_BATCH, DEC_SEQ, PLE_DIM), 1.0)
    v_scale = jnp.array([1.0, 1.0, BETA], f32)
    col_scale = jnp.concatenate([
        jnp.broadcast_to(v_scale[None, :, None], (N_PATTERNS, 3, ATTN_WIDTH)).reshape(-1),
        jnp.ones((POOL_WIDTH,), f32)])
    w_in = nrm(ks[8], (DEPTH, D_MODEL, IN_WIDTH), D_MODEL ** -0.5) * col_scale
    w_out = nrm(ks[9], (DEPTH, D_MODEL, D_MODEL), BETA * D_MODEL ** -0.5)
    w_pool = nrm(ks[10], (DEPTH, len(POOL_WINDOWS), POOL_GROUP, POOL_GROUP), POOL_GROUP ** -0.5)
    pool_scale = 1.0 + nrm(ks[11], (DEPTH, POOL_WIDTH), 0.1)
    ln1_g = 1.0 + nrm(ks[12], (DEPTH, D_MODEL), 0.05)
    ln1_b = nrm(ks[13], (DEPTH, D_MODEL), 0.02)
    w_group_router = nrm(ks[14], (DEPTH, D_MODEL, N_GROUPS), D_MODEL ** -0.5)
    b_group_router = nrm(ks[15], (DEPTH, N_GROUPS), 0.01)
    w_expert_router = nrm(ks[16], (DEPTH, D_MODEL, N_EXPERTS), D_MODEL ** -0.5)
    b_expert_router = nrm(ks[17], (DEPTH, N_EXPERTS), 0.01)
    w_gate = nrm(ks[18], (DEPTH, N_EXPERTS, D_MODEL, D_FF_EXPERT), D_MODEL ** -0.5)
    w_up = nrm(ks[19], (DEPTH, N_EXPERTS, D_MODEL, D_FF_EXPERT), D_MODEL ** -0.5)
    w_down = nrm(ks[20], (DEPTH, N_EXPERTS, D_FF_EXPERT, D_MODEL), BETA * D_FF_EXPERT ** -0.5)
    ln2_g = 1.0 + nrm(ks[21], (DEPTH, D_MODEL), 0.05)
    ln2_b = nrm(ks[22], (DEPTH, D_MODEL), 0.02)
    w_ple = nrm(ks[23], (DEPTH, PLE_DIM, D_MODEL), PLE_DIM ** -0.5)
    w_ple_gate = nrm(ks[24], (DEPTH, D_MODEL, D_MODEL), D_MODEL ** -0.5)
    return {"x_prompt": x_prompt, "x_sample": x_sample,
            "cache_kv_w128_d1": caches[0], "cache_kv_w512_d4": caches[1], "cache_kv_w2048_d16": caches[2],
            "state_pool": state_pool, "p_prompt": p_prompt, "p_sample": p_sample,
            "w_in": w_in, "w_out": w_out, "w_pool": w_pool, "pool_scale": pool_scale,
            "ln1_g": ln1_g, "ln1_b": ln1_b, "w_group_router": w_group_router, "b_group_router": b_group_router,
            "w_expert_router": w_expert_router, "b_expert_router": b_expert_router,
            "w_gate": w_gate, "w_up": w_up, "w_down": w_down, "ln2_g": ln2_g, "ln2_b": ln2_b,
            "w_ple": w_ple, "w_ple_gate": w_ple_gate}


def reference(x_prompt, x_sample, cache_kv_w128_d1, cache_kv_w512_d4, cache_kv_w2048_d16, state_pool,
              p_prompt, p_sample, w_in, w_out, w_pool, pool_scale, ln1_g, ln1_b, w_group_router,
              b_group_router, w_expert_router, b_expert_router, w_gate, w_up, w_down, ln2_g, ln2_b,
              w_ple, w_ple_gate):
    caches = (cache_kv_w128_d1, cache_kv_w512_d4, cache_kv_w2048_d16)
    xp, xs = x_prompt, x_sample
    kvp = [[] for _ in range(N_PATTERNS)]
    kvs = [[] for _ in range(N_PATTERNS)]
    pool_p, pool_s = [], []
    for i in range(DEPTH):
        mix_p, kv_new_p, pool_new_p = prompt_mixer(xp, w_in[i], w_pool[i], pool_scale[i])
        mix_s, kv_new_s, pool_new_s = sample_mixer(xs, [c[i] for c in caches], state_pool[i],
                                                   w_in[i], w_pool[i], pool_scale[i])
        xp = finish_layer(xp, mix_p, p_prompt[i], w_out[i], ln1_g[i], ln1_b[i], w_group_router[i],
                          b_group_router[i], w_expert_router[i], b_expert_router[i], w_gate[i], w_up[i],
                          w_down[i], ln2_g[i], ln2_b[i], w_ple[i], w_ple_gate[i])
        xs = finish_layer(xs, mix_s, p_sample[i], w_out[i], ln1_g[i], ln1_b[i], w_group_router[i],
                          b_group_router[i], w_expert_router[i], b_expert_router[i], w_gate[i], w_up[i],
                          w_down[i], ln2_g[i], ln2_b[i], w_ple[i], w_ple_gate[i])
        for pi in range(N_PATTERNS):
            kvp[pi].append(kv_new_p[pi])
            kvs[pi].append(kv_new_s[pi])
        pool_p.append(pool_new_p)
        pool_s.append(pool_new_s)
    return (xp, xs, jnp.stack(kvp[0]), jnp.stack(kvp[1]), jnp.stack(kvp[2]), jnp.stack(pool_p),
            jnp.stack(kvs[0]), jnp.stack(kvs[1]), jnp.stack(kvs[2]), jnp.stack(pool_s))
```

```python
import numpy as np
from contextlib import ExitStack
import concourse.bass as bass
import concourse.mybir as mybir
from concourse.bass_utils import run_bass_kernel_spmd

F32 = mybir.dt.float32
BF16 = mybir.dt.bfloat16
AF = mybir.ActivationFunctionType
ALU = mybir.AluOpType
AX = mybir.AxisListType

NCORE = 8
OWN = 1024
HALO = [128, 512, 2048]
DIL = [1, 4, 16]
CL = [128, 512, 2048]
ALPHA = 2.0 ** 0.25
LN_EPS = 1e-5
POOL_WINDOWS = (2, 4, 8, 16)
NTOK = OWN + 4
TT = [(128 * i, 128) for i in range(8)] + [(1024, 4)]
CHUNKS = [(0, 512), (512, 512), (1024, 4)]
ENGS = ('pe', 'act', 'dve', 'pool', 'sp')


class _Stop(Exception):
    pass


KSTOP = [99]


DEAD = [False]
KOPS = [None]
NOPS = [0]


def _stop(n):
    if KOPS[0] == -1:
        print("stop marker", n, "ops", NOPS[0])
    if KSTOP[0] <= n:
        DEAD[0] = True


class Trk:
    def __init__(self, nc, es):
        self.nc = nc
        self.es = es
        self.eng = {'pe': nc.tensor, 'act': nc.scalar, 'dve': nc.vector, 'pool': nc.gpsimd, 'sp': nc.sync}
        self.sem = {e: es.enter_context(nc.semaphore("c_" + e)) for e in ('pe', 'act', 'dve', 'pool')}
        self.cnt = {e: 0 for e in self.sem}
        self.waited = {e: {} for e in ENGS}
        self.bufs = {}
        self.chan = {}

    def _buf(self, k):
        b = self.bufs.get(k)
        if b is None:
            b = self.bufs[k] = {'w': {}, 'r': {}}
        return b

    @staticmethod
    def _flat(keys):
        out = []
        for k in keys:
            if isinstance(k, list):
                out.extend(k)
            else:
                out.append(k)
        return out

    def _deps(self, eng, reads, writes):
        deps = {}

        def add(d, skip):
            for s, v in d.items():
                if skip and s == eng:
                    continue
                if deps.get(s, 0) < v:
                    deps[s] = v
        for k in reads:
            add(self._buf(k)['w'], False)
            if isinstance(k, tuple) and k[0] == 'pb':
                add(self._buf(k)['r'], True)
        for k in writes:
            b = self._buf(k)
            add(b['w'], True)
            add(b['r'], True)
        return deps

    def _waits(self, eng, deps):
        e = self.eng[eng]
        for s, v in deps.items():
            if v <= 0 or self.waited[eng].get(s, 0) >= v:
                continue
            semh = self.sem[s] if isinstance(s, str) else self.chan[s[1]][0]
            e.wait_ge(semh, v)
            self.waited[eng][s] = v

    def op(self, eng, fn, reads=(), writes=()):
        NOPS[0] += 1
        if KOPS[0] is not None and NOPS[0] > KOPS[0]:
            DEAD[0] = True
        if DEAD[0]:
            return None
        reads, writes = self._flat(reads), self._flat(writes)
        self._waits(eng, self._deps(eng, reads, writes))
        ins = fn(self.eng[eng])
        self.cnt[eng] += 1
        ins.then_inc(self.sem[eng], 1)
        v = self.cnt[eng]
        for k in reads:
            self._buf(k)['r'][eng] = v
        for k in writes:
            b = self._buf(k)
            b['w'] = {eng: v}
            b['r'] = {}
        return ins

    def dma(self, eng, chan, out, in_, reads=(), writes=()):
        NOPS[0] += 1
        if KOPS[0] is not None and NOPS[0] > KOPS[0]:
            DEAD[0] = True
        if DEAD[0]:
            return None
        reads, writes = self._flat(reads), self._flat(writes)
        if chan not in self.chan:
            self.chan[chan] = [self.es.enter_context(self.nc.semaphore("d_" + chan)), 0]
        self._waits(eng, self._deps(None, reads, writes))
        c = self.chan[chan]
        c[1] += 16
        self.eng[eng].dma_start(out=out, in_=in_).then_inc(c[0], 16)
        key = ('ch', chan)
        for k in reads:
            self._buf(k)['r'][key] = c[1]
        for k in writes:
            b = self._buf(k)
            b['w'] = {key: c[1]}
            b['r'] = {}

    def barrier(self, engines=ENGS):
        if DEAD[0]:
            return
        deps = {e: self.cnt[e] for e in self.sem}
        deps.update({('ch', n): c[1] for n, c in self.chan.items()})
        for e in engines:
            self._waits(e, deps)
        self.bufs = {}


def build_program():
    nc = bass.Bass("TRN2", target_bir_lowering=False)
    DEAD[0] = False
    NOPS[0] = 0

    def din(name, shape):
        return nc.dram_tensor(name, list(shape), F32, kind="ExternalInput").ap()

    def dout(name, shape):
        return nc.dram_tensor(name, list(shape), F32, kind="ExternalOutput").ap()

    xh = din("xh", [3072, 2048])
    xs = din("xs", [4, 2048])
    c_in = [din("c%d" % p, [4, CL[p], 2, 1024]) for p in range(3)]
    spool = din("spool", [4, 15, 1024])
    pp = din("pp", [1024, 256])
    pps = din("pps", [4, 256])
    w_in = din("w_in", [2048, 10240])
    w_out = din("w_out", [2048, 2048])
    w_pool = din("w_pool", [4, 256, 256])
    pool_scale = din("pool_scale", [1024, 1])
    ln_gb = [din(n, [1, 2048]) for n in ("ln1_g", "ln1_b", "ln2_g", "ln2_b")]
    w_gr = din("w_gr", [2048, 4])
    w_er = din("w_er", [2048, 32])
    b_r = din("b_r", [1, 36])
    w_gate = din("w_gate", [32, 2048, 256])
    w_up = din("w_up", [32, 2048, 256])
    w_down = din("w_down", [32, 256, 2048])
    w_ple = din("w_ple", [256, 2048])
    w_pg = din("w_pg", [2048, 2048])
    ropeC_d = din("ropeC", [128, 3076])
    ropeS_d = din("ropeS", [128, 3076])
    rperm_d = din("rperm", [128, 128])
    mk_d = din("mk", [128, 4, 256])
    invc_d = din("invc", [128, 4, 16])
    sel_d = din("sel", [60, 4, 4])

    y = dout("y", [NTOK, 2048])
    kvp = [dout("kvp0", [128, 2, 1024]), dout("kvp1", [512, 2, 1024]), dout("kvp2", [1024, 2, 1024])]
    KVP_FROM = [OWN - 128, OWN - 512, 0]
    poolp = dout("poolp", [16, 1024])
    kvs = [dout("kvs%d" % p, [4, CL[p], 2, 1024]) for p in range(3)]
    pools = dout("pools", [4, 15, 1024])

    with ExitStack() as es:
        T = Trk(nc, es)
        try:

            def sb(scope, name, shape, dt):
                return scope.enter_context(nc.sbuf_tensor("s_" + name, list(shape), dt))

            pb = [es.enter_context(nc.psum_tensor("pb%d" % i, [128, 512], F32)) for i in range(8)]
            PBK = [('pb', i) for i in range(8)]

            ident = sb(es, "ident", [128, 128], F32)
            identb = sb(es, "identb", [128, 128], BF16)
            ones_f = sb(es, "ones_f", [128, 128], F32)
            ones_b = sb(es, "ones_b", [128, 128], BF16)
            T.op('pool', lambda e: e.memset(ident[:], 0.0), writes=['ident'])
            T.op('pool', lambda e: e.affine_select(out=ident[:], in_=ident[:], pattern=[[-1, 128]],
                                                   compare_op=ALU.not_equal, fill=1.0, base=0, channel_multiplier=1),
                 reads=['ident'], writes=['ident'])
            T.op('pool', lambda e: e.tensor_copy(out=identb[:], in_=ident[:]), reads=['ident'], writes=['identb'])
            T.op('pool', lambda e: e.memset(ones_f[:], 1.0), writes=['ones_f'])
            T.op('pool', lambda e: e.memset(ones_b[:], 1.0), writes=['ones_b'])

            for p in range(3):
                L = CL[p]
                for b in range(4):
                    r0 = 1
                    while r0 < L:
                        r1 = min(L, r0 + 512)
                        T.dma('sp', "cc%d" % ((b + p) % 4), kvs[p][b, r0 - 1:r1 - 1, :, :], c_in[p][b, r0:r1, :, :])
                        r0 = r1
            for b in range(4):
                T.dma('sp', "cc%d" % b, pools[b, 0:14, :], spool[b, 1:15, :])

            _stop(0)
            mixT = sb(es, "mixT", [128, 16, NTOK], BF16)

            with ExitStack() as pa:
                xT = sb(pa, "xT", [128, 16, 3072], BF16)
                xsT = sb(pa, "xsT", [128, 16, 4], BF16)
                ropeC = sb(pa, "ropeC_s", [128, 3076], BF16)
                ropeS = sb(pa, "ropeS_s", [128, 3076], BF16)
                rperm = sb(pa, "rperm_s", [128, 128], BF16)
                mk = sb(pa, "mk_s", [128, 4, 256], BF16)
                T.dma('pool', "k0", ropeC[:], ropeC_d[:, :], writes=['ropeC'])
                T.dma('pool', "k1", ropeS[:], ropeS_d[:, :], writes=['ropeS'])
                T.dma('pool', "k2", rperm[:], rperm_d[:, :], writes=['rperm'])
                T.dma('pool', "k3", mk[:], mk_d[:, :, :], writes=['mk'])

                with ExitStack() as px:
                    xb = [sb(px, "xb%d" % i, [128, 2048], BF16) for i in range(3)]
                    ptb = [pb[3 + g][:].bitcast(BF16) for g in range(4)]
                    for t in range(25):
                        s = t % 3
                        nt = 128 if t < 24 else 4
                        src = xh[128 * t:128 * t + 128, :] if t < 24 else xs[:, :]
                        T.dma('pool', "xb%d" % s, xb[s][0:nt, :], src, writes=[('xb', s)])
                        for g in range(2):
                            bi = 2 * (t % 2) + g
                            bank = ptb[bi]
                            w = nt

                            def f(e, g=g, s=s, bank=bank, nt=nt, w=w):
                                for j in range(8):
                                    k = 8 * g + j
                                    ins = e.transpose(out=bank[:, w * j:w * j + w], in_=xb[s][0:nt, 128 * k:128 * k + 128],
                                                      identity=identb[0:nt, 0:nt])
                                return ins
                            T.op('pe', f, reads=[('xb', s), 'identb'], writes=[PBK[3 + bi]])
                            dst = xT[:, 8 * g:8 * g + 8, 128 * t:128 * t + 128] if t < 24 else xsT[:, 8 * g:8 * g + 8, :]
                            srcv = bank[:, 0:8 * w].rearrange("p (j c) -> p j c", c=w)
                            if g == 0:
                                T.op('act', lambda e, dst=dst, srcv=srcv: e.copy(out=dst, in_=srcv),
                                     reads=[PBK[3 + bi]], writes=[('xT', t, g)])
                            else:
                                T.op('dve', lambda e, dst=dst, srcv=srcv: e.tensor_copy(out=dst, in_=srcv),
                                     reads=[PBK[3 + bi]], writes=[('xT', t, g)])
                    T.barrier()
                _stop(1)

                NSLAB = 4
                slab = [sb(pa, "slab%d" % i, [128, 16, 128], BF16) for i in range(NSLAB)]
                pat = ExitStack()
                kT = sb(pat, "kT", [128, 3072], BF16)
                ksT = sb(pat, "ksT", [128, 4], BF16)
                qT = sb(pat, "qT", [128, NTOK], BF16)
                V_ext = sb(pat, "V_ext", [128, 32, 2, 65], BF16)
                Vs_ext = sb(pat, "Vs_ext", [4, 2, 65], BF16)
                zb = [sb(pat, "zb%d" % i, [128, 512], BF16) for i in range(2)]
                t1 = sb(pat, "t1", [128, 512], F32)
                t2 = sb(pat, "t2", [128, 512], F32)
                kf = sb(pat, "kf", [128, 512], F32)
                ksf = sb(pat, "ksf", [128, 4], F32)
                Eb = [sb(pat, "E%d" % i, [128, 256], BF16) for i in range(4)]
                stg = [sb(pat, "stg%d" % i, [128, 512], F32) for i in range(2)]
                stv = [sb(pat, "stv%d" % i, [128, 128], F32) for i in range(4)]
                sts = [sb(pat, "sts%d" % i, [4, 128], F32) for i in range(2)]
                kg = [sb(pat, "kg%d" % i, [128, 128], BF16) for i in range(2)]
                kcT = [sb(pat, "kcT%d" % i, [128, 128], BF16) for i in range(2)]
                vg = [sb(pat, "vg%d" % i, [128, 2, 65], BF16) for i in range(4)]
                qz = sb(pat, "qz", [128, 2, 4], BF16)
                Es = sb(pat, "Es", [128, 8], BF16)
                En = sb(pat, "En", [4, 8], BF16)
                Tsamp = sb(pat, "Tsamp", [65, 8], F32)
                rden = sb(pat, "rden", [65, 512], F32)
                bcs = sb(pat, "bcs", [64, 512], F32)
                T.op('pool', lambda e: e.memset(V_ext[:], 1.0), writes=['V_ext'])
                T.op('pool', lambda e: e.memset(Vs_ext[:], 1.0), writes=['Vs_ext'])
                T.op('pool', lambda e: e.memset(qz[:], 0.0), writes=['qz'])
                for i in range(4):
                    T.op('pool', lambda e, i=i: e.memset(vg[i][:], 1.0), writes=[('vg', i)])

                state = {'slab': 0, 'mm': 0, 'E': 0, 'stg': 0, 'stv': 0, 'sts': 0, 'kg': 0, 'vg': 0}

                def load_slab(col0):
                    s = state['slab'] % NSLAB
                    state['slab'] += 1
                    T.dma('pool', "slab%d" % s, slab[s][:],
                          w_in[:, col0:col0 + 128].rearrange("(k p) n -> p k n", p=128), writes=[('slab', s)])
                    return s

                def mmbank():
                    i = state['mm'] % 2
                    state['mm'] += 1
                    return i

                def proj_fm(s, rhs_fn, N):
                    bi = mmbank()

                    def f(e):
                        for k in range(16):
                            ins = e.matmul(pb[bi][:, 0:N], lhsT=slab[s][:, k, :], rhs=rhs_fn(k),
                                           start=(k == 0), stop=(k == 15))
                        return ins
                    T.op('pe', f, reads=[('slab', s)], writes=[PBK[bi]])
                    return bi

                def rope(bi, N, tc0, dst_bf, dst_key, want_f32=None):
                    z = zb[bi]
                    T.op('act', lambda e: e.copy(out=z[:, 0:N], in_=pb[bi][:, 0:N]), reads=[PBK[bi]], writes=[('zb', bi)])
                    T.op('pe', lambda e: e.matmul(pb[2][:, 0:N], lhsT=rperm[:], rhs=z[:, 0:N], start=True, stop=True),
                         reads=[('zb', bi), 'rperm'], writes=[PBK[2]])
                    T.op('dve', lambda e: e.tensor_tensor(out=t1[:, 0:N], in0=pb[bi][:, 0:N], in1=ropeC[:, tc0:tc0 + N],
                                                          op=ALU.mult), reads=[PBK[bi], 'ropeC'], writes=['t1'])
                    T.op('dve', lambda e: e.tensor_tensor(out=t2[:, 0:N], in0=pb[2][:, 0:N], in1=ropeS[:, tc0:tc0 + N],
                                                          op=ALU.mult), reads=[PBK[2], 'ropeS'], writes=['t2'])
                    if want_f32 is None:
                        T.op('pool', lambda e: e.tensor_tensor(out=dst_bf, in0=t1[:, 0:N], in1=t2[:, 0:N], op=ALU.add),
                             reads=['t1', 't2'], writes=[dst_key])
                    else:
                        fdst, fkey = want_f32
                        T.op('pool', lambda e: e.tensor_tensor(out=fdst, in0=t1[:, 0:N], in1=t2[:, 0:N], op=ALU.add),
                             reads=['t1', 't2'], writes=[fkey])
                        T.op('act', lambda e: e.copy(out=dst_bf, in_=fdst), reads=[fkey], writes=[dst_key])

                for hg in range(8):
                    if hg == 1:
                        _stop(2)
                    TH = [[pb[3], pb[4]], [pb[5], pb[6]]]
                    THK = [[PBK[3], PBK[4]], [PBK[5], PBK[6]]]
                    for hh_ in range(2):
                        for bk_ in range(2):
                            T.op('dve', lambda e, hh_=hh_, bk_=bk_: e.memset(TH[hh_][bk_][0:65, :], 0.0), writes=[THK[hh_][bk_]])
                    for p in range(3):
                        d = DIL[p]
                        halo = HALO[p]
                        s0 = 2048 - halo
                        Tp = halo + OWN
                        qc = p * 3072 + hg * 128
                        s_k = load_slab(qc + 1024)
                        s_v = load_slab(qc + 2048)
                        s_q = load_slab(qc)
                        c = 0
                        while c < Tp:
                            N = min(512, Tp - c)
                            if halo == 128 and c == 0:
                                N = 128
                            xc = s0 + c
                            bi = proj_fm(s_k, lambda k, xc=xc, N=N: xT[:, k, xc:xc + N], N)
                            tau0 = xc - 2048
                            need = (tau0 + N > KVP_FROM[p]) and tau0 >= 0
                            if need:
                                rope(bi, N, xc, kT[:, c:c + N], ('kT', c), want_f32=(kf[:, 0:N], 'kf'))
                                g = state['stg'] % 2
                                state['stg'] += 1
                                j0 = max(0, (KVP_FROM[p] - tau0) // 128)
                                nj = N // 128

                                def f(e, j0=j0, nj=nj, N=N):
                                    for j in range(j0, nj):
                                        ins = e.transpose(out=pb[7][:, 128 * j:128 * j + 128], in_=kf[:, 128 * j:128 * j + 128],
                                                          identity=ident[:])
                                    return ins
                                T.op('pe', f, reads=['kf', 'ident'], writes=[PBK[7]])
                                T.op('dve', lambda e, g=g, j0=j0, N=N: e.tensor_copy(out=stg[g][:, 128 * j0:N],
                                                                                     in_=pb[7][:, 128 * j0:N]),
                                     reads=[PBK[7]], writes=[('stg', g)])
                                r0 = tau0 + 128 * j0 - KVP_FROM[p]
                                T.dma('sp', "stg%d" % g,
                                      kvp[p][r0:r0 + 128 * (nj - j0), 0, hg * 128:hg * 128 + 128].rearrange("(j q) c -> q j c", q=128),
                                      stg[g][:, 128 * j0:N].rearrange("q (j c) -> q j c", c=128), reads=[('stg', g)])
                            else:
                                rope(bi, N, xc, kT[:, c:c + N], ('kT', c))
                            c += N
                        if hg == 0 and p == 0:
                            _stop(1.1)
                        bi = proj_fm(s_k, lambda k: xsT[:, k, :], 4)
                        rope(bi, 4, 3072, ksT[:, :], 'ksT', want_f32=(ksf[:, :], 'ksf'))
                        g = state['sts'] % 2
                        state['sts'] += 1
                        T.op('pe', lambda e: e.transpose(out=pb[7][0:4, 0:128], in_=ksf[:, :], identity=ident[:]),
                             reads=['ksf', 'ident'], writes=[PBK[7]])
                        T.op('dve', lambda e, g=g: e.tensor_copy(out=sts[g][:, :], in_=pb[7][0:4, 0:128]),
                             reads=[PBK[7]], writes=[('sts', g)])
                        T.dma('sp', "sts%d" % g, kvs[p][:, CL[p] - 1, 0, hg * 128:hg * 128 + 128], sts[g][:, :],
                              reads=[('sts', g)])
                        if hg == 0 and p == 0:
                            _stop(1.2)
                        Lr = Tp // d
                        ntr = (Lr + 127) // 128
                        tiles = [(r, b) for r in range(d) for b in range(ntr)]
                        for t0 in range(0, len(tiles), 4):
                            grp = tiles[t0:t0 + 4]
                            bi = mmbank()

                            def f(e, grp=grp, bi=bi):
                                for i, (r, b) in enumerate(grp):
                                    nk = min(128, Lr - 128 * b)
                                    a = s0 + d * 128 * b + r
                                    for k in range(16):
                                        ins = e.matmul(pb[bi][0:nk, 128 * i:128 * i + 128],
                                                       lhsT=xT[:, k, a:a + d * (nk - 1) + 1:d], rhs=slab[s_v][:, k, :],
                                                       start=(k == 0), stop=(k == 15))
                                return ins
                            T.op('pe', f, reads=[('slab', s_v)], writes=[PBK[bi]])
                            ng = len(grp)
                            T.op('act', lambda e, t0=t0, ng=ng, bi=bi: e.copy(
                                out=V_ext[:, t0:t0 + ng, :, 0:64],
                                in_=pb[bi][:, 0:128 * ng].rearrange("q (i h c) -> q i h c", h=2, c=64)),
                                reads=[PBK[bi]], writes=['V_ext'])
                            for i, (r, b) in enumerate(grp):
                                nk = min(128, Lr - 128 * b)
                                tau0 = d * 128 * b + r - halo
                                if tau0 >= KVP_FROM[p]:
                                    g = state['stv'] % 4
                                    state['stv'] += 1
                                    T.op('dve', lambda e, g=g, i=i, nk=nk, bi=bi: e.tensor_copy(
                                        out=stv[g][0:nk, :], in_=pb[bi][0:nk, 128 * i:128 * i + 128]),
                                        reads=[PBK[bi]], writes=[('stv', g)])
                                    r0 = tau0 - KVP_FROM[p]
                                    T.dma('sp', "stv%d" % g, kvp[p][r0:r0 + d * (nk - 1) + 1:d, 1, hg * 128:hg * 128 + 128],
                                          stv[g][0:nk, :], reads=[('stv', g)])
                        bi = mmbank()

                        def f(e, bi=bi):
                            for k in range(16):
                                ins = e.matmul(pb[bi][0:4, 0:128], lhsT=xsT[:, k, :], rhs=slab[s_v][:, k, :],
                                               start=(k == 0), stop=(k == 15))
                            return ins
                        T.op('pe', f, reads=[('slab', s_v)], writes=[PBK[bi]])
                        T.op('act', lambda e, bi=bi: e.copy(out=Vs_ext[:, :, 0:64],
                                                            in_=pb[bi][0:4, 0:128].rearrange("q (h c) -> q h c", c=64)),
                             reads=[PBK[bi]], writes=['Vs_ext'])
                        g = state['sts'] % 2
                        state['sts'] += 1
                        T.op('dve', lambda e, g=g, bi=bi: e.tensor_copy(out=sts[g][:, :], in_=pb[bi][0:4, 0:128]),
                             reads=[PBK[bi]], writes=[('sts', g)])
                        T.dma('sp', "sts%d" % g, kvs[p][:, CL[p] - 1, 1, hg * 128:hg * 128 + 128], sts[g][:, :],
                              reads=[('sts', g)])
                        if hg == 0 and p == 0:
                            _stop(1.3)
                        for (c0, N) in CHUNKS:
                            if c0 < OWN:
                                bi = proj_fm(s_q, lambda k, c0=c0, N=N: xT[:, k, 2048 + c0:2048 + c0 + N], N)
                                rope(bi, N, 2048 + c0, qT[:, c0:c0 + N], ('qT', c0))
                            else:
                                bi = proj_fm(s_q, lambda k: xsT[:, k, :], 4)
                                rope(bi, 4, 3072, qT[:, OWN:OWN + 4], ('qT', c0))
                                T.op('dve', lambda e: e.tensor_copy(out=qz[0:64, 0, :], in_=qT[0:64, OWN:OWN + 4]),
                                     reads=[('qT', c0)], writes=['qz'])
                                T.op('dve', lambda e: e.tensor_copy(out=qz[64:128, 1, :], in_=qT[64:128, OWN:OWN + 4]),
                                     reads=[('qT', c0)], writes=['qz'])
                        kT_keys = [('kT', cc) for cc in ([0, 128, 640] if halo == 128 else list(range(0, Tp, 512)))]
                        qT_keys = [('qT', 0), ('qT', 512), ('qT', 1024)]

                        if hg == 0 and p == 0:
                            _stop(1.4)
                        def att_tile(hh, k_aps, q_ap, nq, mask_ap, pv):
                            es_ = state['E'] % 4
                            state['E'] += 1
                            pk = PBK[7]
                            base = 0

                            def f(e):
                                for b, (kap, nk) in enumerate(k_aps):
                                    ins = e.matmul(pb[7][0:nk, base + nq * b:base + nq * b + nq], lhsT=kap, rhs=q_ap,
                                                   start=True, stop=True)
                                return ins
                            T.op('pe', f, reads=kT_keys + qT_keys + ['ksT'], writes=[pk])
                            W = nq * len(k_aps)
                            T.op('act', lambda e: e.activation(out=Eb[es_][:, 0:W], in_=pb[7][:, base:base + W], func=AF.Exp,
                                                               scale=0.125), reads=[pk], writes=[('E', es_)])
                            T.op('pool', lambda e: e.tensor_tensor(out=Eb[es_][:, 0:W], in0=Eb[es_][:, 0:W], in1=mask_ap,
                                                                   op=ALU.mult), reads=[('E', es_), 'mk'], writes=[('E', es_)])
                            for (b, vt, ec0, en, bk, oap, colkey) in pv:
                                nk = k_aps[b][1]
                                first = False
                                T.op('pe', lambda e, vt=vt, ec0=ec0, en=en, oap=oap, nk=nk, first=first: e.matmul(
                                    oap, lhsT=V_ext[0:nk, vt, hh, :], rhs=Eb[es_][0:nk, ec0:ec0 + en], start=first, stop=False,
                                    skip_group_check=True), reads=[('E', es_), 'V_ext'], writes=[THK[hh][bk]])

                        for hh in range(2):
                            P0 = 64 * hh
                            if p == 0:
                                for i in range(8):
                                    k_aps = [(kT[P0:P0 + 64, 128 * (i + b):128 * (i + b) + 128], 128) for b in range(2)]
                                    q_ap = qT[P0:P0 + 64, 128 * i:128 * i + 128]
                                    m = mk[:, 1 if i == 0 else 0, :]
                                    bk = i // 4
                                    oap = TH[hh][bk][0:65, 128 * (i % 4):128 * (i % 4) + 128]
                                    pv = [(b, i + b, 128 * b, 128, bk, oap, ('c', i)) for b in range(2)]
                                    att_tile(hh, k_aps, q_ap, 128, m, pv)
                            elif p == 1:
                                for r in range(4):
                                    for j in range(2):
                                        k_aps = [(kT[P0:P0 + 64, 512 * (j + b) + r:512 * (j + b + 1):4], 128) for b in range(2)]
                                        q_ap = qT[P0:P0 + 64, 512 * j + r:512 * (j + 1):4]
                                        m = mk[:, 2 if j == 0 else 0, :]
                                        oap = TH[hh][j][0:65, r:512:4]
                                        pv = [(b, r * 3 + j + b, 128 * b, 128, j, oap, ('s4', r)) for b in range(2)]
                                        att_tile(hh, k_aps, q_ap, 128, m, pv)
                            else:
                                for r in range(16):
                                    k_aps = [(kT[P0:P0 + 64, r:2048:16], 128), (kT[P0:P0 + 64, 2048 + r:3072:16], 64)]
                                    q_ap = qT[P0:P0 + 64, r:1024:16]
                                    m = mk[:, 3, 0:128]
                                    pv = []
                                    for b in range(2):
                                        for bk in range(2):
                                            oap = TH[hh][bk][0:65, r:512:16]
                                            pv.append((b, r * 2 + b, 64 * b + 32 * bk, 32, bk, oap, ('s16', r)))
                                    att_tile(hh, k_aps, q_ap, 64, m, pv)

                        if hg == 0 and p == 0:
                            _stop(1.5)
                        L = CL[p]
                        for b in range(4):
                            g = state['kg'] % 2
                            state['kg'] += 1
                            gv = state['vg'] % 4
                            state['vg'] += 1
                            T.dma('pool', "kg%d" % g, kg[g][:, :], c_in[p][b, 0:L:d, 0, hg * 128:hg * 128 + 128],
                                  writes=[('kg', g)])
                            T.dma('pool', "vg%d" % gv, vg[gv][:, :, 0:64],
                                  c_in[p][b, 0:L:d, 1, hg * 128:hg * 128 + 128].rearrange("q (h c) -> q h c", c=64),
                                  writes=[('vg', gv)])
                            kb = pb[1][:].bitcast(BF16)
                            T.op('pe', lambda e, g=g, kb=kb: e.transpose(out=kb[:, 0:128], in_=kg[g][:, :], identity=identb[:]),
                                 reads=[('kg', g), 'identb'], writes=[PBK[1]])
                            T.op('dve', lambda e, g=g, kb=kb: e.tensor_copy(out=kcT[g][:, :], in_=kb[:, 0:128]),
                                 reads=[PBK[1]], writes=[('kcT', g)])

                            def f(e, g=g, b=b):
                                for hh in range(2):
                                    ins = e.matmul(pb[0][:, 4 * hh + b:4 * hh + b + 1], lhsT=kcT[g][:, :],
                                                   rhs=qz[:, hh, b:b + 1], start=True, stop=True)
                                return ins
                            T.op('pe', f, reads=[('kcT', g), 'qz'], writes=[PBK[0]])
                            T.op('act', lambda e, b=b: e.activation(out=Es[:, b:8:4], in_=pb[0][:, b:8:4], func=AF.Exp, scale=0.125),
                                 reads=[PBK[0]], writes=['Es'])

                            def f2(e, gv=gv, b=b):
                                for hh in range(2):
                                    ins = e.matmul(pb[0][0:65, 32 + 4 * hh + b:32 + 4 * hh + b + 1], lhsT=vg[gv][:, hh, :],
                                                   rhs=Es[:, 4 * hh + b:4 * hh + b + 1], start=True, stop=False,
                                                   skip_group_check=True)
                                return ins
                            T.op('pe', f2, reads=[('vg', gv), 'Es'], writes=[PBK[0]])
                        if hg == 0 and p == 0:
                            _stop(1.6)
                        def f3(e):
                            for hh in range(2):
                                ins = e.matmul(pb[0][0:4, 16 + 4 * hh:16 + 4 * hh + 4], lhsT=ksT[:, :],
                                               rhs=qz[:, hh, :], start=True, stop=True)
                            return ins
                        T.op('pe', f3, reads=['ksT', 'qz'], writes=[PBK[0]])
                        T.op('act', lambda e: e.activation(out=En[:, :], in_=pb[0][0:4, 16:24], func=AF.Exp, scale=0.125),
                             reads=[PBK[0]], writes=['En'])
                        for hh_ in range(2):
                            T.op('dve', lambda e, hh_=hh_: e.tensor_tensor(out=En[:, 4 * hh_:4 * hh_ + 4], in0=En[:, 4 * hh_:4 * hh_ + 4],
                                                                           in1=identb[0:4, 0:4], op=ALU.mult),
                                 reads=['En', 'identb'], writes=['En'])

                        def f4(e):
                            for hh in range(2):
                                ins = e.matmul(pb[0][0:65, 40 + 4 * hh:40 + 4 * hh + 4], lhsT=Vs_ext[:, hh, :],
                                               rhs=En[:, 4 * hh:4 * hh + 4], start=True, stop=True, skip_group_check=True)
                            return ins
                        T.op('pe', f4, reads=['Vs_ext', 'En'], writes=[PBK[0]])
                        if p == 0:
                            T.op('dve', lambda e: e.tensor_copy(out=Tsamp[:, :], in_=pb[0][0:65, 32:40]), reads=[PBK[0]], writes=['TS'])
                        else:
                            T.op('dve', lambda e: e.tensor_tensor(out=Tsamp[:, :], in0=pb[0][0:65, 32:40], in1=Tsamp[:, :], op=ALU.add),
                                 reads=[PBK[0], 'TS'], writes=['TS'])
                        T.op('dve', lambda e: e.tensor_tensor(out=Tsamp[:, :], in0=pb[0][0:65, 40:48], in1=Tsamp[:, :], op=ALU.add),
                             reads=[PBK[0], 'TS'], writes=['TS'])

                    if hg == 0:
                        _stop(1.8)
                    for hh in range(2):
                        P0 = 64 * hh
                        for bk in range(3):
                            if bk < 2:
                                src = TH[hh][bk]
                                skey = THK[hh][bk]
                                c_a, c_b, N = 0, 512 * bk, 512
                            else:
                                src = Tsamp
                                skey = 'TS'
                                c_a, c_b, N = 4 * hh, OWN, 4
                            T.op('dve', lambda e, src=src, c_a=c_a, N=N: e.reciprocal(out=rden[64:65, 0:N], in_=src[64:65, c_a:c_a + N]),
                                 reads=[skey], writes=['rden'])
                            T.op('pe', lambda e, N=N: e.matmul(pb[0][0:64, 0:N], lhsT=ones_f[64:65, 0:64], rhs=rden[64:65, 0:N],
                                                               start=True, stop=True), reads=['rden', 'ones_f'], writes=[PBK[0]])
                            T.op('act', lambda e, N=N: e.copy(out=bcs[:, 0:N], in_=pb[0][0:64, 0:N]), reads=[PBK[0]], writes=['bcs'])
                            T.op('dve', lambda e, src=src, c_a=c_a, c_b=c_b, N=N, P0=P0: e.tensor_tensor(
                                out=mixT[P0:P0 + 64, hg, c_b:c_b + N], in0=src[0:64, c_a:c_a + N], in1=bcs[:, 0:N], op=ALU.mult),
                                reads=[skey, 'bcs'], writes=[('mixT', hg, hh, bk)])

                T.barrier()
                pat.close()
                _stop(3)
                with ExitStack() as pp_:
                    uA = sb(pp_, "uA", [128, 2, 1040], F32)
                    uB = sb(pp_, "uB", [128, 1040], F32)
                    uC = sb(pp_, "uC", [128, 1040], F32)
                    usT = sb(pp_, "usT", [128, 2, 4], F32)
                    diffT = sb(pp_, "diffT", [128, 2, NTOK], BF16)
                    wpl = sb(pp_, "wpl", [128, 2, 256], BF16)
                    psc = sb(pp_, "psc", [128, 8], F32)
                    invc = sb(pp_, "invc", [128, 4, 16], F32)
                    sel = sb(pp_, "sel", [60, 4, 4], F32)
                    stt = sb(pp_, "stt", [60, 1024], F32)
                    d16 = sb(pp_, "d16", [128, 16], F32)
                    ssum = sb(pp_, "ssum", [128, 4], F32)
                    ost = sb(pp_, "ost", [16, 128], F32)
                    ost2 = sb(pp_, "ost2", [4, 128], F32)
                    T.dma('sp', "k0", invc[:], invc_d[:, :, :], writes=['invc'])
                    T.dma('sp', "k1", sel[:], sel_d[:, :, :], writes=['sel'])
                    T.dma('sp', "k2", stt[:], spool.rearrange("b j c -> (b j) c"), writes=['stt'])
                    for j in range(8):
                        T.dma('sp', "k3", psc[:, j:j + 1], pool_scale[128 * j:128 * j + 128, :], writes=['psc'])
                    for gi in range(4):
                        win = POOL_WINDOWS[gi]
                        T.dma('pool', "wpl", wpl[:], w_pool[gi].rearrange("(k q) n -> q k n", q=128), writes=['wpl'])
                        for ct in range(2):
                            jt = 2 * gi + ct
                            s_u = load_slab(9216 + 128 * jt)
                            for (xc, N, dst, dkey) in [(2032, 16, uA[:, ct, 0:16], ('uA', ct, 0)),
                                                       (2048, 512, uA[:, ct, 16:528], ('uA', ct, 1)),
                                                       (2560, 512, uA[:, ct, 528:1040], ('uA', ct, 2)),
                                                       (-1, 4, usT[:, ct, :], ('usT', ct))]:
                                if xc >= 0:
                                    bi = proj_fm(s_u, lambda k, xc=xc, N=N: xT[:, k, xc:xc + N], N)
                                else:
                                    bi = proj_fm(s_u, lambda k: xsT[:, k, :], 4)
                                T.op('act', lambda e, bi=bi, N=N, dst=dst: e.copy(out=dst, in_=pb[bi][:, 0:N]),
                                     reads=[PBK[bi]], writes=[dkey])
                            ukeys = [('uA', ct, i) for i in range(3)]
                            T.op('pe', lambda e, ct=ct: e.transpose(out=pb[7][0:16, 0:128], in_=uA[:, ct, 1024:1040], identity=ident[:]),
                                 reads=ukeys + ['ident'], writes=[PBK[7]])
                            T.op('dve', lambda e: e.tensor_copy(out=ost[:, :], in_=pb[7][0:16, 0:128]), reads=[PBK[7]], writes=['ost'])
                            T.dma('sp', "ost", poolp[:, 128 * jt:128 * jt + 128], ost[:, :], reads=['ost'])
                            T.op('pe', lambda e, ct=ct: e.transpose(out=pb[7][0:4, 128:256], in_=usT[:, ct, :], identity=ident[:]),
                                 reads=[('usT', ct), 'ident'], writes=[PBK[7]])
                            T.op('dve', lambda e: e.tensor_copy(out=ost2[:, :], in_=pb[7][0:4, 128:256]), reads=[PBK[7]], writes=['ost2'])
                            T.dma('sp', "ost2", pools[:, 14, 128 * jt:128 * jt + 128], ost2[:, :], reads=['ost2'])
                            cur, ckey = uA[:, ct, :], ukeys
                            bufs2 = [(uB, 'uB'), (uC, 'uC')]
                            step, n = 1, 0
                            while step < win:
                                dst, dk = bufs2[n % 2]
                                T.op('dve', lambda e, cur=cur, dst=dst, step=step: e.tensor_tensor(
                                    out=dst[:, step:1040], in0=cur[:, step:1040], in1=cur[:, 0:1040 - step], op=ALU.add),
                                    reads=(ckey if isinstance(ckey, list) else [ckey]), writes=[dk])
                                cur, ckey = dst[:, :], dk
                                step *= 2
                                n += 1
                            T.op('dve', lambda e, cur=cur, ct=ct: e.scalar_tensor_tensor(
                                out=diffT[:, ct, 0:OWN], in0=cur[:, 16:1040], scalar=1.0 / win, in1=uA[:, ct, 16:1040],
                                op0=ALU.mult, op1=ALU.subtract), reads=[ckey] + ukeys, writes=[('diffT', ct)])
                            T.op('dve', lambda e, cur=cur: e.tensor_tensor(out=d16[:, :], in0=cur[:, 16:32], in1=invc[:, gi, :],
                                                                           op=ALU.mult), reads=[ckey, 'invc'], writes=['d16'])
                            T.op('dve', lambda e, ct=ct: e.tensor_tensor(out=diffT[:, ct, 0:16], in0=d16[:, :], in1=uA[:, ct, 16:32],
                                                                         op=ALU.subtract), reads=['d16'] + ukeys, writes=[('diffT', ct)])
                            T.op('pe', lambda e, jt=jt: e.matmul(pb[7][:, 256:260], lhsT=stt[:, 128 * jt:128 * jt + 128], rhs=sel[:, gi, :],
                                                                 start=True, stop=True), reads=['stt', 'sel'], writes=[PBK[7]])
                            T.op('dve', lambda e, ct=ct: e.tensor_tensor(out=ssum[:, :], in0=pb[7][:, 256:260], in1=usT[:, ct, :], op=ALU.add),
                                 reads=[PBK[7], ('usT', ct)], writes=['ssum'])
                            T.op('dve', lambda e, ct=ct: e.scalar_tensor_tensor(
                                out=diffT[:, ct, OWN:OWN + 4], in0=ssum[:, :], scalar=1.0 / win, in1=usT[:, ct, :],
                                op0=ALU.mult, op1=ALU.subtract), reads=['ssum', ('usT', ct)], writes=[('diffT', ct)])
                        for dt_ in range(2):
                            for (c0, N) in CHUNKS:
                                bi = mmbank()

                                def f(e, bi=bi, dt_=dt_, c0=c0, N=N):
                                    for cc in range(2):
                                        ins = e.matmul(pb[bi][:, 0:N], lhsT=wpl[:, cc, 128 * dt_:128 * dt_ + 128],
                                                       rhs=diffT[:, cc, c0:c0 + N], start=(cc == 0), stop=(cc == 1))
                                    return ins
                                T.op('pe', f, reads=['wpl', ('diffT', 0), ('diffT', 1)], writes=[PBK[bi]])
                                jd = 2 * gi + dt_
                                T.op('dve', lambda e, bi=bi, jd=jd, c0=c0, N=N: e.tensor_scalar(
                                    out=mixT[:, 8 + jd, c0:c0 + N], in0=pb[bi][:, 0:N], scalar1=psc[:, jd:jd + 1], scalar2=None,
                                    op0=ALU.mult), reads=[PBK[bi], 'psc'], writes=[('mixTp', jd, c0)])
                    T.barrier()
            T.barrier()

            _stop(4)
            stream = sb(es, "stream", [128, 9, 2048], F32)
            xnT = mixT
            comb = sb(es, "comb", [128, 9, 32], F32)
            st1 = sb(es, "st1", [128, 8], F32)
            LB = {}

            def alloc_ln(scope, tag):
                LB['gam'] = sb(scope, "gam" + tag, [128, 2048], F32)
                LB['bet'] = sb(scope, "bet" + tag, [128, 2048], F32)
                LB['sq'] = sb(scope, "sq" + tag, [128, 2048], F32)
                LB['xnb'] = sb(scope, "xnb" + tag, [128, 2048], BF16)

            def layer_norm(tt, nt, eps):
                v = stream[0:nt, tt, :]
                k = ('stream', tt)
                gam, bet, sq = LB['gam'], LB['bet'], LB['sq']
                T.op('dve', lambda e: e.tensor_reduce(out=st1[0:nt, 0:1], in_=v, axis=AX.X, op=ALU.add), reads=[k], writes=['st_a'])
                T.op('pool', lambda e: e.tensor_tensor(out=sq[0:nt, :], in0=v, in1=v, op=ALU.mult), reads=[k], writes=['sq'])
                T.op('dve', lambda e: e.tensor_reduce(out=st1[0:nt, 1:2], in_=sq[0:nt, :], axis=AX.X, op=ALU.add),
                     reads=['sq'], writes=['st_b'])
                T.op('dve', lambda e: e.tensor_scalar(out=st1[0:nt, 2:4], in0=st1[0:nt, 0:2], scalar1=1.0 / 2048, scalar2=None,
                                                      op0=ALU.mult), reads=['st_a', 'st_b'], writes=['st_c'])
                T.op('dve', lambda e: e.tensor_tensor(out=st1[0:nt, 4:5], in0=st1[0:nt, 2:3], in1=st1[0:nt, 2:3], op=ALU.mult),
                     reads=['st_c'], writes=['st_d'])
                T.op('dve', lambda e: e.tensor_tensor(out=st1[0:nt, 5:6], in0=st1[0:nt, 3:4], in1=st1[0:nt, 4:5], op=ALU.subtract),
                     reads=['st_c', 'st_d'], writes=['st_e'])
                T.op('dve', lambda e: e.tensor_scalar(out=st1[0:nt, 7:8], in0=st1[0:nt, 5:6], scalar1=eps, scalar2=None,
                                                      op0=ALU.add), reads=['st_e'], writes=['st_g'])
                T.op('act', lambda e: e.activation(out=st1[0:nt, 7:8], in_=st1[0:nt, 7:8], func=AF.Sqrt), reads=['st_g'], writes=['st_g'])
                T.op('dve', lambda e: e.reciprocal(out=st1[0:nt, 6:7], in_=st1[0:nt, 7:8]), reads=['st_g'], writes=['st_f'])
                T.op('dve', lambda e: e.tensor_scalar(out=v, in0=v, scalar1=st1[0:nt, 2:3], scalar2=st1[0:nt, 6:7],
                                                      op0=ALU.subtract, op1=ALU.mult), reads=[k, 'st_c', 'st_f'], writes=[k])
                T.op('pool', lambda e: e.tensor_tensor(out=v, in0=v, in1=gam[0:nt, :], op=ALU.mult), reads=[k, 'gam'], writes=[k])
                T.op('pool', lambda e: e.tensor_tensor(out=v, in0=v, in1=bet[0:nt, :], op=ALU.add), reads=[k, 'bet'], writes=[k])

            def to_T(tt, t0, nt):
                k = ('stream', tt)
                xnb = LB['xnb']
                T.op('act', lambda e: e.copy(out=xnb[0:nt, :], in_=stream[0:nt, tt, :]), reads=[k], writes=['xnb'])
                for g in range(2):
                    bi = mmbank()
                    bank = pb[bi][:].bitcast(BF16)

                    def f(e, g=g, bank=bank):
                        for j in range(8):
                            kk = 8 * g + j
                            ins = e.transpose(out=bank[:, nt * j:nt * j + nt], in_=xnb[0:nt, 128 * kk:128 * kk + 128],
                                              identity=identb[0:nt, 0:nt])
                        return ins
                    T.op('pe', f, reads=['xnb', 'identb'], writes=[PBK[bi]])
                    T.op('dve', lambda e, g=g, bank=bank: e.tensor_copy(out=xnT[:, 8 * g:8 * g + 8, t0:t0 + nt],
                                                                        in_=bank[:, 0:8 * nt].rearrange("q (j c) -> q j c", c=nt)),
                         reads=[PBK[bi]], writes=[('xnT', tt, g)])

            with ExitStack() as pB:
                wo = [sb(pB, "wo%d" % i, [128, 16, 512], BF16) for i in range(2)]
                xres = [sb(pB, "xres%d" % i, [128, 512], F32) for i in range(3)]
                alloc_ln(pB, "B")
                gam, bet = LB['gam'], LB['bet']
                T.dma('sp', "k0", gam[:], ln_gb[0].partition_broadcast(128), writes=['gam'])
                T.dma('sp', "k1", bet[:], ln_gb[1].partition_broadcast(128), writes=['bet'])
                n_x = 0
                for cg in range(4):
                    s = cg % 2
                    T.dma('pool', "wo%d" % s, wo[s][:], w_out[:, 512 * cg:512 * cg + 512].rearrange("(k q) n -> q k n", q=128),
                          writes=[('wo', s)])
                    for tt, (t0, nt) in enumerate(TT):
                        xs_ = n_x % 3
                        n_x += 1
                        src = xh[2048 + t0:2048 + t0 + nt, 512 * cg:512 * cg + 512] if tt < 8 else xs[:, 512 * cg:512 * cg + 512]
                        T.dma('sp', "xres%d" % xs_, xres[xs_][0:nt, :], src, writes=[('xres', xs_)])
                        bi = mmbank()

                        def f(e, bi=bi, s=s, t0=t0, nt=nt):
                            for k in range(16):
                                ins = e.matmul(pb[bi][0:nt, :], lhsT=mixT[:, k, t0:t0 + nt], rhs=wo[s][:, k, :],
                                               start=(k == 0), stop=(k == 15))
                            return ins
                        T.op('pe', f, reads=[('wo', s)], writes=[PBK[bi]])
                        T.op('dve', lambda e, bi=bi, xs_=xs_, tt=tt, nt=nt, cg=cg: e.scalar_tensor_tensor(
                            out=stream[0:nt, tt, 512 * cg:512 * cg + 512], in0=xres[xs_][0:nt, :], scalar=ALPHA, in1=pb[bi][0:nt, :],
                            op0=ALU.mult, op1=ALU.add), reads=[PBK[bi], ('xres', xs_)], writes=[('stream', tt)])
                for tt, (t0, nt) in enumerate(TT):
                    layer_norm(tt, nt, LN_EPS)
                    to_T(tt, t0, nt)
            T.barrier()

            _stop(5)
            with ExitStack() as pC:
                wr = sb(pC, "wr", [128, 16, 36], BF16)
                brow = sb(pC, "brow", [1, 36], BF16)
                lg = sb(pC, "lg", [128, 36], F32)
                rt = sb(pC, "rt", [128, 160], F32)
                T.dma('pool', "k0", wr[:, :, 0:4], w_gr.rearrange("(k q) n -> q k n", q=128), writes=['wr'])
                T.dma('pool', "k1", wr[:, :, 4:36], w_er.rearrange("(k q) n -> q k n", q=128), writes=['wr'])
                T.dma('pool', "k2", brow[:], b_r[:, :], writes=['brow'])
                BIG = 1.0e9
                for tt, (t0, nt) in enumerate(TT):
                    bi = mmbank()

                    def f(e, bi=bi, t0=t0, nt=nt):
                        for k in range(16):
                            e.matmul(pb[bi][0:nt, 0:36], lhsT=xnT[:, k, t0:t0 + nt], rhs=wr[:, k, :], start=(k == 0), stop=False)
                        return e.matmul(pb[bi][0:nt, 0:36], lhsT=ones_b[0:1, 0:nt], rhs=brow[0:1, :], start=False, stop=True)
                    T.op('pe', f, reads=['wr', 'brow', 'ones_b'], writes=[PBK[bi]])
                    R = lambda a, b_: rt[0:nt, a:b_]
                    ops = []
                    V = lambda fn: ops.append(fn)
                    V(lambda e: e.tensor_copy(out=lg[0:nt, :], in_=pb[bi][0:nt, 0:36]))
                    V(lambda e: e.tensor_reduce(out=R(0, 1), in_=lg[0:nt, 0:4], axis=AX.X, op=ALU.max))
                    V(lambda e: e.tensor_scalar(out=R(4, 8), in0=lg[0:nt, 0:4], scalar1=R(0, 1), scalar2=None, op0=ALU.is_equal))
                    V(lambda e: e.tensor_scalar(out=R(8, 12), in0=lg[0:nt, 0:4], scalar1=R(0, 1), scalar2=None, op0=ALU.subtract))
                    V(('act', lambda e: e.activation(out=R(8, 12), in_=R(8, 12), func=AF.Exp)))
                    V(lambda e: e.tensor_reduce(out=R(1, 2), in_=R(8, 12), axis=AX.X, op=ALU.add))
                    V(lambda e: e.reciprocal(out=R(2, 3), in_=R(1, 2)))
                    V(lambda e: e.tensor_scalar(out=R(12, 16), in0=R(4, 8), scalar1=-1.0, scalar2=BIG, op0=ALU.add, op1=ALU.mult))
                    V(lambda e: e.tensor_tensor(out=R(16, 48).rearrange("q (g c) -> q g c", c=8),
                                                in0=lg[0:nt, 4:36].rearrange("q (g c) -> q g c", c=8),
                                                in1=R(12, 16).unsqueeze(2).to_broadcast([nt, 4, 8]), op=ALU.add))
                    V(lambda e: e.tensor_reduce(out=R(3, 4), in_=R(16, 48), axis=AX.X, op=ALU.max))
                    V(lambda e: e.tensor_scalar(out=R(48, 80), in0=R(16, 48), scalar1=R(3, 4), scalar2=None, op0=ALU.is_equal))
                    V(lambda e: e.scalar_tensor_tensor(out=R(80, 112), in0=R(48, 80), scalar=-BIG, in1=R(16, 48),
                                                       op0=ALU.mult, op1=ALU.add))
                    V(lambda e: e.tensor_reduce(out=R(112, 113), in_=R(80, 112), axis=AX.X, op=ALU.max))
                    V(lambda e: e.tensor_scalar(out=R(116, 148), in0=R(80, 112), scalar1=R(112, 113), scalar2=None, op0=ALU.is_equal))
                    V(lambda e: e.tensor_tensor(out=R(113, 114), in0=R(112, 113), in1=R(3, 4), op=ALU.subtract))
                    V(('act', lambda e: e.activation(out=R(113, 114), in_=R(113, 114), func=AF.Exp)))
                    V(lambda e: e.tensor_scalar(out=R(114, 115), in0=R(113, 114), scalar1=1.0, scalar2=None, op0=ALU.add))
                    V(lambda e: e.reciprocal(out=R(115, 116), in_=R(114, 115)))
                    V(lambda e: e.tensor_scalar(out=R(148, 149), in0=R(2, 3), scalar1=1.0 / ALPHA, scalar2=None, op0=ALU.mult))
                    V(lambda e: e.tensor_tensor(out=R(149, 150), in0=R(148, 149), in1=R(115, 116), op=ALU.mult))
                    V(lambda e: e.tensor_tensor(out=R(150, 151), in0=R(148, 149), in1=R(149, 150), op=ALU.subtract))
                    V(lambda e: e.tensor_scalar(out=R(48, 80), in0=R(48, 80), scalar1=R(149, 150), scalar2=None, op0=ALU.mult))
                    V(lambda e: e.scalar_tensor_tensor(out=comb[0:nt, tt, :], in0=R(116, 148), scalar=R(150, 151), in1=R(48, 80),
                                                       op0=ALU.mult, op1=ALU.add))
                    first = True
                    for o in ops:
                        eng, fn = (o if isinstance(o, tuple) else ('dve', o))
                        T.op(eng, fn, reads=['rt'] + ([PBK[bi]] if first else []), writes=['rt'])
                        first = False
                T.barrier()

                wg = [sb(pC, "wg%d" % i, [128, 16, 256], BF16) for i in range(2)]
                wu = [sb(pC, "wu%d" % i, [128, 16, 256], BF16) for i in range(2)]
                wd = [sb(pC, "wd%d" % i, [128, 2, 2048], BF16) for i in range(2)]
                hT = [sb(pC, "hT%d" % i, [128, 2, NTOK], BF16) for i in range(2)]
                sg = [sb(pC, "sg%d" % i, [128, 512], BF16) for i in range(2)]
                n_sg = 0
                n_gu = 0
                n_dn = 0
                for ex in range(32):
                    s = ex % 2
                    T.dma('pool', "wg%d" % s, wg[s][:], w_gate[ex].rearrange("(k q) n -> q k n", q=128), writes=[('wg', s)])
                    T.dma('pool', "wu%d" % s, wu[s][:], w_up[ex].rearrange("(k q) n -> q k n", q=128), writes=[('wu', s)])
                    T.dma('pool', "wd%d" % s, wd[s][:], w_down[ex].rearrange("(k q) n -> q k n", q=128), writes=[('wd', s)])
                    for (c0, N) in CHUNKS:
                        for fi in range(2):
                            bg = 2 * (n_gu % 2)
                            bu = bg + 1
                            n_gu += 1

                            def f(e, bg=bg, bu=bu, fi=fi, c0=c0, N=N, s=s):
                                for k in range(16):
                                    e.matmul(pb[bg][:, 0:N], lhsT=wg[s][:, k, 128 * fi:128 * fi + 128], rhs=xnT[:, k, c0:c0 + N],
                                             start=(k == 0), stop=(k == 15))
                                for k in range(16):
                                    ins = e.matmul(pb[bu][:, 0:N], lhsT=wu[s][:, k, 128 * fi:128 * fi + 128], rhs=xnT[:, k, c0:c0 + N],
                                                   start=(k == 0), stop=(k == 15))
                                return ins
                            T.op('pe', f, reads=[('wg', s), ('wu', s)], writes=[PBK[bg], PBK[bu]])
                            q = n_sg % 2
                            n_sg += 1
                            T.op('act', lambda e, q=q, bg=bg, N=N: e.activation(out=sg[q][:, 0:N], in_=pb[bg][:, 0:N], func=AF.Silu),
                                 reads=[PBK[bg]], writes=[('sg', q)])
                            T.op('dve', lambda e, q=q, bu=bu, N=N, fi=fi, c0=c0, s=s: e.tensor_tensor(
                                out=hT[s][:, fi, c0:c0 + N], in0=pb[bu][:, 0:N], in1=sg[q][:, 0:N], op=ALU.mult),
                                reads=[PBK[bu], ('sg', q)], writes=[('hT', s, fi, c0)])
                    hkeys = [('hT', s, fi, c0) for fi in range(2) for (c0, _) in CHUNKS]
                    for tt, (t0, nt) in enumerate(TT):
                        for cg in range(4):
                            bi = 4 + (n_dn % 4)
                            n_dn += 1

                            def f(e, bi=bi, t0=t0, nt=nt, cg=cg, s=s):
                                for fi in range(2):
                                    ins = e.matmul(pb[bi][0:nt, :], lhsT=hT[s][:, fi, t0:t0 + nt], rhs=wd[s][:, fi, 512 * cg:512 * cg + 512],
                                                   start=(fi == 0), stop=(fi == 1))
                                return ins
                            T.op('pe', f, reads=hkeys + [('wd', s)], writes=[PBK[bi]])
                            eng = 'dve' if (cg % 2 == 0) else 'pool'
                            if eng == 'dve':
                                T.op('dve', lambda e, bi=bi, tt=tt, nt=nt, cg=cg, ex=ex: e.scalar_tensor_tensor(
                                    out=stream[0:nt, tt, 512 * cg:512 * cg + 512], in0=pb[bi][0:nt, :], scalar=comb[0:nt, tt, ex:ex + 1],
                                    in1=stream[0:nt, tt, 512 * cg:512 * cg + 512], op0=ALU.mult, op1=ALU.add),
                                    reads=[PBK[bi], ('stream', tt, cg)], writes=[('stream', tt, cg)])
                            else:
                                T.op('dve', lambda e, bi=bi, tt=tt, nt=nt, cg=cg, ex=ex: e.scalar_tensor_tensor(
                                    out=stream[0:nt, tt, 512 * cg:512 * cg + 512], in0=pb[bi][0:nt, :], scalar=comb[0:nt, tt, ex:ex + 1],
                                    in1=stream[0:nt, tt, 512 * cg:512 * cg + 512], op0=ALU.mult, op1=ALU.add),
                                    reads=[PBK[bi], ('stream', tt, cg)], writes=[('stream', tt, cg)])
            T.barrier()

            _stop(6)
            with ExitStack() as pD:
                wpg = [sb(pD, "wpg%d" % i, [128, 16, 512], BF16) for i in range(2)]
                wple = sb(pD, "wple", [128, 2, 2048], BF16)
                pTt = sb(pD, "pT", [128, 2, NTOK], BF16)
                pbf = sb(pD, "pbf", [128, 256], BF16)
                sgm = [sb(pD, "sgm%d" % i, [128, 512], F32) for i in range(2)]
                osb = [sb(pD, "osb%d" % i, [128, 512], F32) for i in range(3)]
                alloc_ln(pD, "D")
                gam, bet = LB['gam'], LB['bet']
                T.dma('sp', "k0", gam[:], ln_gb[2].partition_broadcast(128), writes=['gam'])
                T.dma('sp', "k1", bet[:], ln_gb[3].partition_broadcast(128), writes=['bet'])
                T.dma('pool', "k2", wple[:], w_ple.rearrange("(k q) n -> q k n", q=128), writes=['wple'])
                for tt, (t0, nt) in enumerate(TT):
                    layer_norm(tt, nt, LN_EPS / (ALPHA * ALPHA))
                    to_T(tt, t0, nt)
                    src = pp[t0:t0 + nt, :] if tt < 8 else pps[:, :]
                    T.dma('pool', "pbf", pbf[0:nt, :], src, writes=['pbf'])
                    bi = mmbank()
                    bank = pb[bi][:].bitcast(BF16)

                    def f(e, bank=bank, nt=nt):
                        for j in range(2):
                            ins = e.transpose(out=bank[:, nt * j:nt * j + nt], in_=pbf[0:nt, 128 * j:128 * j + 128],
                                              identity=identb[0:nt, 0:nt])
                        return ins
                    T.op('pe', f, reads=['pbf', 'identb'], writes=[PBK[bi]])
                    T.op('dve', lambda e, bank=bank, t0=t0, nt=nt: e.tensor_copy(
                        out=pTt[:, :, t0:t0 + nt], in_=bank[:, 0:2 * nt].rearrange("q (j c) -> q j c", c=nt)),
                        reads=[PBK[bi]], writes=[('pT', tt)])
                n_o = 0
                for cg in range(4):
                    s = cg % 2
                    T.dma('pool', "wpg%d" % s, wpg[s][:], w_pg[:, 512 * cg:512 * cg + 512].rearrange("(k q) n -> q k n", q=128),
                          writes=[('wpg', s)])
                    for tt, (t0, nt) in enumerate(TT):
                        bg = 2 * (n_o % 2)
                        bp = bg + 1

                        def f(e, bg=bg, bp=bp, s=s, t0=t0, nt=nt, cg=cg):
                            for k in range(16):
                                e.matmul(pb[bg][0:nt, :], lhsT=xnT[:, k, t0:t0 + nt], rhs=wpg[s][:, k, :], start=(k == 0), stop=(k == 15))
                            for j in range(2):
                                ins = e.matmul(pb[bp][0:nt, :], lhsT=pTt[:, j, t0:t0 + nt], rhs=wple[:, j, 512 * cg:512 * cg + 512],
                                               start=(j == 0), stop=(j == 1))
                            return ins
                        T.op('pe', f, reads=[('wpg', s), 'wple', ('pT', tt), ('xnT', tt, 0), ('xnT', tt, 1)], writes=[PBK[bg], PBK[bp]])
                        q = n_o % 2
                        o = n_o % 3
                        n_o += 1
                        T.op('act', lambda e, q=q, bg=bg, nt=nt: e.activation(out=sgm[q][0:nt, :], in_=pb[bg][0:nt, :], func=AF.Sigmoid),
                             reads=[PBK[bg]], writes=[('sgm', q)])
                        T.op('dve', lambda e, q=q, bp=bp, nt=nt: e.tensor_tensor(out=sgm[q][0:nt, :], in0=pb[bp][0:nt, :], in1=sgm[q][0:nt, :],
                                                                                op=ALU.mult), reads=[PBK[bp], ('sgm', q)], writes=[('sgm', q)])
                        T.op('pool', lambda e, q=q, o=o, tt=tt, nt=nt, cg=cg: e.tensor_tensor(
                            out=osb[o][0:nt, :], in0=sgm[q][0:nt, :], in1=stream[0:nt, tt, 512 * cg:512 * cg + 512], op=ALU.add),
                            reads=[('sgm', q), ('stream', tt)], writes=[('osb', o)])
                        T.dma('sp', "osb%d" % o, y[t0:t0 + nt, 512 * cg:512 * cg + 512], osb[o][0:nt, :], reads=[('osb', o)])
        except _Stop:
            pass
        DEAD[0] = False
        T.barrier()
    return nc


def _consts(core):
    own0 = OWN * core
    inv_freq = (np.float32(500000.0) ** (-(np.arange(0, 16, 2, dtype=np.float32)) / np.float32(16))).astype(np.float32)
    pos = np.concatenate([np.arange(own0 - 2048, own0 + OWN), np.full(4, 8192)]).astype(np.float32)
    ang = (pos[None, :] * inv_freq[:, None]).astype(np.float32).astype(np.float64)
    C = np.ones((128, 3076), np.float32)
    S = np.zeros((128, 3076), np.float32)
    R = np.zeros((128, 128), np.float32)
    for hh in range(2):
        b = 64 * hh
        C[b:b + 8] = np.cos(ang)
        C[b + 8:b + 16] = np.cos(ang)
        S[b:b + 8] = -np.sin(ang)
        S[b + 8:b + 16] = np.sin(ang)
        for i in range(8):
            R[b + 8 + i, b + i] = 1.0
            R[b + i, b + 8 + i] = 1.0
    kk = np.arange(128)[:, None]
    qq = np.arange(128)[None, :]
    gen = np.concatenate([(kk >= qq), (kk <= qq)], axis=1).astype(np.float32)
    mk = np.zeros((128, 4, 256), np.float32)
    mk[:, 0] = gen
    first = gen.copy()
    if core == 0:
        first[:, 0:128] = 0.0
    mk[:, 1] = first
    mk[:, 2] = first
    q64 = np.arange(64)[None, :]
    m_glob = 64 * core - 128 + kk
    b0 = ((kk >= q64) & (m_glob >= 0)).astype(np.float32)
    b1 = (kk <= q64).astype(np.float32)
    mk[:, 3, 0:64] = b0
    mk[:, 3, 64:128] = b1
    invc = np.zeros((128, 4, 16), np.float32)
    for gi, win in enumerate(POOL_WINDOWS):
        p_ = own0 + np.arange(16)
        invc[:, gi, :] = (1.0 / np.minimum(p_ + 1, win)).astype(np.float32)[None, :]
    sel = np.zeros((60, 4, 4), np.float32)
    for b in range(4):
        for gi, win in enumerate(POOL_WINDOWS):
            for j in range(15):
                if j >= 15 - (win - 1):
                    sel[b * 15 + j, gi, b] = 1.0
    return dict(ropeC=C, ropeS=S, rperm=R, mk=mk, invc=invc, sel=sel)


_PROG = None
DEBUG_CORES = [None]


def kernel(**inp):
    global _PROG
    if _PROG is None:
        _PROG = build_program()
    nc = _PROG
    f = lambda a: np.ascontiguousarray(a, dtype=np.float32)
    x = inp["x_prompt"][0]
    xpad = np.concatenate([np.zeros((2048, 2048), np.float32), x], axis=0)
    shared = dict(
        w_in=f(inp["w_in"][0]), w_out=f(inp["w_out"][0]), w_pool=f(inp["w_pool"][0]),
        pool_scale=f(inp["pool_scale"][0].reshape(1024, 1)),
        ln1_g=f(inp["ln1_g"]), ln1_b=f(inp["ln1_b"]), ln2_g=f(inp["ln2_g"]), ln2_b=f(inp["ln2_b"]),
        w_gr=f(inp["w_group_router"][0]), w_er=f(inp["w_expert_router"][0]),
        b_r=f(np.concatenate([inp["b_group_router"][0], inp["b_expert_router"][0]])[None, :]),
        w_gate=f(inp["w_gate"][0]), w_up=f(inp["w_up"][0]), w_down=f(inp["w_down"][0]),
        w_ple=f(inp["w_ple"][0]), w_pg=f(inp["w_ple_gate"][0]))
    caches = [inp["cache_kv_w128_d1"][0], inp["cache_kv_w512_d4"][0], inp["cache_kv_w2048_d16"][0]]
    in_maps = []
    for c in range(NCORE):
        m = dict(shared)
        m["xh"] = f(xpad[OWN * c:OWN * c + 3072])
        m["xs"] = f(inp["x_sample"][4 * c:4 * c + 4, 0])
        for p in range(3):
            m["c%d" % p] = f(caches[p][4 * c:4 * c + 4].reshape(4, CL[p], 2, 1024))
        m["spool"] = f(inp["state_pool"][0, 4 * c:4 * c + 4])
        m["pp"] = f(inp["p_prompt"][0, 0, OWN * c:OWN * c + OWN])
        m["pps"] = f(inp["p_sample"][0, 4 * c:4 * c + 4, 0])
        m.update(_consts(c))
        in_maps.append(m)
    if DEBUG_CORES[0] is not None:
        sub = [in_maps[c] for c in DEBUG_CORES[0]]
        return run_bass_kernel_spmd(nc, sub, core_ids=list(range(len(sub)))).results
    res = run_bass_kernel_spmd(nc, in_maps, core_ids=list(range(NCORE))).results
    y_p = np.concatenate([r["y"][0:OWN] for r in res], 0)[None]
    y_s = np.concatenate([r["y"][OWN:OWN + 4] for r in res], 0)[:, None, :]
    kv0 = res[7]["kvp0"].reshape(1, 1, 128, 2, 16, 64)
    kv1 = res[7]["kvp1"].reshape(1, 1, 512, 2, 16, 64)
    kv2 = np.concatenate([res[6]["kvp2"], res[7]["kvp2"]], 0).reshape(1, 1, 2048, 2, 16, 64)
    pl = res[7]["poolp"][1:16].reshape(1, 1, 15, 1024)
    ks = [np.concatenate([r["kvs%d" % p] for r in res], 0).reshape(1, 32, CL[p], 2, 16, 64) for p in range(3)]
    pls = np.concatenate([r["pools"] for r in res], 0).reshape(1, 32, 15, 1024)
    outs = (y_p, y_s, kv0, kv1, kv2, pl, ks[0], ks[1], ks[2], pls)
    return tuple(np.ascontiguousarray(o, dtype=np.float32) for o in outs)
```

```python
import numpy as np
from contextlib import ExitStack
import concourse.bass as bass
import concourse.mybir as mybir
from concourse.bass_utils import run_bass_kernel_spmd

F32 = mybir.dt.float32
BF16 = mybir.dt.bfloat16
AF = mybir.ActivationFunctionType
ALU = mybir.AluOpType
AX = mybir.AxisListType

NCORE = 8
OWN = 1024
HALO = [128, 512, 2048]
DIL = [1, 4, 16]
CL = [128, 512, 2048]
ALPHA = 2.0 ** 0.25
LN_EPS = 1e-5
POOL_WINDOWS = (2, 4, 8, 16)
NTOK = OWN + 4
TT = [(128 * i, 128) for i in range(8)] + [(1024, 4)]
CHUNKS = [(0, 512), (512, 512), (1024, 4)]
ENGS = ('pe', 'act', 'dve', 'pool', 'sp')


class _Stop(Exception):
    pass


KSTOP = [99]


DEAD = [False]
KOPS = [None]
NOATT = [False]
NOSAMP = [False]
NOPS = [0]


def _stop(n):
    if KOPS[0] == -1:
        print("stop marker", n, "ops", NOPS[0])
    if KSTOP[0] <= n:
        DEAD[0] = True


class Trk:
    def __init__(self, nc, es):
        self.nc = nc
        self.es = es
        self.eng = {'pe': nc.tensor, 'act': nc.scalar, 'dve': nc.vector, 'pool': nc.gpsimd, 'sp': nc.sync}
        self.sem = {e: es.enter_context(nc.semaphore("c_" + e)) for e in ('pe', 'act', 'dve', 'pool')}
        self.cnt = {e: 0 for e in self.sem}
        self.waited = {e: {} for e in ENGS}
        self.bufs = {}
        self.chan = {}

    def _buf(self, k):
        b = self.bufs.get(k)
        if b is None:
            b = self.bufs[k] = {'w': {}, 'r': {}}
        return b

    @staticmethod
    def _flat(keys):
        out = []
        for k in keys:
            if isinstance(k, list):
                out.extend(k)
            else:
                out.append(k)
        return out

    def _deps(self, eng, reads, writes):
        deps = {}

        def add(d, skip):
            for s, v in d.items():
                if skip and s == eng:
                    continue
                if deps.get(s, 0) < v:
                    deps[s] = v
        for k in reads:
            add(self._buf(k)['w'], False)
            if isinstance(k, tuple) and k[0] == 'pb':
                add(self._buf(k)['r'], True)
        for k in writes:
            b = self._buf(k)
            add(b['w'], True)
            add(b['r'], True)
        return deps

    def _waits(self, eng, deps):
        e = self.eng[eng]
        for s, v in deps.items():
            if v <= 0 or self.waited[eng].get(s, 0) >= v:
                continue
            semh = self.sem[s] if isinstance(s, str) else self.chan[s[1]][0]
            e.wait_ge(semh, v)
            self.waited[eng][s] = v

    def op(self, eng, fn, reads=(), writes=()):
        NOPS[0] += 1
        if KOPS[0] is not None and NOPS[0] > KOPS[0]:
            DEAD[0] = True
        if DEAD[0]:
            return None
        reads, writes = self._flat(reads), self._flat(writes)
        self._waits(eng, self._deps(eng, reads, writes))
        ins = fn(self.eng[eng])
        self.cnt[eng] += 1
        ins.then_inc(self.sem[eng], 1)
        v = self.cnt[eng]
        for k in reads:
            self._buf(k)['r'][eng] = v
        for k in writes:
            b = self._buf(k)
            b['w'] = {eng: v}
            b['r'] = {}
        return ins

    def dma(self, eng, chan, out, in_, reads=(), writes=()):
        NOPS[0] += 1
        if KOPS[0] is not None and NOPS[0] > KOPS[0]:
            DEAD[0] = True
        if DEAD[0]:
            return None
        reads, writes = self._flat(reads), self._flat(writes)
        if chan not in self.chan:
            self.chan[chan] = [self.es.enter_context(self.nc.semaphore("d_" + chan)), 0]
        self._waits(eng, self._deps(None, reads, writes))
        c = self.chan[chan]
        c[1] += 16
        self.eng[eng].dma_start(out=out, in_=in_).then_inc(c[0], 16)
        key = ('ch', chan)
        for k in reads:
            self._buf(k)['r'][key] = c[1]
        for k in writes:
            b = self._buf(k)
            b['w'] = {key: c[1]}
            b['r'] = {}

    def barrier(self, engines=ENGS, final=False):
        if DEAD[0]:
            return
        deps = {e: self.cnt[e] for e in self.sem}
        deps.update({('ch', n): c[1] for n, c in self.chan.items() if final or not n.startswith("cc")})
        for e in engines:
            self._waits(e, deps)
        self.bufs = {}


def build_program():
    nc = bass.Bass("TRN2", target_bir_lowering=False)
    DEAD[0] = False
    NOPS[0] = 0

    def din(name, shape):
        return nc.dram_tensor(name, list(shape), F32, kind="ExternalInput").ap()

    def dout(name, shape):
        return nc.dram_tensor(name, list(shape), F32, kind="ExternalOutput").ap()

    xh = din("xh", [3072, 2048])
    xs = din("xs", [4, 2048])
    c_in = [din("c%d" % p, [4, CL[p], 2, 1024]) for p in range(3)]
    spool = din("spool", [4, 15, 1024])
    pp = din("pp", [1024, 256])
    pps = din("pps", [4, 256])
    w_in = din("w_in", [2048, 10240])
    w_out = din("w_out", [2048, 2048])
    w_pool = din("w_pool", [4, 256, 256])
    pool_scale = din("pool_scale", [1024, 1])
    ln_gb = [din(n, [1, 2048]) for n in ("ln1_g", "ln1_b", "ln2_g", "ln2_b")]
    w_gr = din("w_gr", [2048, 4])
    w_er = din("w_er", [2048, 32])
    b_r = din("b_r", [1, 36])
    w_gate = din("w_gate", [32, 2048, 256])
    w_up = din("w_up", [32, 2048, 256])
    w_down = din("w_down", [32, 256, 2048])
    w_ple = din("w_ple", [256, 2048])
    w_pg = din("w_pg", [2048, 2048])
    ropeC_d = din("ropeC", [128, 3076])
    ropeS_d = din("ropeS", [128, 3076])
    rperm_d = din("rperm", [128, 128])
    mk_d = din("mk", [128, 4, 256])
    invc_d = din("invc", [128, 4, 16])
    sel_d = din("sel", [60, 4, 4])

    y = dout("y", [NTOK, 2048])
    kvp = [dout("kvp0", [128, 2, 1024]), dout("kvp1", [512, 2, 1024]), dout("kvp2", [1024, 2, 1024])]
    KVP_FROM = [OWN - 128, OWN - 512, 0]
    poolp = dout("poolp", [16, 1024])
    kvs = [dout("kvs%d" % p, [4, CL[p], 2, 1024]) for p in range(3)]
    pools = dout("pools", [4, 15, 1024])

    with ExitStack() as es:
        T = Trk(nc, es)
        try:

            def sb(scope, name, shape, dt):
                return scope.enter_context(nc.sbuf_tensor("s_" + name, list(shape), dt))

            pb = [es.enter_context(nc.psum_tensor("pb%d" % i, [128, 512], F32)) for i in range(8)]
            PBK = [('pb', i) for i in range(8)]

            ident = sb(es, "ident", [128, 128], F32)
            identb = sb(es, "identb", [128, 128], BF16)
            ones_f = sb(es, "ones_f", [128, 128], F32)
            ones_b = sb(es, "ones_b", [128, 128], BF16)
            T.op('pool', lambda e: e.memset(ident[:], 0.0), writes=['ident'])
            T.op('pool', lambda e: e.affine_select(out=ident[:], in_=ident[:], pattern=[[-1, 128]],
                                                   compare_op=ALU.not_equal, fill=1.0, base=0, channel_multiplier=1),
                 reads=['ident'], writes=['ident'])
            T.op('pool', lambda e: e.tensor_copy(out=identb[:], in_=ident[:]), reads=['ident'], writes=['identb'])
            T.op('pool', lambda e: e.memset(ones_f[:], 1.0), writes=['ones_f'])
            T.op('pool', lambda e: e.memset(ones_b[:], 1.0), writes=['ones_b'])

            for p in range(3):
                L = CL[p]
                for b in range(4):
                    r0 = 1
                    while r0 < L:
                        r1 = min(L, r0 + 512)
                        T.dma('sp', "cc%d" % ((b + p) % 4), kvs[p][b, r0 - 1:r1 - 1, :, :], c_in[p][b, r0:r1, :, :])
                        r0 = r1
            for b in range(4):
                T.dma('sp', "cc%d" % b, pools[b, 0:14, :], spool[b, 1:15, :])

            _stop(0)
            mixT = sb(es, "mixT", [128, 16, NTOK], BF16)

            with ExitStack() as pa:
                xT = sb(pa, "xT", [128, 16, 3072], BF16)
                xsT = sb(pa, "xsT", [128, 16, 4], BF16)
                ropeC = sb(pa, "ropeC_s", [128, 3076], BF16)
                ropeS = sb(pa, "ropeS_s", [128, 3076], BF16)
                rperm = sb(pa, "rperm_s", [128, 128], BF16)
                mk = sb(pa, "mk_s", [128, 4, 256], BF16)
                T.dma('pool', "k0", ropeC[:], ropeC_d[:, :], writes=['ropeC'])
                T.dma('pool', "k1", ropeS[:], ropeS_d[:, :], writes=['ropeS'])
                T.dma('pool', "k2", rperm[:], rperm_d[:, :], writes=['rperm'])
                T.dma('pool', "k3", mk[:], mk_d[:, :, :], writes=['mk'])

                with ExitStack() as px:
                    xb = [sb(px, "xb%d" % i, [128, 2048], BF16) for i in range(3)]
                    ptb = [pb[3 + g][:].bitcast(BF16) for g in range(4)]
                    for t in range(25):
                        s = t % 3
                        nt = 128 if t < 24 else 4
                        src = xh[128 * t:128 * t + 128, :] if t < 24 else xs[:, :]
                        T.dma('pool', "xb%d" % s, xb[s][0:nt, :], src, writes=[('xb', s)])
                        for g in range(2):
                            bi = 2 * (t % 2) + g
                            bank = ptb[bi]
                            w = nt

                            def f(e, g=g, s=s, bank=bank, nt=nt, w=w):
                                for j in range(8):
                                    k = 8 * g + j
                                    ins = e.transpose(out=bank[:, w * j:w * j + w], in_=xb[s][0:nt, 128 * k:128 * k + 128],
                                                      identity=identb[0:nt, 0:nt])
                                return ins
                            T.op('pe', f, reads=[('xb', s), 'identb'], writes=[PBK[3 + bi]])
                            dst = xT[:, 8 * g:8 * g + 8, 128 * t:128 * t + 128] if t < 24 else xsT[:, 8 * g:8 * g + 8, :]
                            srcv = bank[:, 0:8 * w].rearrange("p (j c) -> p j c", c=w)
                            if g == 0:
                                T.op('act', lambda e, dst=dst, srcv=srcv: e.copy(out=dst, in_=srcv),
                                     reads=[PBK[3 + bi]], writes=[('xT', t, g)])
                            else:
                                T.op('dve', lambda e, dst=dst, srcv=srcv: e.tensor_copy(out=dst, in_=srcv),
                                     reads=[PBK[3 + bi]], writes=[('xT', t, g)])
                    T.barrier()
                _stop(1)

                NSLAB = 4
                slab = [sb(pa, "slab%d" % i, [128, 16, 128], BF16) for i in range(NSLAB)]
                pat = ExitStack()
                kT = sb(pat, "kT", [128, 3072], BF16)
                ksT = sb(pat, "ksT", [128, 4], BF16)
                qT = sb(pat, "qT", [128, NTOK], BF16)
                V_ext = sb(pat, "V_ext", [128, 32, 2, 65], BF16)
                Vs_ext = sb(pat, "Vs_ext", [4, 2, 65], BF16)
                zb = [sb(pat, "zb%d" % i, [128, 512], BF16) for i in range(2)]
                t1s = [sb(pat, "t1_%d" % i, [128, 512], F32) for i in range(2)]
                t2s = [sb(pat, "t2_%d" % i, [128, 512], F32) for i in range(2)]
                kf = sb(pat, "kf", [128, 512], F32)
                ksf = sb(pat, "ksf", [128, 4], F32)
                Eb = [sb(pat, "E%d" % i, [128, 256], BF16) for i in range(4)]
                stg = [sb(pat, "stg%d" % i, [128, 512], F32) for i in range(2)]
                stv = [sb(pat, "stv%d" % i, [128, 128], F32) for i in range(4)]
                sts = [sb(pat, "sts%d" % i, [4, 128], F32) for i in range(2)]
                kg = [sb(pat, "kg%d" % i, [128, 128], BF16) for i in range(4)]
                kcT = [sb(pat, "kcT%d" % i, [128, 128], BF16) for i in range(4)]
                vg = [sb(pat, "vg%d" % i, [128, 2, 65], BF16) for i in range(4)]
                qz = sb(pat, "qz", [128, 2, 4], BF16)
                Es = sb(pat, "Es", [128, 8], BF16)
                En = sb(pat, "En", [4, 8], BF16)
                Tsamp = sb(pat, "Tsamp", [65, 8], F32)
                rden = sb(pat, "rden", [65, 512], F32)
                bcs = sb(pat, "bcs", [64, 512], F32)
                T.op('pool', lambda e: e.memset(V_ext[:], 1.0), writes=['V_ext'])
                T.op('pool', lambda e: e.memset(Vs_ext[:], 1.0), writes=['Vs_ext'])
                T.op('pool', lambda e: e.memset(qz[:], 0.0), writes=['qz'])
                for i in range(4):
                    T.op('pool', lambda e, i=i: e.memset(vg[i][:], 1.0), writes=[('vg', i)])

                state = {'slab': 0, 'mm': 0, 'E': 0, 'stg': 0, 'stv': 0, 'sts': 0, 'kg': 0, 'vg': 0}

                def load_slab(col0):
                    s = state['slab'] % NSLAB
                    state['slab'] += 1
                    T.dma('pool', "slab%d" % s, slab[s][:],
                          w_in[:, col0:col0 + 128].rearrange("(k p) n -> p k n", p=128), writes=[('slab', s)])
                    return s

                def mmbank():
                    i = state['mm'] % 2
                    state['mm'] += 1
                    return i

                def proj_fm(s, rhs_fn, N):
                    bi = mmbank()

                    def f(e):
                        for k in range(16):
                            ins = e.matmul(pb[bi][:, 0:N], lhsT=slab[s][:, k, :], rhs=rhs_fn(k),
                                           start=(k == 0), stop=(k == 15))
                        return ins
                    T.op('pe', f, reads=[('slab', s)], writes=[PBK[bi]])
                    return bi

                def rope(bi, N, tc0, dst_bf, dst_key, want_f32=None):
                    z = zb[bi]
                    t1, t2 = t1s[bi], t2s[bi]
                    k1, k2 = ('t1', bi), ('t2', bi)
                    T.op('act', lambda e: e.copy(out=z[:, 0:N], in_=pb[bi][:, 0:N]), reads=[PBK[bi]], writes=[('zb', bi)])
                    T.op('pe', lambda e: e.matmul(pb[2][:, 0:N], lhsT=rperm[:], rhs=z[:, 0:N], start=True, stop=True),
                         reads=[('zb', bi), 'rperm'], writes=[PBK[2]])
                    T.op('dve', lambda e: e.tensor_tensor(out=t1[:, 0:N], in0=pb[bi][:, 0:N], in1=ropeC[:, tc0:tc0 + N],
                                                          op=ALU.mult), reads=[PBK[bi], 'ropeC'], writes=[k1])
                    T.op('dve', lambda e: e.tensor_tensor(out=t2[:, 0:N], in0=pb[2][:, 0:N], in1=ropeS[:, tc0:tc0 + N],
                                                          op=ALU.mult), reads=[PBK[2], 'ropeS'], writes=[k2])
                    if want_f32 is None:
                        T.op('pool', lambda e: e.tensor_tensor(out=dst_bf, in0=t1[:, 0:N], in1=t2[:, 0:N], op=ALU.add),
                             reads=[k1, k2], writes=[dst_key])
                    else:
                        fdst, fkey = want_f32
                        T.op('pool', lambda e: e.tensor_tensor(out=fdst, in0=t1[:, 0:N], in1=t2[:, 0:N], op=ALU.add),
                             reads=[k1, k2], writes=[fkey])
                        T.op('act', lambda e: e.copy(out=dst_bf, in_=fdst), reads=[fkey], writes=[dst_key])

                for hg in range(8):
                    if hg == 1:
                        _stop(2)
                    TH = [[pb[3], pb[4]], [pb[5], pb[6]]]
                    THK = [[PBK[3], PBK[4]], [PBK[5], PBK[6]]]
                    for hh_ in range(2):
                        for bk_ in range(2):
                            T.op('dve', lambda e, hh_=hh_, bk_=bk_: e.memset(TH[hh_][bk_][0:65, :], 0.0), writes=[THK[hh_][bk_]])
                    for p in range(3):
                        d = DIL[p]
                        halo = HALO[p]
                        s0 = 2048 - halo
                        Tp = halo + OWN
                        qc = p * 3072 + hg * 128
                        s_k = load_slab(qc + 1024)
                        s_v = load_slab(qc + 2048)
                        s_q = load_slab(qc)
                        for b in range(4):
                            T.dma('pool', "kg%d" % b, kg[b][:, :], c_in[p][b, 0:CL[p]:d, 0, hg * 128:hg * 128 + 128],
                                  writes=[('kg', b)])
                            T.dma('pool', "vg%d" % b, vg[b][:, :, 0:64],
                                  c_in[p][b, 0:CL[p]:d, 1, hg * 128:hg * 128 + 128].rearrange("q (h c) -> q h c", c=64),
                                  writes=[('vg', b)])
                        pend_rope = []
                        c = 0
                        while c < Tp:
                            N = min(512, Tp - c)
                            if halo == 128 and c == 0:
                                N = 128
                            xc = s0 + c
                            bi = proj_fm(s_k, lambda k, xc=xc, N=N: xT[:, k, xc:xc + N], N)
                            def post_k(bi=bi, N=N, xc=xc, c=c):
                                tau0 = xc - 2048
                                need = (tau0 + N > KVP_FROM[p]) and tau0 >= 0
                                if need:
                                    rope(bi, N, xc, kT[:, c:c + N], ('kT', c), want_f32=(kf[:, 0:N], 'kf'))
                                    g = state['stg'] % 2
                                    state['stg'] += 1
                                    j0 = max(0, (KVP_FROM[p] - tau0) // 128)
                                    nj = N // 128

                                    def f(e, j0=j0, nj=nj, N=N):
                                        for j in range(j0, nj):
                                            ins = e.transpose(out=pb[7][:, 128 * j:128 * j + 128], in_=kf[:, 128 * j:128 * j + 128],
                                                              identity=ident[:])
                                        return ins
                                    T.op('pe', f, reads=['kf', 'ident'], writes=[PBK[7]])
                                    T.op('dve', lambda e, g=g, j0=j0, N=N: e.tensor_copy(out=stg[g][:, 128 * j0:N],
                                                                                         in_=pb[7][:, 128 * j0:N]),
                                         reads=[PBK[7]], writes=[('stg', g)])
                                    r0 = tau0 + 128 * j0 - KVP_FROM[p]
                                    T.dma('sp', "stg%d" % g,
                                          kvp[p][r0:r0 + 128 * (nj - j0), 0, hg * 128:hg * 128 + 128].rearrange("(j q) c -> q j c", q=128),
                                          stg[g][:, 128 * j0:N].rearrange("q (j c) -> q j c", c=128), reads=[('stg', g)])
                                else:
                                    rope(bi, N, xc, kT[:, c:c + N], ('kT', c))

                            if pend_rope:
                                pend_rope.pop(0)()
                            pend_rope.append(post_k)
                            c += N
                        while pend_rope:
                            pend_rope.pop(0)()
                        if hg == 0 and p == 0:
                            _stop(1.1)
                        bi = proj_fm(s_k, lambda k: xsT[:, k, :], 4)
                        rope(bi, 4, 3072, ksT[:, :], 'ksT', want_f32=(ksf[:, :], 'ksf'))
                        g = state['sts'] % 2
                        state['sts'] += 1
                        T.op('pe', lambda e: e.transpose(out=pb[7][0:4, 0:128], in_=ksf[:, :], identity=ident[:]),
                             reads=['ksf', 'ident'], writes=[PBK[7]])
                        T.op('dve', lambda e, g=g: e.tensor_copy(out=sts[g][:, :], in_=pb[7][0:4, 0:128]),
                             reads=[PBK[7]], writes=[('sts', g)])
                        T.dma('sp', "sts%d" % g, kvs[p][:, CL[p] - 1, 0, hg * 128:hg * 128 + 128], sts[g][:, :],
                              reads=[('sts', g)])
                        if hg == 0 and p == 0:
                            _stop(1.2)
                        Lr = Tp // d
                        ntr = (Lr + 127) // 128
                        tiles = [(r, b) for r in range(d) for b in range(ntr)]
                        for t0 in range(0, len(tiles), 4):
                            grp = tiles[t0:t0 + 4]
                            bi = mmbank()

                            def f(e, grp=grp, bi=bi):
                                for i, (r, b) in enumerate(grp):
                                    nk = min(128, Lr - 128 * b)
                                    a = s0 + d * 128 * b + r
                                    for k in range(16):
                                        ins = e.matmul(pb[bi][0:nk, 128 * i:128 * i + 128],
                                                       lhsT=xT[:, k, a:a + d * (nk - 1) + 1:d], rhs=slab[s_v][:, k, :],
                                                       start=(k == 0), stop=(k == 15))
                                return ins
                            T.op('pe', f, reads=[('slab', s_v)], writes=[PBK[bi]])
                            ng = len(grp)
                            T.op('act', lambda e, t0=t0, ng=ng, bi=bi: e.copy(
                                out=V_ext[:, t0:t0 + ng, :, 0:64],
                                in_=pb[bi][:, 0:128 * ng].rearrange("q (i h c) -> q i h c", h=2, c=64)),
                                reads=[PBK[bi]], writes=['V_ext'])
                            for i, (r, b) in enumerate(grp):
                                nk = min(128, Lr - 128 * b)
                                tau0 = d * 128 * b + r - halo
                                if tau0 >= KVP_FROM[p]:
                                    g = state['stv'] % 4
                                    state['stv'] += 1
                                    T.op('dve', lambda e, g=g, i=i, nk=nk, bi=bi: e.tensor_copy(
                                        out=stv[g][0:nk, :], in_=pb[bi][0:nk, 128 * i:128 * i + 128]),
                                        reads=[PBK[bi]], writes=[('stv', g)])
                                    r0 = tau0 - KVP_FROM[p]
                                    T.dma('sp', "stv%d" % g, kvp[p][r0:r0 + d * (nk - 1) + 1:d, 1, hg * 128:hg * 128 + 128],
                                          stv[g][0:nk, :], reads=[('stv', g)])
                        bi = mmbank()

                        def f(e, bi=bi):
                            for k in range(16):
                                ins = e.matmul(pb[bi][0:4, 0:128], lhsT=xsT[:, k, :], rhs=slab[s_v][:, k, :],
                                               start=(k == 0), stop=(k == 15))
                            return ins
                        T.op('pe', f, reads=[('slab', s_v)], writes=[PBK[bi]])
                        T.op('act', lambda e, bi=bi: e.copy(out=Vs_ext[:, :, 0:64],
                                                            in_=pb[bi][0:4, 0:128].rearrange("q (h c) -> q h c", c=64)),
                             reads=[PBK[bi]], writes=['Vs_ext'])
                        g = state['sts'] % 2
                        state['sts'] += 1
                        T.op('dve', lambda e, g=g, bi=bi: e.tensor_copy(out=sts[g][:, :], in_=pb[bi][0:4, 0:128]),
                             reads=[PBK[bi]], writes=[('sts', g)])
                        T.dma('sp', "sts%d" % g, kvs[p][:, CL[p] - 1, 1, hg * 128:hg * 128 + 128], sts[g][:, :],
                              reads=[('sts', g)])
                        if hg == 0 and p == 0:
                            _stop(1.3)
                        for (c0, N) in CHUNKS:
                            if c0 < OWN:
                                bi = proj_fm(s_q, lambda k, c0=c0, N=N: xT[:, k, 2048 + c0:2048 + c0 + N], N)
                                if pend_rope:
                                    pend_rope.pop(0)()
                                pend_rope.append(lambda bi=bi, N=N, c0=c0: rope(bi, N, 2048 + c0, qT[:, c0:c0 + N], ('qT', c0)))
                            else:
                                bi = proj_fm(s_q, lambda k: xsT[:, k, :], 4)
                                while pend_rope:
                                    pend_rope.pop(0)()
                                rope(bi, 4, 3072, qT[:, OWN:OWN + 4], ('qT', c0))
                                T.op('dve', lambda e: e.tensor_copy(out=qz[0:64, 0, :], in_=qT[0:64, OWN:OWN + 4]),
                                     reads=[('qT', c0)], writes=['qz'])
                                T.op('dve', lambda e: e.tensor_copy(out=qz[64:128, 1, :], in_=qT[64:128, OWN:OWN + 4]),
                                     reads=[('qT', c0)], writes=['qz'])
                        kT_keys = [('kT', cc) for cc in ([0, 128, 640] if halo == 128 else list(range(0, Tp, 512)))]
                        qT_keys = [('qT', 0), ('qT', 512), ('qT', 1024)]

                        if hg == 0 and p == 0:
                            _stop(1.4)
                        pend_pv = []

                        def att_tile(hh, k_aps, q_ap, nq, mask_ap, pv):
                            if NOATT[0]:
                                return
                            es_ = state['E'] % 4
                            sbank = (0, 1, 2, 7)[state['E'] % 4]
                            state['E'] += 1
                            pk = PBK[sbank]
                            base = 0

                            def f(e):
                                for b, (kap, nk) in enumerate(k_aps):
                                    ins = e.matmul(pb[sbank][0:nk, base + nq * b:base + nq * b + nq], lhsT=kap, rhs=q_ap,
                                                   start=True, stop=True)
                                return ins
                            T.op('pe', f, reads=kT_keys + qT_keys + ['ksT'], writes=[pk])
                            W = nq * len(k_aps)
                            T.op('act', lambda e: e.activation(out=Eb[es_][:, 0:W], in_=pb[sbank][:, base:base + W], func=AF.Exp,
                                                               scale=0.125), reads=[pk], writes=[('E', es_)])
                            T.op('dve' if (state['E'] % 2) else 'pool', lambda e: e.tensor_tensor(out=Eb[es_][:, 0:W], in0=Eb[es_][:, 0:W], in1=mask_ap,
                                                                   op=ALU.mult), reads=[('E', es_), 'mk'], writes=[('E', es_)])
                            def do_pv():
                                for (b, vt, ec0, en, bk, oap, colkey) in pv:
                                    nk = k_aps[b][1]
                                    T.op('pe', lambda e, vt=vt, ec0=ec0, en=en, oap=oap, nk=nk: e.matmul(
                                        oap, lhsT=V_ext[0:nk, vt, hh, :], rhs=Eb[es_][0:nk, ec0:ec0 + en], start=False, stop=False,
                                        skip_group_check=True), reads=[('E', es_), 'V_ext'], writes=[THK[hh][bk]])
                            pend_pv.append(do_pv)
                            if len(pend_pv) > 2:
                                pend_pv.pop(0)()

                        for hh in range(2):
                            P0 = 64 * hh
                            if p == 0:
                                for i in range(8):
                                    k_aps = [(kT[P0:P0 + 64, 128 * (i + b):128 * (i + b) + 128], 128) for b in range(2)]
                                    q_ap = qT[P0:P0 + 64, 128 * i:128 * i + 128]
                                    m = mk[:, 1 if i == 0 else 0, :]
                                    bk = i // 4
                                    oap = TH[hh][bk][0:65, 128 * (i % 4):128 * (i % 4) + 128]
                                    pv = [(b, i + b, 128 * b, 128, bk, oap, ('c', i)) for b in range(2)]
                                    att_tile(hh, k_aps, q_ap, 128, m, pv)
                            elif p == 1:
                                for r in range(4):
                                    for j in range(2):
                                        k_aps = [(kT[P0:P0 + 64, 512 * (j + b) + r:512 * (j + b + 1):4], 128) for b in range(2)]
                                        q_ap = qT[P0:P0 + 64, 512 * j + r:512 * (j + 1):4]
                                        m = mk[:, 2 if j == 0 else 0, :]
                                        oap = TH[hh][j][0:65, r:512:4]
                                        pv = [(b, r * 3 + j + b, 128 * b, 128, j, oap, ('s4', r)) for b in range(2)]
                                        att_tile(hh, k_aps, q_ap, 128, m, pv)
                            else:
                                for r in range(16):
                                    k_aps = [(kT[P0:P0 + 64, r:2048:16], 128), (kT[P0:P0 + 64, 2048 + r:3072:16], 64)]
                                    q_ap = qT[P0:P0 + 64, r:1024:16]
                                    m = mk[:, 3, 0:128]
                                    pv = []
                                    for b in range(2):
                                        for bk in range(2):
                                            oap = TH[hh][bk][0:65, r:512:16]
                                            pv.append((b, r * 2 + b, 64 * b + 32 * bk, 32, bk, oap, ('s16', r)))
                                    att_tile(hh, k_aps, q_ap, 64, m, pv)

                        while pend_pv:
                            pend_pv.pop(0)()
                        if hg == 0 and p == 0:
                            _stop(1.5)
                        L = CL[p]
                        for b in (range(4) if not NOSAMP[0] else []):
                            g = b
                            gv = b
                            kb = pb[1][:].bitcast(BF16)
                            T.op('pe', lambda e, g=g, kb=kb: e.transpose(out=kb[:, 0:128], in_=kg[g][:, :], identity=identb[:]),
                                 reads=[('kg', g), 'identb'], writes=[PBK[1]])
                            T.op('dve', lambda e, g=g, kb=kb: e.tensor_copy(out=kcT[g][:, :], in_=kb[:, 0:128]),
                                 reads=[PBK[1]], writes=[('kcT', g)])

                            def f(e, g=g, b=b):
                                for hh in range(2):
                                    ins = e.matmul(pb[0][:, 4 * hh + b:4 * hh + b + 1], lhsT=kcT[g][:, :],
                                                   rhs=qz[:, hh, b:b + 1], start=True, stop=True)
                                return ins
                            T.op('pe', f, reads=[('kcT', g), 'qz'], writes=[PBK[0]])
                            T.op('act', lambda e, b=b: e.activation(out=Es[:, b:8:4], in_=pb[0][:, b:8:4], func=AF.Exp, scale=0.125),
                                 reads=[PBK[0]], writes=['Es'])

                            def f2(e, gv=gv, b=b):
                                for hh in range(2):
                                    ins = e.matmul(pb[0][0:65, 32 + 4 * hh + b:32 + 4 * hh + b + 1], lhsT=vg[gv][:, hh, :],
                                                   rhs=Es[:, 4 * hh + b:4 * hh + b + 1], start=True, stop=False,
                                                   skip_group_check=True)
                                return ins
                            T.op('pe', f2, reads=[('vg', gv), 'Es'], writes=[PBK[0]])
                        if hg == 0 and p == 0:
                            _stop(1.6)
                        def f3(e):
                            for hh in range(2):
                                ins = e.matmul(pb[0][0:4, 16 + 4 * hh:16 + 4 * hh + 4], lhsT=ksT[:, :],
                                               rhs=qz[:, hh, :], start=True, stop=True)
                            return ins
                        T.op('pe', f3, reads=['ksT', 'qz'], writes=[PBK[0]])
                        T.op('act', lambda e: e.activation(out=En[:, :], in_=pb[0][0:4, 16:24], func=AF.Exp, scale=0.125),
                             reads=[PBK[0]], writes=['En'])
                        for hh_ in range(2):
                            T.op('dve', lambda e, hh_=hh_: e.tensor_tensor(out=En[:, 4 * hh_:4 * hh_ + 4], in0=En[:, 4 * hh_:4 * hh_ + 4],
                                                                           in1=identb[0:4, 0:4], op=ALU.mult),
                                 reads=['En', 'identb'], writes=['En'])

                        def f4(e):
                            for hh in range(2):
                                ins = e.matmul(pb[0][0:65, 40 + 4 * hh:40 + 4 * hh + 4], lhsT=Vs_ext[:, hh, :],
                                               rhs=En[:, 4 * hh:4 * hh + 4], start=True, stop=True, skip_group_check=True)
                            return ins
                        T.op('pe', f4, reads=['Vs_ext', 'En'], writes=[PBK[0]])
                        if p == 0:
                            T.op('dve', lambda e: e.tensor_copy(out=Tsamp[:, :], in_=pb[0][0:65, 32:40]), reads=[PBK[0]], writes=['TS'])
                        else:
                            T.op('dve', lambda e: e.tensor_tensor(out=Tsamp[:, :], in0=pb[0][0:65, 32:40], in1=Tsamp[:, :], op=ALU.add),
                                 reads=[PBK[0], 'TS'], writes=['TS'])
                        T.op('dve', lambda e: e.tensor_tensor(out=Tsamp[:, :], in0=pb[0][0:65, 40:48], in1=Tsamp[:, :], op=ALU.add),
                             reads=[PBK[0], 'TS'], writes=['TS'])

                    if hg == 0:
                        _stop(1.8)
                    for hh in range(2):
                        P0 = 64 * hh
                        for bk in range(3):
                            if bk < 2:
                                src = TH[hh][bk]
                                skey = THK[hh][bk]
                                c_a, c_b, N = 0, 512 * bk, 512
                            else:
                                src = Tsamp
                                skey = 'TS'
                                c_a, c_b, N = 4 * hh, OWN, 4
                            T.op('dve', lambda e, src=src, c_a=c_a, N=N: e.reciprocal(out=rden[64:65, 0:N], in_=src[64:65, c_a:c_a + N]),
                                 reads=[skey], writes=['rden'])
                            T.op('pe', lambda e, N=N: e.matmul(pb[0][0:64, 0:N], lhsT=ones_f[64:65, 0:64], rhs=rden[64:65, 0:N],
                                                               start=True, stop=True), reads=['rden', 'ones_f'], writes=[PBK[0]])
                            T.op('act', lambda e, N=N: e.copy(out=bcs[:, 0:N], in_=pb[0][0:64, 0:N]), reads=[PBK[0]], writes=['bcs'])
                            T.op('dve', lambda e, src=src, c_a=c_a, c_b=c_b, N=N, P0=P0: e.tensor_tensor(
                                out=mixT[P0:P0 + 64, hg, c_b:c_b + N], in0=src[0:64, c_a:c_a + N], in1=bcs[:, 0:N], op=ALU.mult),
                                reads=[skey, 'bcs'], writes=[('mixT', hg, hh, bk)])

                T.barrier()
                pat.close()
                _stop(3)
                with ExitStack() as pp_:
                    uA = sb(pp_, "uA", [128, 2, 1040], F32)
                    uB = sb(pp_, "uB", [128, 1040], F32)
                    uC = sb(pp_, "uC", [128, 1040], F32)
                    usT = sb(pp_, "usT", [128, 2, 4], F32)
                    diffT = sb(pp_, "diffT", [128, 2, NTOK], BF16)
                    wpl = sb(pp_, "wpl", [128, 2, 256], BF16)
                    psc = sb(pp_, "psc", [128, 8], F32)
                    invc = sb(pp_, "invc", [128, 4, 16], F32)
                    sel = sb(pp_, "sel", [60, 4, 4], F32)
                    stt = sb(pp_, "stt", [60, 1024], F32)
                    d16 = sb(pp_, "d16", [128, 16], F32)
                    ssum = sb(pp_, "ssum", [128, 4], F32)
                    ost = sb(pp_, "ost", [16, 128], F32)
                    ost2 = sb(pp_, "ost2", [4, 128], F32)
                    T.dma('sp', "k0", invc[:], invc_d[:, :, :], writes=['invc'])
                    T.dma('sp', "k1", sel[:], sel_d[:, :, :], writes=['sel'])
                    T.dma('sp', "k2", stt[:], spool.rearrange("b j c -> (b j) c"), writes=['stt'])
                    for j in range(8):
                        T.dma('sp', "k3", psc[:, j:j + 1], pool_scale[128 * j:128 * j + 128, :], writes=['psc'])
                    for gi in range(4):
                        win = POOL_WINDOWS[gi]
                        T.dma('pool', "wpl", wpl[:], w_pool[gi].rearrange("(k q) n -> q k n", q=128), writes=['wpl'])
                        for ct in range(2):
                            jt = 2 * gi + ct
                            s_u = load_slab(9216 + 128 * jt)
                            for (xc, N, dst, dkey) in [(2032, 16, uA[:, ct, 0:16], ('uA', ct, 0)),
                                                       (2048, 512, uA[:, ct, 16:528], ('uA', ct, 1)),
                                                       (2560, 512, uA[:, ct, 528:1040], ('uA', ct, 2)),
                                                       (-1, 4, usT[:, ct, :], ('usT', ct))]:
                                if xc >= 0:
                                    bi = proj_fm(s_u, lambda k, xc=xc, N=N: xT[:, k, xc:xc + N], N)
                                else:
                                    bi = proj_fm(s_u, lambda k: xsT[:, k, :], 4)
                                T.op('act', lambda e, bi=bi, N=N, dst=dst: e.copy(out=dst, in_=pb[bi][:, 0:N]),
                                     reads=[PBK[bi]], writes=[dkey])
                            ukeys = [('uA', ct, i) for i in range(3)]
                            T.op('pe', lambda e, ct=ct: e.transpose(out=pb[7][0:16, 0:128], in_=uA[:, ct, 1024:1040], identity=ident[:]),
                                 reads=ukeys + ['ident'], writes=[PBK[7]])
                            T.op('dve', lambda e: e.tensor_copy(out=ost[:, :], in_=pb[7][0:16, 0:128]), reads=[PBK[7]], writes=['ost'])
                            T.dma('sp', "ost", poolp[:, 128 * jt:128 * jt + 128], ost[:, :], reads=['ost'])
                            T.op('pe', lambda e, ct=ct: e.transpose(out=pb[7][0:4, 128:256], in_=usT[:, ct, :], identity=ident[:]),
                                 reads=[('usT', ct), 'ident'], writes=[PBK[7]])
                            T.op('dve', lambda e: e.tensor_copy(out=ost2[:, :], in_=pb[7][0:4, 128:256]), reads=[PBK[7]], writes=['ost2'])
                            T.dma('sp', "ost2", pools[:, 14, 128 * jt:128 * jt + 128], ost2[:, :], reads=['ost2'])
                            cur, ckey = uA[:, ct, :], ukeys
                            bufs2 = [(uB, 'uB'), (uC, 'uC')]
                            step, n = 1, 0
                            while step < win:
                                dst, dk = bufs2[n % 2]
                                T.op('dve', lambda e, cur=cur, dst=dst, step=step: e.tensor_tensor(
                                    out=dst[:, step:1040], in0=cur[:, step:1040], in1=cur[:, 0:1040 - step], op=ALU.add),
                                    reads=(ckey if isinstance(ckey, list) else [ckey]), writes=[dk])
                                cur, ckey = dst[:, :], dk
                                step *= 2
                                n += 1
                            T.op('dve', lambda e, cur=cur, ct=ct: e.scalar_tensor_tensor(
                                out=diffT[:, ct, 0:OWN], in0=cur[:, 16:1040], scalar=1.0 / win, in1=uA[:, ct, 16:1040],
                                op0=ALU.mult, op1=ALU.subtract), reads=[ckey] + ukeys, writes=[('diffT', ct)])
                            T.op('dve', lambda e, cur=cur: e.tensor_tensor(out=d16[:, :], in0=cur[:, 16:32], in1=invc[:, gi, :],
                                                                           op=ALU.mult), reads=[ckey, 'invc'], writes=['d16'])
                            T.op('dve', lambda e, ct=ct: e.tensor_tensor(out=diffT[:, ct, 0:16], in0=d16[:, :], in1=uA[:, ct, 16:32],
                                                                         op=ALU.subtract), reads=['d16'] + ukeys, writes=[('diffT', ct)])
                            T.op('pe', lambda e, jt=jt: e.matmul(pb[7][:, 256:260], lhsT=stt[:, 128 * jt:128 * jt + 128], rhs=sel[:, gi, :],
                                                                 start=True, stop=True), reads=['stt', 'sel'], writes=[PBK[7]])
                            T.op('dve', lambda e, ct=ct: e.tensor_tensor(out=ssum[:, :], in0=pb[7][:, 256:260], in1=usT[:, ct, :], op=ALU.add),
                                 reads=[PBK[7], ('usT', ct)], writes=['ssum'])
                            T.op('dve', lambda e, ct=ct: e.scalar_tensor_tensor(
                                out=diffT[:, ct, OWN:OWN + 4], in0=ssum[:, :], scalar=1.0 / win, in1=usT[:, ct, :],
                                op0=ALU.mult, op1=ALU.subtract), reads=['ssum', ('usT', ct)], writes=[('diffT', ct)])
                        for dt_ in range(2):
                            for (c0, N) in CHUNKS:
                                bi = mmbank()

                                def f(e, bi=bi, dt_=dt_, c0=c0, N=N):
                                    for cc in range(2):
                                        ins = e.matmul(pb[bi][:, 0:N], lhsT=wpl[:, cc, 128 * dt_:128 * dt_ + 128],
                                                       rhs=diffT[:, cc, c0:c0 + N], start=(cc == 0), stop=(cc == 1))
                                    return ins
                                T.op('pe', f, reads=['wpl', ('diffT', 0), ('diffT', 1)], writes=[PBK[bi]])
                                jd = 2 * gi + dt_
                                T.op('dve', lambda e, bi=bi, jd=jd, c0=c0, N=N: e.tensor_scalar(
                                    out=mixT[:, 8 + jd, c0:c0 + N], in0=pb[bi][:, 0:N], scalar1=psc[:, jd:jd + 1], scalar2=None,
                                    op0=ALU.mult), reads=[PBK[bi], 'psc'], writes=[('mixTp', jd, c0)])
                    T.barrier()
            T.barrier()

            _stop(4)
            stream = sb(es, "stream", [128, 9, 2048], F32)
            xnT = mixT
            comb = sb(es, "comb", [128, 9, 32], F32)
            st1 = sb(es, "st1", [128, 8], F32)
            LB = {}

            def alloc_ln(scope, tag):
                LB['gam'] = sb(scope, "gam" + tag, [128, 2048], F32)
                LB['bet'] = sb(scope, "bet" + tag, [128, 2048], F32)
                LB['sq'] = sb(scope, "sq" + tag, [128, 2048], F32)
                LB['xnb'] = sb(scope, "xnb" + tag, [128, 2048], BF16)

            def layer_norm(tt, nt, eps):
                v = stream[0:nt, tt, :]
                k = ('stream', tt)
                gam, bet, sq = LB['gam'], LB['bet'], LB['sq']
                T.op('dve', lambda e: e.tensor_reduce(out=st1[0:nt, 0:1], in_=v, axis=AX.X, op=ALU.add), reads=[k], writes=['st_a'])
                T.op('pool', lambda e: e.tensor_tensor(out=sq[0:nt, :], in0=v, in1=v, op=ALU.mult), reads=[k], writes=['sq'])
                T.op('dve', lambda e: e.tensor_reduce(out=st1[0:nt, 1:2], in_=sq[0:nt, :], axis=AX.X, op=ALU.add),
                     reads=['sq'], writes=['st_b'])
                T.op('dve', lambda e: e.tensor_scalar(out=st1[0:nt, 2:4], in0=st1[0:nt, 0:2], scalar1=1.0 / 2048, scalar2=None,
                                                      op0=ALU.mult), reads=['st_a', 'st_b'], writes=['st_c'])
                T.op('dve', lambda e: e.tensor_tensor(out=st1[0:nt, 4:5], in0=st1[0:nt, 2:3], in1=st1[0:nt, 2:3], op=ALU.mult),
                     reads=['st_c'], writes=['st_d'])
                T.op('dve', lambda e: e.tensor_tensor(out=st1[0:nt, 5:6], in0=st1[0:nt, 3:4], in1=st1[0:nt, 4:5], op=ALU.subtract),
                     reads=['st_c', 'st_d'], writes=['st_e'])
                T.op('dve', lambda e: e.tensor_scalar(out=st1[0:nt, 7:8], in0=st1[0:nt, 5:6], scalar1=eps, scalar2=None,
                                                      op0=ALU.add), reads=['st_e'], writes=['st_g'])
                T.op('act', lambda e: e.activation(out=st1[0:nt, 7:8], in_=st1[0:nt, 7:8], func=AF.Sqrt), reads=['st_g'], writes=['st_g'])
                T.op('dve', lambda e: e.reciprocal(out=st1[0:nt, 6:7], in_=st1[0:nt, 7:8]), reads=['st_g'], writes=['st_f'])
                T.op('dve', lambda e: e.tensor_scalar(out=v, in0=v, scalar1=st1[0:nt, 2:3], scalar2=st1[0:nt, 6:7],
                                                      op0=ALU.subtract, op1=ALU.mult), reads=[k, 'st_c', 'st_f'], writes=[k])
                T.op('pool', lambda e: e.tensor_tensor(out=v, in0=v, in1=gam[0:nt, :], op=ALU.mult), reads=[k, 'gam'], writes=[k])
                T.op('pool', lambda e: e.tensor_tensor(out=v, in0=v, in1=bet[0:nt, :], op=ALU.add), reads=[k, 'bet'], writes=[k])

            def to_T(tt, t0, nt):
                k = ('stream', tt)
                xnb = LB['xnb']
                T.op('act', lambda e: e.copy(out=xnb[0:nt, :], in_=stream[0:nt, tt, :]), reads=[k], writes=['xnb'])
                for g in range(2):
                    bi = mmbank()
                    bank = pb[bi][:].bitcast(BF16)

                    def f(e, g=g, bank=bank):
                        for j in range(8):
                            kk = 8 * g + j
                            ins = e.transpose(out=bank[:, nt * j:nt * j + nt], in_=xnb[0:nt, 128 * kk:128 * kk + 128],
                                              identity=identb[0:nt, 0:nt])
                        return ins
                    T.op('pe', f, reads=['xnb', 'identb'], writes=[PBK[bi]])
                    T.op('dve', lambda e, g=g, bank=bank: e.tensor_copy(out=xnT[:, 8 * g:8 * g + 8, t0:t0 + nt],
                                                                        in_=bank[:, 0:8 * nt].rearrange("q (j c) -> q j c", c=nt)),
                         reads=[PBK[bi]], writes=[('xnT', tt, g)])

            with ExitStack() as pB:
                wo = [sb(pB, "wo%d" % i, [128, 16, 512], BF16) for i in range(2)]
                xres = [sb(pB, "xres%d" % i, [128, 512], F32) for i in range(3)]
                alloc_ln(pB, "B")
                gam, bet = LB['gam'], LB['bet']
                T.dma('sp', "k0", gam[:], ln_gb[0].partition_broadcast(128), writes=['gam'])
                T.dma('sp', "k1", bet[:], ln_gb[1].partition_broadcast(128), writes=['bet'])
                n_x = 0
                for cg in range(4):
                    s = cg % 2
                    T.dma('pool', "wo%d" % s, wo[s][:], w_out[:, 512 * cg:512 * cg + 512].rearrange("(k q) n -> q k n", q=128),
                          writes=[('wo', s)])
                    for tt, (t0, nt) in enumerate(TT):
                        xs_ = n_x % 3
                        n_x += 1
                        src = xh[2048 + t0:2048 + t0 + nt, 512 * cg:512 * cg + 512] if tt < 8 else xs[:, 512 * cg:512 * cg + 512]
                        T.dma('sp', "xres%d" % xs_, xres[xs_][0:nt, :], src, writes=[('xres', xs_)])
                        bi = mmbank()

                        def f(e, bi=bi, s=s, t0=t0, nt=nt):
                            for k in range(16):
                                ins = e.matmul(pb[bi][0:nt, :], lhsT=mixT[:, k, t0:t0 + nt], rhs=wo[s][:, k, :],
                                               start=(k == 0), stop=(k == 15))
                            return ins
                        T.op('pe', f, reads=[('wo', s)], writes=[PBK[bi]])
                        T.op('dve', lambda e, bi=bi, xs_=xs_, tt=tt, nt=nt, cg=cg: e.scalar_tensor_tensor(
                            out=stream[0:nt, tt, 512 * cg:512 * cg + 512], in0=xres[xs_][0:nt, :], scalar=ALPHA, in1=pb[bi][0:nt, :],
                            op0=ALU.mult, op1=ALU.add), reads=[PBK[bi], ('xres', xs_)], writes=[('stream', tt)])
                        if cg == 3:
                            layer_norm(tt, nt, LN_EPS)
                            to_T(tt, t0, nt)
            T.barrier()

            _stop(5)
            with ExitStack() as pC:
                wr = sb(pC, "wr", [128, 16, 36], BF16)
                brow = sb(pC, "brow", [1, 36], BF16)
                lg = sb(pC, "lg", [128, 36], F32)
                rt = sb(pC, "rt", [128, 160], F32)
                T.dma('pool', "k0", wr[:, :, 0:4], w_gr.rearrange("(k q) n -> q k n", q=128), writes=['wr'])
                T.dma('pool', "k1", wr[:, :, 4:36], w_er.rearrange("(k q) n -> q k n", q=128), writes=['wr'])
                T.dma('pool', "k2", brow[:], b_r[:, :], writes=['brow'])
                BIG = 1.0e9
                for tt, (t0, nt) in enumerate(TT):
                    bi = mmbank()

                    def f(e, bi=bi, t0=t0, nt=nt):
                        for k in range(16):
                            e.matmul(pb[bi][0:nt, 0:36], lhsT=xnT[:, k, t0:t0 + nt], rhs=wr[:, k, :], start=(k == 0), stop=False)
                        return e.matmul(pb[bi][0:nt, 0:36], lhsT=ones_b[0:1, 0:nt], rhs=brow[0:1, :], start=False, stop=True)
                    T.op('pe', f, reads=['wr', 'brow', 'ones_b'], writes=[PBK[bi]])
                    R = lambda a, b_: rt[0:nt, a:b_]
                    ops = []
                    V = lambda fn: ops.append(fn)
                    V(lambda e: e.tensor_copy(out=lg[0:nt, :], in_=pb[bi][0:nt, 0:36]))
                    V(lambda e: e.tensor_reduce(out=R(0, 1), in_=lg[0:nt, 0:4], axis=AX.X, op=ALU.max))
                    V(lambda e: e.tensor_scalar(out=R(4, 8), in0=lg[0:nt, 0:4], scalar1=R(0, 1), scalar2=None, op0=ALU.is_equal))
                    V(lambda e: e.tensor_scalar(out=R(8, 12), in0=lg[0:nt, 0:4], scalar1=R(0, 1), scalar2=None, op0=ALU.subtract))
                    V(('act', lambda e: e.activation(out=R(8, 12), in_=R(8, 12), func=AF.Exp)))
                    V(lambda e: e.tensor_reduce(out=R(1, 2), in_=R(8, 12), axis=AX.X, op=ALU.add))
                    V(lambda e: e.reciprocal(out=R(2, 3), in_=R(1, 2)))
                    V(lambda e: e.tensor_scalar(out=R(12, 16), in0=R(4, 8), scalar1=-1.0, scalar2=BIG, op0=ALU.add, op1=ALU.mult))
                    V(lambda e: e.tensor_tensor(out=R(16, 48).rearrange("q (g c) -> q g c", c=8),
                                                in0=lg[0:nt, 4:36].rearrange("q (g c) -> q g c", c=8),
                                                in1=R(12, 16).unsqueeze(2).to_broadcast([nt, 4, 8]), op=ALU.add))
                    V(lambda e: e.tensor_reduce(out=R(3, 4), in_=R(16, 48), axis=AX.X, op=ALU.max))
                    V(lambda e: e.tensor_scalar(out=R(48, 80), in0=R(16, 48), scalar1=R(3, 4), scalar2=None, op0=ALU.is_equal))
                    V(lambda e: e.scalar_tensor_tensor(out=R(80, 112), in0=R(48, 80), scalar=-BIG, in1=R(16, 48),
                                                       op0=ALU.mult, op1=ALU.add))
                    V(lambda e: e.tensor_reduce(out=R(112, 113), in_=R(80, 112), axis=AX.X, op=ALU.max))
                    V(lambda e: e.tensor_scalar(out=R(116, 148), in0=R(80, 112), scalar1=R(112, 113), scalar2=None, op0=ALU.is_equal))
                    V(lambda e: e.tensor_tensor(out=R(113, 114), in0=R(112, 113), in1=R(3, 4), op=ALU.subtract))
                    V(('act', lambda e: e.activation(out=R(113, 114), in_=R(113, 114), func=AF.Exp)))
                    V(lambda e: e.tensor_scalar(out=R(114, 115), in0=R(113, 114), scalar1=1.0, scalar2=None, op0=ALU.add))
                    V(lambda e: e.reciprocal(out=R(115, 116), in_=R(114, 115)))
                    V(lambda e: e.tensor_scalar(out=R(148, 149), in0=R(2, 3), scalar1=1.0 / ALPHA, scalar2=None, op0=ALU.mult))
                    V(lambda e: e.tensor_tensor(out=R(149, 150), in0=R(148, 149), in1=R(115, 116), op=ALU.mult))
                    V(lambda e: e.tensor_tensor(out=R(150, 151), in0=R(148, 149), in1=R(149, 150), op=ALU.subtract))
                    V(lambda e: e.tensor_scalar(out=R(48, 80), in0=R(48, 80), scalar1=R(149, 150), scalar2=None, op0=ALU.mult))
                    V(lambda e: e.scalar_tensor_tensor(out=comb[0:nt, tt, :], in0=R(116, 148), scalar=R(150, 151), in1=R(48, 80),
                                                       op0=ALU.mult, op1=ALU.add))
                    first = True
                    for o in ops:
                        eng, fn = (o if isinstance(o, tuple) else ('dve', o))
                        T.op(eng, fn, reads=['rt'] + ([PBK[bi]] if first else []), writes=['rt'])
                        first = False
                T.barrier()

                wg = [sb(pC, "wg%d" % i, [128, 16, 256], BF16) for i in range(2)]
                wu = [sb(pC, "wu%d" % i, [128, 16, 256], BF16) for i in range(2)]
                wd = [sb(pC, "wd%d" % i, [128, 2, 2048], BF16) for i in range(2)]
                hT = [sb(pC, "hT%d" % i, [128, 2, NTOK], BF16) for i in range(2)]
                sg = [sb(pC, "sg%d" % i, [128, 512], BF16) for i in range(2)]
                n_sg = 0
                n_gu = 0
                n_dn = 0
                for ex in range(32):
                    s = ex % 2
                    T.dma('pool', "wg%d" % s, wg[s][:], w_gate[ex].rearrange("(k q) n -> q k n", q=128), writes=[('wg', s)])
                    T.dma('pool', "wu%d" % s, wu[s][:], w_up[ex].rearrange("(k q) n -> q k n", q=128), writes=[('wu', s)])
                    T.dma('pool', "wd%d" % s, wd[s][:], w_down[ex].rearrange("(k q) n -> q k n", q=128), writes=[('wd', s)])
                    for (c0, N) in CHUNKS:
                        for fi in range(2):
                            bg = 2 * (n_gu % 2)
                            bu = bg + 1
                            n_gu += 1

                            def f(e, bg=bg, bu=bu, fi=fi, c0=c0, N=N, s=s):
                                for k in range(16):
                                    e.matmul(pb[bg][:, 0:N], lhsT=wg[s][:, k, 128 * fi:128 * fi + 128], rhs=xnT[:, k, c0:c0 + N],
                                             start=(k == 0), stop=(k == 15))
                                for k in range(16):
                                    ins = e.matmul(pb[bu][:, 0:N], lhsT=wu[s][:, k, 128 * fi:128 * fi + 128], rhs=xnT[:, k, c0:c0 + N],
                                                   start=(k == 0), stop=(k == 15))
                                return ins
                            T.op('pe', f, reads=[('wg', s), ('wu', s)], writes=[PBK[bg], PBK[bu]])
                            q = n_sg % 2
                            n_sg += 1
                            T.op('act', lambda e, q=q, bg=bg, N=N: e.activation(out=sg[q][:, 0:N], in_=pb[bg][:, 0:N], func=AF.Silu),
                                 reads=[PBK[bg]], writes=[('sg', q)])
                            T.op('dve', lambda e, q=q, bu=bu, N=N, fi=fi, c0=c0, s=s: e.tensor_tensor(
                                out=hT[s][:, fi, c0:c0 + N], in0=pb[bu][:, 0:N], in1=sg[q][:, 0:N], op=ALU.mult),
                                reads=[PBK[bu], ('sg', q)], writes=[('hT', s, fi, c0)])
                    hkeys = [('hT', s, fi, c0) for fi in range(2) for (c0, _) in CHUNKS]
                    for tt, (t0, nt) in enumerate(TT):
                        for cg in range(4):
                            bi = 4 + (n_dn % 4)
                            n_dn += 1

                            def f(e, bi=bi, t0=t0, nt=nt, cg=cg, s=s):
                                for fi in range(2):
                                    ins = e.matmul(pb[bi][0:nt, :], lhsT=hT[s][:, fi, t0:t0 + nt], rhs=wd[s][:, fi, 512 * cg:512 * cg + 512],
                                                   start=(fi == 0), stop=(fi == 1))
                                return ins
                            T.op('pe', f, reads=hkeys + [('wd', s)], writes=[PBK[bi]])
                            eng = 'dve' if (cg % 2 == 0) else 'pool'
                            if eng == 'dve':
                                T.op('dve', lambda e, bi=bi, tt=tt, nt=nt, cg=cg, ex=ex: e.scalar_tensor_tensor(
                                    out=stream[0:nt, tt, 512 * cg:512 * cg + 512], in0=pb[bi][0:nt, :], scalar=comb[0:nt, tt, ex:ex + 1],
                                    in1=stream[0:nt, tt, 512 * cg:512 * cg + 512], op0=ALU.mult, op1=ALU.add),
                                    reads=[PBK[bi], ('stream', tt, cg)], writes=[('stream', tt, cg)])
                            else:
                                T.op('dve', lambda e, bi=bi, tt=tt, nt=nt, cg=cg, ex=ex: e.scalar_tensor_tensor(
                                    out=stream[0:nt, tt, 512 * cg:512 * cg + 512], in0=pb[bi][0:nt, :], scalar=comb[0:nt, tt, ex:ex + 1],
                                    in1=stream[0:nt, tt, 512 * cg:512 * cg + 512], op0=ALU.mult, op1=ALU.add),
                                    reads=[PBK[bi], ('stream', tt, cg)], writes=[('stream', tt, cg)])
            T.barrier()

            _stop(6)
            with ExitStack() as pD:
                wpg = [sb(pD, "wpg%d" % i, [128, 16, 512], BF16) for i in range(2)]
                wple = sb(pD, "wple", [128, 2, 2048], BF16)
                pTt = sb(pD, "pT", [128, 2, NTOK], BF16)
                pbf = sb(pD, "pbf", [128, 256], BF16)
                sgm = [sb(pD, "sgm%d" % i, [128, 512], F32) for i in range(2)]
                osb = [sb(pD, "osb%d" % i, [128, 512], F32) for i in range(3)]
                alloc_ln(pD, "D")
                gam, bet = LB['gam'], LB['bet']
                T.dma('sp', "k0", gam[:], ln_gb[2].partition_broadcast(128), writes=['gam'])
                T.dma('sp', "k1", bet[:], ln_gb[3].partition_broadcast(128), writes=['bet'])
                T.dma('pool', "k2", wple[:], w_ple.rearrange("(k q) n -> q k n", q=128), writes=['wple'])
                def prep_tile(tt, t0, nt):
                    layer_norm(tt, nt, LN_EPS / (ALPHA * ALPHA))
                    to_T(tt, t0, nt)
                    src = pp[t0:t0 + nt, :] if tt < 8 else pps[:, :]
                    T.dma('pool', "pbf", pbf[0:nt, :], src, writes=['pbf'])
                    bi = mmbank()
                    bank = pb[bi][:].bitcast(BF16)

                    def f(e, bank=bank, nt=nt):
                        for j in range(2):
                            ins = e.transpose(out=bank[:, nt * j:nt * j + nt], in_=pbf[0:nt, 128 * j:128 * j + 128],
                                              identity=identb[0:nt, 0:nt])
                        return ins
                    T.op('pe', f, reads=['pbf', 'identb'], writes=[PBK[bi]])
                    T.op('dve', lambda e, bank=bank, t0=t0, nt=nt: e.tensor_copy(
                        out=pTt[:, :, t0:t0 + nt], in_=bank[:, 0:2 * nt].rearrange("q (j c) -> q j c", c=nt)),
                        reads=[PBK[bi]], writes=[('pT', tt)])

                n_o = 0
                for cg in range(4):
                    s = cg % 2
                    T.dma('pool', "wpg%d" % s, wpg[s][:], w_pg[:, 512 * cg:512 * cg + 512].rearrange("(k q) n -> q k n", q=128),
                          writes=[('wpg', s)])
                    for tt, (t0, nt) in enumerate(TT):
                        if cg == 0:
                            prep_tile(tt, t0, nt)
                        bg = 2 + 2 * (n_o % 2)
                        bp = bg + 1

                        def f(e, bg=bg, bp=bp, s=s, t0=t0, nt=nt, cg=cg):
                            for k in range(16):
                                e.matmul(pb[bg][0:nt, :], lhsT=xnT[:, k, t0:t0 + nt], rhs=wpg[s][:, k, :], start=(k == 0), stop=(k == 15))
                            for j in range(2):
                                ins = e.matmul(pb[bp][0:nt, :], lhsT=pTt[:, j, t0:t0 + nt], rhs=wple[:, j, 512 * cg:512 * cg + 512],
                                               start=(j == 0), stop=(j == 1))
                            return ins
                        T.op('pe', f, reads=[('wpg', s), 'wple', ('pT', tt), ('xnT', tt, 0), ('xnT', tt, 1)], writes=[PBK[bg], PBK[bp]])
                        q = n_o % 2
                        o = n_o % 3
                        n_o += 1
                        T.op('act', lambda e, q=q, bg=bg, nt=nt: e.activation(out=sgm[q][0:nt, :], in_=pb[bg][0:nt, :], func=AF.Sigmoid),
                             reads=[PBK[bg]], writes=[('sgm', q)])
                        T.op('dve', lambda e, q=q, bp=bp, nt=nt: e.tensor_tensor(out=sgm[q][0:nt, :], in0=pb[bp][0:nt, :], in1=sgm[q][0:nt, :],
                                                                                op=ALU.mult), reads=[PBK[bp], ('sgm', q)], writes=[('sgm', q)])
                        T.op('pool', lambda e, q=q, o=o, tt=tt, nt=nt, cg=cg: e.tensor_tensor(
                            out=osb[o][0:nt, :], in0=sgm[q][0:nt, :], in1=stream[0:nt, tt, 512 * cg:512 * cg + 512], op=ALU.add),
                            reads=[('sgm', q), ('stream', tt)], writes=[('osb', o)])
                        T.dma('sp', "osb%d" % o, y[t0:t0 + nt, 512 * cg:512 * cg + 512], osb[o][0:nt, :], reads=[('osb', o)])
        except _Stop:
            pass
        DEAD[0] = False
        T.barrier(final=True)
    return nc


def _consts(core):
    own0 = OWN * core
    inv_freq = (np.float32(500000.0) ** (-(np.arange(0, 16, 2, dtype=np.float32)) / np.float32(16))).astype(np.float32)
    pos = np.concatenate([np.arange(own0 - 2048, own0 + OWN), np.full(4, 8192)]).astype(np.float32)
    ang = (pos[None, :] * inv_freq[:, None]).astype(np.float32).astype(np.float64)
    C = np.ones((128, 3076), np.float32)
    S = np.zeros((128, 3076), np.float32)
    R = np.zeros((128, 128), np.float32)
    for hh in range(2):
        b = 64 * hh
        C[b:b + 8] = np.cos(ang)
        C[b + 8:b + 16] = np.cos(ang)
        S[b:b + 8] = -np.sin(ang)
        S[b + 8:b + 16] = np.sin(ang)
        for i in range(8):
            R[b + 8 + i, b + i] = 1.0
            R[b + i, b + 8 + i] = 1.0
    kk = np.arange(128)[:, None]
    qq = np.arange(128)[None, :]
    gen = np.concatenate([(kk >= qq), (kk <= qq)], axis=1).astype(np.float32)
    mk = np.zeros((128, 4, 256), np.float32)
    mk[:, 0] = gen
    first = gen.copy()
    if core == 0:
        first[:, 0:128] = 0.0
    mk[:, 1] = first
    mk[:, 2] = first
    q64 = np.arange(64)[None, :]
    m_glob = 64 * core - 128 + kk
    b0 = ((kk >= q64) & (m_glob >= 0)).astype(np.float32)
    b1 = (kk <= q64).astype(np.float32)
    mk[:, 3, 0:64] = b0
    mk[:, 3, 64:128] = b1
    invc = np.zeros((128, 4, 16), np.float32)
    for gi, win in enumerate(POOL_WINDOWS):
        p_ = own0 + np.arange(16)
        invc[:, gi, :] = (1.0 / np.minimum(p_ + 1, win)).astype(np.float32)[None, :]
    sel = np.zeros((60, 4, 4), np.float32)
    for b in range(4):
        for gi, win in enumerate(POOL_WINDOWS):
            for j in range(15):
                if j >= 15 - (win - 1):
                    sel[b * 15 + j, gi, b] = 1.0
    return dict(ropeC=C, ropeS=S, rperm=R, mk=mk, invc=invc, sel=sel)


_PROG = None
DEBUG_CORES = [None]


def kernel(**inp):
    global _PROG
    if _PROG is None:
        _PROG = build_program()
    nc = _PROG
    f = lambda a: np.ascontiguousarray(a, dtype=np.float32)
    x = inp["x_prompt"][0]
    xpad = np.concatenate([np.zeros((2048, 2048), np.float32), x], axis=0)
    shared = dict(
        w_in=f(inp["w_in"][0]), w_out=f(inp["w_out"][0]), w_pool=f(inp["w_pool"][0]),
        pool_scale=f(inp["pool_scale"][0].reshape(1024, 1)),
        ln1_g=f(inp["ln1_g"]), ln1_b=f(inp["ln1_b"]), ln2_g=f(inp["ln2_g"]), ln2_b=f(inp["ln2_b"]),
        w_gr=f(inp["w_group_router"][0]), w_er=f(inp["w_expert_router"][0]),
        b_r=f(np.concatenate([inp["b_group_router"][0], inp["b_expert_router"][0]])[None, :]),
        w_gate=f(inp["w_gate"][0]), w_up=f(inp["w_up"][0]), w_down=f(inp["w_down"][0]),
        w_ple=f(inp["w_ple"][0]), w_pg=f(inp["w_ple_gate"][0]))
    caches = [inp["cache_kv_w128_d1"][0], inp["cache_kv_w512_d4"][0], inp["cache_kv_w2048_d16"][0]]
    in_maps = []
    for c in range(NCORE):
        m = dict(shared)
        m["xh"] = f(xpad[OWN * c:OWN * c + 3072])
        m["xs"] = f(inp["x_sample"][4 * c:4 * c + 4, 0])
        for p in range(3):
            m["c%d" % p] = f(caches[p][4 * c:4 * c + 4].reshape(4, CL[p], 2, 1024))
        m["spool"] = f(inp["state_pool"][0, 4 * c:4 * c + 4])
        m["pp"] = f(inp["p_prompt"][0, 0, OWN * c:OWN * c + OWN])
        m["pps"] = f(inp["p_sample"][0, 4 * c:4 * c + 4, 0])
        m.update(_consts(c))
        in_maps.append(m)
    if DEBUG_CORES[0] is not None:
        sub = [in_maps[c] for c in DEBUG_CORES[0]]
        return run_bass_kernel_spmd(nc, sub, core_ids=list(range(len(sub)))).results
    res = run_bass_kernel_spmd(nc, in_maps, core_ids=list(range(NCORE))).results
    y_p = np.concatenate([r["y"][0:OWN] for r in res], 0)[None]
    y_s = np.concatenate([r["y"][OWN:OWN + 4] for r in res], 0)[:, None, :]
    kv0 = res[7]["kvp0"].reshape(1, 1, 128, 2, 16, 64)
    kv1 = res[7]["kvp1"].reshape(1, 1, 512, 2, 16, 64)
    kv2 = np.concatenate([res[6]["kvp2"], res[7]["kvp2"]], 0).reshape(1, 1, 2048, 2, 16, 64)
    pl = res[7]["poolp"][1:16].reshape(1, 1, 15, 1024)
    ks = [np.concatenate([r["kvs%d" % p] for r in res], 0).reshape(1, 32, CL[p], 2, 16, 64) for p in range(3)]
    pls = np.concatenate([r["pools"] for r in res], 0).reshape(1, 32, 15, 1024)
    outs = (y_p, y_s, kv0, kv1, kv2, pl, ks[0], ks[1], ks[2], pls)
    return tuple(np.ascontiguousarray(o, dtype=np.float32) for o in outs)
```

```python
import numpy as np
from contextlib import ExitStack
import concourse.bass as bass
import concourse.mybir as mybir
from concourse.bass_utils import run_bass_kernel_spmd

F32 = mybir.dt.float32
BF16 = mybir.dt.bfloat16
AF = mybir.ActivationFunctionType
ALU = mybir.AluOpType
AX = mybir.AxisListType

NCORE = 8
OWN = 1024
HALO = [128, 512, 2048]
DIL = [1, 4, 16]
CL = [128, 512, 2048]
ALPHA = 2.0 ** 0.25
LN_EPS = 1e-5
POOL_WINDOWS = (2, 4, 8, 16)
NTOK = OWN + 4
TT = [(128 * i, 128) for i in range(8)] + [(1024, 4)]
CHUNKS = [(0, 512), (512, 512), (1024, 4)]
ENGS = ('pe', 'act', 'dve', 'pool', 'sp')


class _Stop(Exception):
    pass


KSTOP = [99]


DEAD = [False]
KOPS = [None]
NOATT = [False]
NOSAMP = [False]
NOPS = [0]


def _stop(n):
    if KOPS[0] == -1:
        print("stop marker", n, "ops", NOPS[0])
    if KSTOP[0] <= n:
        DEAD[0] = True


class Trk:
    def __init__(self, nc, es):
        self.nc = nc
        self.es = es
        self.eng = {'pe': nc.tensor, 'act': nc.scalar, 'dve': nc.vector, 'pool': nc.gpsimd, 'sp': nc.sync}
        self.sem = {e: es.enter_context(nc.semaphore("c_" + e)) for e in ('pe', 'act', 'dve', 'pool')}
        self.cnt = {e: 0 for e in self.sem}
        self.waited = {e: {} for e in ENGS}
        self.bufs = {}
        self.chan = {}

    def _buf(self, k):
        b = self.bufs.get(k)
        if b is None:
            b = self.bufs[k] = {'w': {}, 'r': {}}
        return b

    @staticmethod
    def _flat(keys):
        out = []
        for k in keys:
            if isinstance(k, list):
                out.extend(k)
            else:
                out.append(k)
        return out

    def _deps(self, eng, reads, writes):
        deps = {}

        def add(d, skip):
            for s, v in d.items():
                if skip and s == eng:
                    continue
                if deps.get(s, 0) < v:
                    deps[s] = v
        for k in reads:
            add(self._buf(k)['w'], False)
            if isinstance(k, tuple) and k[0] == 'pb':
                add(self._buf(k)['r'], True)
        for k in writes:
            b = self._buf(k)
            add(b['w'], True)
            add(b['r'], True)
        return deps

    def _waits(self, eng, deps):
        e = self.eng[eng]
        for s, v in deps.items():
            if v <= 0 or self.waited[eng].get(s, 0) >= v:
                continue
            semh = self.sem[s] if isinstance(s, str) else self.chan[s[1]][0]
            e.wait_ge(semh, v)
            self.waited[eng][s] = v

    def op(self, eng, fn, reads=(), writes=()):
        NOPS[0] += 1
        if KOPS[0] is not None and NOPS[0] > KOPS[0]:
            DEAD[0] = True
        if DEAD[0]:
            return None
        reads, writes = self._flat(reads), self._flat(writes)
        self._waits(eng, self._deps(eng, reads, writes))
        ins = fn(self.eng[eng])
        self.cnt[eng] += 1
        ins.then_inc(self.sem[eng], 1)
        v = self.cnt[eng]
        for k in reads:
            self._buf(k)['r'][eng] = v
        for k in writes:
            b = self._buf(k)
            b['w'] = {eng: v}
            b['r'] = {}
        return ins

    def dma(self, eng, chan, out, in_, reads=(), writes=()):
        NOPS[0] += 1
        if KOPS[0] is not None and NOPS[0] > KOPS[0]:
            DEAD[0] = True
        if DEAD[0]:
            return None
        reads, writes = self._flat(reads), self._flat(writes)
        if chan not in self.chan:
            self.chan[chan] = [self.es.enter_context(self.nc.semaphore("d_" + chan)), 0]
        self._waits(eng, self._deps(None, reads, writes))
        c = self.chan[chan]
        c[1] += 16
        self.eng[eng].dma_start(out=out, in_=in_).then_inc(c[0], 16)
        key = ('ch', chan)
        for k in reads:
            self._buf(k)['r'][key] = c[1]
        for k in writes:
            b = self._buf(k)
            b['w'] = {key: c[1]}
            b['r'] = {}

    def barrier(self, engines=ENGS, final=False):
        if DEAD[0]:
            return
        deps = {e: self.cnt[e] for e in self.sem}
        deps.update({('ch', n): c[1] for n, c in self.chan.items() if final or not n.startswith("cc")})
        for e in engines:
            self._waits(e, deps)
        self.bufs = {}


def build_program():
    nc = bass.Bass("TRN2", target_bir_lowering=False)
    DEAD[0] = False
    NOPS[0] = 0

    def din(name, shape):
        return nc.dram_tensor(name, list(shape), F32, kind="ExternalInput").ap()

    def dout(name, shape):
        return nc.dram_tensor(name, list(shape), F32, kind="ExternalOutput").ap()

    xh = din("xh", [3072, 2048])
    xs = din("xs", [4, 2048])
    c_in = [din("c%d" % p, [4, CL[p], 2, 1024]) for p in range(3)]
    spool = din("spool", [4, 15, 1024])
    pp = din("pp", [1024, 256])
    pps = din("pps", [4, 256])
    w_in = din("w_in", [2048, 10240])
    w_out = din("w_out", [2048, 2048])
    w_pool = din("w_pool", [4, 256, 256])
    pool_scale = din("pool_scale", [1024, 1])
    ln_gb = [din(n, [1, 2048]) for n in ("ln1_g", "ln1_b", "ln2_g", "ln2_b")]
    w_gr = din("w_gr", [2048, 4])
    w_er = din("w_er", [2048, 32])
    b_r = din("b_r", [1, 36])
    w_gate = din("w_gate", [32, 2048, 256])
    w_up = din("w_up", [32, 2048, 256])
    w_down = din("w_down", [32, 256, 2048])
    w_ple = din("w_ple", [256, 2048])
    w_pg = din("w_pg", [2048, 2048])
    ropeC_d = din("ropeC", [128, 3076])
    ropeS_d = din("ropeS", [128, 3076])
    rperm_d = din("rperm", [128, 128])
    mk_d = din("mk", [128, 4, 256])
    invc_d = din("invc", [128, 4, 16])
    sel_d = din("sel", [60, 4, 4])

    y = dout("y", [NTOK, 2048])
    kvp = [dout("kvp0", [128, 2, 1024]), dout("kvp1", [512, 2, 1024]), dout("kvp2", [1024, 2, 1024])]
    KVP_FROM = [OWN - 128, OWN - 512, 0]
    poolp = dout("poolp", [16, 1024])
    kvs = [dout("kvs%d" % p, [4, CL[p], 2, 1024]) for p in range(3)]
    pools = dout("pools", [4, 15, 1024])

    with ExitStack() as es:
        T = Trk(nc, es)
        try:

            def sb(scope, name, shape, dt):
                return scope.enter_context(nc.sbuf_tensor("s_" + name, list(shape), dt))

            pb = [es.enter_context(nc.psum_tensor("pb%d" % i, [128, 512], F32)) for i in range(8)]
            PBK = [('pb', i) for i in range(8)]

            ident = sb(es, "ident", [128, 128], F32)
            identb = sb(es, "identb", [128, 128], BF16)
            ones_f = sb(es, "ones_f", [128, 128], F32)
            ones_b = sb(es, "ones_b", [128, 128], BF16)
            T.op('pool', lambda e: e.memset(ident[:], 0.0), writes=['ident'])
            T.op('pool', lambda e: e.affine_select(out=ident[:], in_=ident[:], pattern=[[-1, 128]],
                                                   compare_op=ALU.not_equal, fill=1.0, base=0, channel_multiplier=1),
                 reads=['ident'], writes=['ident'])
            T.op('pool', lambda e: e.tensor_copy(out=identb[:], in_=ident[:]), reads=['ident'], writes=['identb'])
            T.op('pool', lambda e: e.memset(ones_f[:], 1.0), writes=['ones_f'])
            T.op('pool', lambda e: e.memset(ones_b[:], 1.0), writes=['ones_b'])

            for p in range(3):
                L = CL[p]
                for b in range(4):
                    r0 = 1
                    while r0 < L:
                        r1 = min(L, r0 + 512)
                        T.dma('sp', "cc%d" % ((b + p) % 4), kvs[p][b, r0 - 1:r1 - 1, :, :], c_in[p][b, r0:r1, :, :])
                        r0 = r1
            for b in range(4):
                T.dma('sp', "cc%d" % b, pools[b, 0:14, :], spool[b, 1:15, :])

            _stop(0)
            mixT = sb(es, "mixT", [128, 16, NTOK], BF16)

            with ExitStack() as pa:
                xT = sb(pa, "xT", [128, 16, 3072], BF16)
                xsT = sb(pa, "xsT", [128, 16, 4], BF16)
                ropeC = sb(pa, "ropeC_s", [128, 3076], BF16)
                ropeS = sb(pa, "ropeS_s", [128, 3076], BF16)
                rperm = sb(pa, "rperm_s", [128, 128], BF16)
                mk = sb(pa, "mk_s", [128, 4, 256], BF16)
                T.dma('pool', "k0", ropeC[:], ropeC_d[:, :], writes=['ropeC'])
                T.dma('pool', "k1", ropeS[:], ropeS_d[:, :], writes=['ropeS'])
                T.dma('pool', "k2", rperm[:], rperm_d[:, :], writes=['rperm'])
                T.dma('pool', "k3", mk[:], mk_d[:, :, :], writes=['mk'])

                with ExitStack() as px:
                    xb = [sb(px, "xb%d" % i, [128, 2048], BF16) for i in range(3)]
                    ptb = [pb[3 + g][:].bitcast(BF16) for g in range(4)]
                    for t in range(25):
                        s = t % 3
                        nt = 128 if t < 24 else 4
                        src = xh[128 * t:128 * t + 128, :] if t < 24 else xs[:, :]
                        T.dma('pool', "xb%d" % s, xb[s][0:nt, :], src, writes=[('xb', s)])
                        for g in range(2):
                            bi = 2 * (t % 2) + g
                            bank = ptb[bi]
                            w = nt

                            def f(e, g=g, s=s, bank=bank, nt=nt, w=w):
                                for j in range(8):
                                    k = 8 * g + j
                                    ins = e.transpose(out=bank[:, w * j:w * j + w], in_=xb[s][0:nt, 128 * k:128 * k + 128],
                                                      identity=identb[0:nt, 0:nt])
                                return ins
                            T.op('pe', f, reads=[('xb', s), 'identb'], writes=[PBK[3 + bi]])
                            dst = xT[:, 8 * g:8 * g + 8, 128 * t:128 * t + 128] if t < 24 else xsT[:, 8 * g:8 * g + 8, :]
                            srcv = bank[:, 0:8 * w].rearrange("p (j c) -> p j c", c=w)
                            if g == 0:
                                T.op('act', lambda e, dst=dst, srcv=srcv: e.copy(out=dst, in_=srcv),
                                     reads=[PBK[3 + bi]], writes=[('xT', t, g)])
                            else:
                                T.op('dve', lambda e, dst=dst, srcv=srcv: e.tensor_copy(out=dst, in_=srcv),
                                     reads=[PBK[3 + bi]], writes=[('xT', t, g)])
                    T.barrier()
                _stop(1)

                NSLAB = 4
                slab = [sb(pa, "slab%d" % i, [128, 16, 128], BF16) for i in range(NSLAB)]
                pat = ExitStack()
                kT = sb(pat, "kT", [128, 3072], BF16)
                ksT = sb(pat, "ksT", [128, 4], BF16)
                qT = sb(pat, "qT", [128, NTOK], BF16)
                V_ext = sb(pat, "V_ext", [128, 32, 2, 65], BF16)
                Vs_ext = sb(pat, "Vs_ext", [4, 2, 65], BF16)
                zb = [sb(pat, "zb%d" % i, [128, 512], BF16) for i in range(2)]
                t1s = [sb(pat, "t1_%d" % i, [128, 512], F32) for i in range(2)]
                t2s = [sb(pat, "t2_%d" % i, [128, 512], F32) for i in range(2)]
                kf = sb(pat, "kf", [128, 512], F32)
                ksf = sb(pat, "ksf", [128, 4], F32)
                Eb = [sb(pat, "E%d" % i, [128, 256], BF16) for i in range(4)]
                stg = [sb(pat, "stg%d" % i, [128, 512], F32) for i in range(2)]
                stv = [sb(pat, "stv%d" % i, [128, 128], F32) for i in range(4)]
                sts = [sb(pat, "sts%d" % i, [4, 128], F32) for i in range(2)]
                kg = [sb(pat, "kg%d" % i, [128, 128], BF16) for i in range(4)]
                kcT = [sb(pat, "kcT%d" % i, [128, 128], BF16) for i in range(4)]
                vg = [sb(pat, "vg%d" % i, [128, 2, 65], BF16) for i in range(4)]
                qz = sb(pat, "qz", [128, 2, 4], BF16)
                Es = sb(pat, "Es", [128, 8], BF16)
                En = sb(pat, "En", [4, 8], BF16)
                Tsamp = sb(pat, "Tsamp", [65, 8], F32)
                rden = sb(pat, "rden", [65, 512], F32)
                bcs = sb(pat, "bcs", [64, 512], F32)
                T.op('pool', lambda e: e.memset(V_ext[:], 1.0), writes=['V_ext'])
                T.op('pool', lambda e: e.memset(Vs_ext[:], 1.0), writes=['Vs_ext'])
                T.op('pool', lambda e: e.memset(qz[:], 0.0), writes=['qz'])
                for i in range(4):
                    T.op('pool', lambda e, i=i: e.memset(vg[i][:], 1.0), writes=[('vg', i)])

                pend_norm = []
                state = {'slab': 0, 'mm': 0, 'E': 0, 'stg': 0, 'stv': 0, 'sts': 0, 'kg': 0, 'vg': 0}

                def load_slab(col0):
                    s = state['slab'] % NSLAB
                    state['slab'] += 1
                    T.dma('pool', "slab%d" % s, slab[s][:],
                          w_in[:, col0:col0 + 128].rearrange("(k p) n -> p k n", p=128), writes=[('slab', s)])
                    return s

                def mmbank():
                    i = state['mm'] % 2
                    state['mm'] += 1
                    return i

                def proj_fm(s, rhs_fn, N):
                    bi = mmbank()

                    def f(e):
                        for k in range(16):
                            ins = e.matmul(pb[bi][:, 0:N], lhsT=slab[s][:, k, :], rhs=rhs_fn(k),
                                           start=(k == 0), stop=(k == 15))
                        return ins
                    T.op('pe', f, reads=[('slab', s)], writes=[PBK[bi]])
                    if pend_norm:
                        pend_norm.pop(0)()
                    return bi

                def rope(bi, N, tc0, dst_bf, dst_key, want_f32=None):
                    z = zb[bi]
                    t1, t2 = t1s[bi], t2s[bi]
                    k1, k2 = ('t1', bi), ('t2', bi)
                    T.op('act', lambda e: e.copy(out=z[:, 0:N], in_=pb[bi][:, 0:N]), reads=[PBK[bi]], writes=[('zb', bi)])
                    T.op('pe', lambda e: e.matmul(pb[2][:, 0:N], lhsT=rperm[:], rhs=z[:, 0:N], start=True, stop=True),
                         reads=[('zb', bi), 'rperm'], writes=[PBK[2]])
                    T.op('dve', lambda e: e.tensor_tensor(out=t1[:, 0:N], in0=pb[bi][:, 0:N], in1=ropeC[:, tc0:tc0 + N],
                                                          op=ALU.mult), reads=[PBK[bi], 'ropeC'], writes=[k1])
                    T.op('dve', lambda e: e.tensor_tensor(out=t2[:, 0:N], in0=pb[2][:, 0:N], in1=ropeS[:, tc0:tc0 + N],
                                                          op=ALU.mult), reads=[PBK[2], 'ropeS'], writes=[k2])
                    if want_f32 is None:
                        T.op('pool', lambda e: e.tensor_tensor(out=dst_bf, in0=t1[:, 0:N], in1=t2[:, 0:N], op=ALU.add),
                             reads=[k1, k2], writes=[dst_key])
                    else:
                        fdst, fkey = want_f32
                        T.op('pool', lambda e: e.tensor_tensor(out=fdst, in0=t1[:, 0:N], in1=t2[:, 0:N], op=ALU.add),
                             reads=[k1, k2], writes=[fkey])
                        T.op('act', lambda e: e.copy(out=dst_bf, in_=fdst), reads=[fkey], writes=[dst_key])

                for hg in range(8):
                    if hg == 1:
                        _stop(2)
                    TH = [[pb[3], pb[4]], [pb[5], pb[6]]]
                    THK = [[PBK[3], PBK[4]], [PBK[5], PBK[6]]]
                    def zero_th():
                        for hh_ in range(2):
                            for bk_ in range(2):
                                T.op('dve', lambda e, hh_=hh_, bk_=bk_: e.memset(TH[hh_][bk_][0:65, :], 0.0), writes=[THK[hh_][bk_]])
                    for p in range(3):
                        d = DIL[p]
                        halo = HALO[p]
                        s0 = 2048 - halo
                        Tp = halo + OWN
                        qc = p * 3072 + hg * 128
                        s_k = load_slab(qc + 1024)
                        s_v = load_slab(qc + 2048)
                        s_q = load_slab(qc)
                        for b in range(4):
                            T.dma('pool', "kg%d" % b, kg[b][:, :], c_in[p][b, 0:CL[p]:d, 0, hg * 128:hg * 128 + 128],
                                  writes=[('kg', b)])
                            T.dma('pool', "vg%d" % b, vg[b][:, :, 0:64],
                                  c_in[p][b, 0:CL[p]:d, 1, hg * 128:hg * 128 + 128].rearrange("q (h c) -> q h c", c=64),
                                  writes=[('vg', b)])
                        pend_rope = []
                        c = 0
                        while c < Tp:
                            N = min(512, Tp - c)
                            if halo == 128 and c == 0:
                                N = 128
                            xc = s0 + c
                            bi = proj_fm(s_k, lambda k, xc=xc, N=N: xT[:, k, xc:xc + N], N)
                            def post_k(bi=bi, N=N, xc=xc, c=c):
                                tau0 = xc - 2048
                                need = (tau0 + N > KVP_FROM[p]) and tau0 >= 0
                                if need:
                                    rope(bi, N, xc, kT[:, c:c + N], ('kT', c), want_f32=(kf[:, 0:N], 'kf'))
                                    g = state['stg'] % 2
                                    state['stg'] += 1
                                    j0 = max(0, (KVP_FROM[p] - tau0) // 128)
                                    nj = N // 128

                                    def f(e, j0=j0, nj=nj, N=N):
                                        for j in range(j0, nj):
                                            ins = e.transpose(out=pb[7][:, 128 * j:128 * j + 128], in_=kf[:, 128 * j:128 * j + 128],
                                                              identity=ident[:])
                                        return ins
                                    T.op('pe', f, reads=['kf', 'ident'], writes=[PBK[7]])
                                    T.op('dve', lambda e, g=g, j0=j0, N=N: e.tensor_copy(out=stg[g][:, 128 * j0:N],
                                                                                         in_=pb[7][:, 128 * j0:N]),
                                         reads=[PBK[7]], writes=[('stg', g)])
                                    r0 = tau0 + 128 * j0 - KVP_FROM[p]
                                    T.dma('sp', "stg%d" % g,
                                          kvp[p][r0:r0 + 128 * (nj - j0), 0, hg * 128:hg * 128 + 128].rearrange("(j q) c -> q j c", q=128),
                                          stg[g][:, 128 * j0:N].rearrange("q (j c) -> q j c", c=128), reads=[('stg', g)])
                                else:
                                    rope(bi, N, xc, kT[:, c:c + N], ('kT', c))

                            if pend_rope:
                                pend_rope.pop(0)()
                            pend_rope.append(post_k)
                            c += N
                        while pend_rope:
                            pend_rope.pop(0)()
                        if hg == 0 and p == 0:
                            _stop(1.1)
                        bi = proj_fm(s_k, lambda k: xsT[:, k, :], 4)
                        rope(bi, 4, 3072, ksT[:, :], 'ksT', want_f32=(ksf[:, :], 'ksf'))
                        g = state['sts'] % 2
                        state['sts'] += 1
                        T.op('pe', lambda e: e.transpose(out=pb[7][0:4, 0:128], in_=ksf[:, :], identity=ident[:]),
                             reads=['ksf', 'ident'], writes=[PBK[7]])
                        T.op('dve', lambda e, g=g: e.tensor_copy(out=sts[g][:, :], in_=pb[7][0:4, 0:128]),
                             reads=[PBK[7]], writes=[('sts', g)])
                        T.dma('sp', "sts%d" % g, kvs[p][:, CL[p] - 1, 0, hg * 128:hg * 128 + 128], sts[g][:, :],
                              reads=[('sts', g)])
                        if hg == 0 and p == 0:
                            _stop(1.2)
                        Lr = Tp // d
                        ntr = (Lr + 127) // 128
                        tiles = [(r, b) for r in range(d) for b in range(ntr)]
                        for t0 in range(0, len(tiles), 4):
                            grp = tiles[t0:t0 + 4]
                            bi = mmbank()

                            def f(e, grp=grp, bi=bi):
                                for i, (r, b) in enumerate(grp):
                                    nk = min(128, Lr - 128 * b)
                                    a = s0 + d * 128 * b + r
                                    for k in range(16):
                                        ins = e.matmul(pb[bi][0:nk, 128 * i:128 * i + 128],
                                                       lhsT=xT[:, k, a:a + d * (nk - 1) + 1:d], rhs=slab[s_v][:, k, :],
                                                       start=(k == 0), stop=(k == 15))
                                return ins
                            T.op('pe', f, reads=[('slab', s_v)], writes=[PBK[bi]])
                            ng = len(grp)
                            T.op('act', lambda e, t0=t0, ng=ng, bi=bi: e.copy(
                                out=V_ext[:, t0:t0 + ng, :, 0:64],
                                in_=pb[bi][:, 0:128 * ng].rearrange("q (i h c) -> q i h c", h=2, c=64)),
                                reads=[PBK[bi]], writes=['V_ext'])
                            for i, (r, b) in enumerate(grp):
                                nk = min(128, Lr - 128 * b)
                                tau0 = d * 128 * b + r - halo
                                if tau0 >= KVP_FROM[p]:
                                    g = state['stv'] % 4
                                    state['stv'] += 1
                                    T.op('dve', lambda e, g=g, i=i, nk=nk, bi=bi: e.tensor_copy(
                                        out=stv[g][0:nk, :], in_=pb[bi][0:nk, 128 * i:128 * i + 128]),
                                        reads=[PBK[bi]], writes=[('stv', g)])
                                    r0 = tau0 - KVP_FROM[p]
                                    T.dma('sp', "stv%d" % g, kvp[p][r0:r0 + d * (nk - 1) + 1:d, 1, hg * 128:hg * 128 + 128],
                                          stv[g][0:nk, :], reads=[('stv', g)])
                        bi = mmbank()

                        def f(e, bi=bi):
                            for k in range(16):
                                ins = e.matmul(pb[bi][0:4, 0:128], lhsT=xsT[:, k, :], rhs=slab[s_v][:, k, :],
                                               start=(k == 0), stop=(k == 15))
                            return ins
                        T.op('pe', f, reads=[('slab', s_v)], writes=[PBK[bi]])
                        T.op('act', lambda e, bi=bi: e.copy(out=Vs_ext[:, :, 0:64],
                                                            in_=pb[bi][0:4, 0:128].rearrange("q (h c) -> q h c", c=64)),
                             reads=[PBK[bi]], writes=['Vs_ext'])
                        g = state['sts'] % 2
                        state['sts'] += 1
                        T.op('dve', lambda e, g=g, bi=bi: e.tensor_copy(out=sts[g][:, :], in_=pb[bi][0:4, 0:128]),
                             reads=[PBK[bi]], writes=[('sts', g)])
                        T.dma('sp', "sts%d" % g, kvs[p][:, CL[p] - 1, 1, hg * 128:hg * 128 + 128], sts[g][:, :],
                              reads=[('sts', g)])
                        if hg == 0 and p == 0:
                            _stop(1.3)
                        for (c0, N) in CHUNKS:
                            if c0 < OWN:
                                bi = proj_fm(s_q, lambda k, c0=c0, N=N: xT[:, k, 2048 + c0:2048 + c0 + N], N)
                                if pend_rope:
                                    pend_rope.pop(0)()
                                pend_rope.append(lambda bi=bi, N=N, c0=c0: rope(bi, N, 2048 + c0, qT[:, c0:c0 + N], ('qT', c0)))
                            else:
                                bi = proj_fm(s_q, lambda k: xsT[:, k, :], 4)
                                while pend_rope:
                                    pend_rope.pop(0)()
                                rope(bi, 4, 3072, qT[:, OWN:OWN + 4], ('qT', c0))
                                T.op('dve', lambda e: e.tensor_copy(out=qz[0:64, 0, :], in_=qT[0:64, OWN:OWN + 4]),
                                     reads=[('qT', c0)], writes=['qz'])
                                T.op('dve', lambda e: e.tensor_copy(out=qz[64:128, 1, :], in_=qT[64:128, OWN:OWN + 4]),
                                     reads=[('qT', c0)], writes=['qz'])
                        kT_keys = [('kT', cc) for cc in ([0, 128, 640] if halo == 128 else list(range(0, Tp, 512)))]
                        qT_keys = [('qT', 0), ('qT', 512), ('qT', 1024)]

                        if hg == 0 and p == 0:
                            _stop(1.4)
                        if p == 0:
                            while pend_norm:
                                pend_norm.pop(0)()
                            zero_th()
                        pend_pv = []

                        def att_tile(hh, k_aps, q_ap, nq, mask_ap, pv):
                            if NOATT[0]:
                                return
                            es_ = state['E'] % 4
                            sbank = (0, 1, 2, 7)[state['E'] % 4]
                            state['E'] += 1
                            pk = PBK[sbank]
                            base = 0

                            def f(e):
                                for b, (kap, nk) in enumerate(k_aps):
                                    ins = e.matmul(pb[sbank][0:nk, base + nq * b:base + nq * b + nq], lhsT=kap, rhs=q_ap,
                                                   start=True, stop=True)
                                return ins
                            T.op('pe', f, reads=kT_keys + qT_keys + ['ksT'], writes=[pk])
                            W = nq * len(k_aps)
                            T.op('act', lambda e: e.activation(out=Eb[es_][:, 0:W], in_=pb[sbank][:, base:base + W], func=AF.Exp,
                                                               scale=0.125), reads=[pk], writes=[('E', es_)])
                            T.op('dve' if (state['E'] % 2) else 'pool', lambda e: e.tensor_tensor(out=Eb[es_][:, 0:W], in0=Eb[es_][:, 0:W], in1=mask_ap,
                                                                   op=ALU.mult), reads=[('E', es_), 'mk'], writes=[('E', es_)])
                            def do_pv():
                                for (b, vt, ec0, en, bk, oap, colkey) in pv:
                                    nk = k_aps[b][1]
                                    T.op('pe', lambda e, vt=vt, ec0=ec0, en=en, oap=oap, nk=nk: e.matmul(
                                        oap, lhsT=V_ext[0:nk, vt, hh, :], rhs=Eb[es_][0:nk, ec0:ec0 + en], start=False, stop=False,
                                        skip_group_check=True), reads=[('E', es_), 'V_ext'], writes=[THK[hh][bk]])
                            pend_pv.append(do_pv)
                            if len(pend_pv) > 2:
                                pend_pv.pop(0)()

                        for hh in range(2):
                            P0 = 64 * hh
                            if p == 0:
                                for i in range(8):
                                    k_aps = [(kT[P0:P0 + 64, 128 * (i + b):128 * (i + b) + 128], 128) for b in range(2)]
                                    q_ap = qT[P0:P0 + 64, 128 * i:128 * i + 128]
                                    m = mk[:, 1 if i == 0 else 0, :]
                                    bk = i // 4
                                    oap = TH[hh][bk][0:65, 128 * (i % 4):128 * (i % 4) + 128]
                                    pv = [(b, i + b, 128 * b, 128, bk, oap, ('c', i)) for b in range(2)]
                                    att_tile(hh, k_aps, q_ap, 128, m, pv)
                            elif p == 1:
                                for r in range(4):
                                    for j in range(2):
                                        k_aps = [(kT[P0:P0 + 64, 512 * (j + b) + r:512 * (j + b + 1):4], 128) for b in range(2)]
                                        q_ap = qT[P0:P0 + 64, 512 * j + r:512 * (j + 1):4]
                                        m = mk[:, 2 if j == 0 else 0, :]
                                        oap = TH[hh][j][0:65, r:512:4]
                                        pv = [(b, r * 3 + j + b, 128 * b, 128, j, oap, ('s4', r)) for b in range(2)]
                                        att_tile(hh, k_aps, q_ap, 128, m, pv)
                            else:
                                for r in range(16):
                                    k_aps = [(kT[P0:P0 + 64, r:2048:16], 128), (kT[P0:P0 + 64, 2048 + r:3072:16], 64)]
                                    q_ap = qT[P0:P0 + 64, r:1024:16]
                                    m = mk[:, 3, 0:128]
                                    pv = []
                                    for b in range(2):
                                        for bk in range(2):
                                            oap = TH[hh][bk][0:65, r:512:16]
                                            pv.append((b, r * 2 + b, 64 * b + 32 * bk, 32, bk, oap, ('s16', r)))
                                    att_tile(hh, k_aps, q_ap, 64, m, pv)

                        while pend_pv:
                            pend_pv.pop(0)()
                        if hg == 0 and p == 0:
                            _stop(1.5)
                        L = CL[p]
                        for b in (range(4) if not NOSAMP[0] else []):
                            g = b
                            gv = b
                            kb = pb[1][:].bitcast(BF16)
                            T.op('pe', lambda e, g=g, kb=kb: e.transpose(out=kb[:, 0:128], in_=kg[g][:, :], identity=identb[:]),
                                 reads=[('kg', g), 'identb'], writes=[PBK[1]])
                            T.op('dve', lambda e, g=g, kb=kb: e.tensor_copy(out=kcT[g][:, :], in_=kb[:, 0:128]),
                                 reads=[PBK[1]], writes=[('kcT', g)])

                            def f(e, g=g, b=b):
                                for hh in range(2):
                                    ins = e.matmul(pb[0][:, 4 * hh + b:4 * hh + b + 1], lhsT=kcT[g][:, :],
                                                   rhs=qz[:, hh, b:b + 1], start=True, stop=True)
                                return ins
                            T.op('pe', f, reads=[('kcT', g), 'qz'], writes=[PBK[0]])
                            T.op('act', lambda e, b=b: e.activation(out=Es[:, b:8:4], in_=pb[0][:, b:8:4], func=AF.Exp, scale=0.125),
                                 reads=[PBK[0]], writes=['Es'])

                            def f2(e, gv=gv, b=b):
                                for hh in range(2):
                                    ins = e.matmul(pb[0][0:65, 32 + 4 * hh + b:32 + 4 * hh + b + 1], lhsT=vg[gv][:, hh, :],
                                                   rhs=Es[:, 4 * hh + b:4 * hh + b + 1], start=True, stop=False,
                                                   skip_group_check=True)
                                return ins
                            T.op('pe', f2, reads=[('vg', gv), 'Es'], writes=[PBK[0]])
                        if hg == 0 and p == 0:
                            _stop(1.6)
                        def f3(e):
                            for hh in range(2):
                                ins = e.matmul(pb[0][0:4, 16 + 4 * hh:16 + 4 * hh + 4], lhsT=ksT[:, :],
                                               rhs=qz[:, hh, :], start=True, stop=True)
                            return ins
                        T.op('pe', f3, reads=['ksT', 'qz'], writes=[PBK[0]])
                        T.op('act', lambda e: e.activation(out=En[:, :], in_=pb[0][0:4, 16:24], func=AF.Exp, scale=0.125),
                             reads=[PBK[0]], writes=['En'])
                        for hh_ in range(2):
                            T.op('dve', lambda e, hh_=hh_: e.tensor_tensor(out=En[:, 4 * hh_:4 * hh_ + 4], in0=En[:, 4 * hh_:4 * hh_ + 4],
                                                                           in1=identb[0:4, 0:4], op=ALU.mult),
                                 reads=['En', 'identb'], writes=['En'])

                        def f4(e):
                            for hh in range(2):
                                ins = e.matmul(pb[0][0:65, 40 + 4 * hh:40 + 4 * hh + 4], lhsT=Vs_ext[:, hh, :],
                                               rhs=En[:, 4 * hh:4 * hh + 4], start=True, stop=True, skip_group_check=True)
                            return ins
                        T.op('pe', f4, reads=['Vs_ext', 'En'], writes=[PBK[0]])
                        if p == 0:
                            T.op('dve', lambda e: e.tensor_copy(out=Tsamp[:, :], in_=pb[0][0:65, 32:40]), reads=[PBK[0]], writes=['TS'])
                        else:
                            T.op('dve', lambda e: e.tensor_tensor(out=Tsamp[:, :], in0=pb[0][0:65, 32:40], in1=Tsamp[:, :], op=ALU.add),
                                 reads=[PBK[0], 'TS'], writes=['TS'])
                        T.op('dve', lambda e: e.tensor_tensor(out=Tsamp[:, :], in0=pb[0][0:65, 40:48], in1=Tsamp[:, :], op=ALU.add),
                             reads=[PBK[0], 'TS'], writes=['TS'])

                    if hg == 0:
                        _stop(1.8)
                    def normalise(hh, bk, hg=hg, TH=TH, THK=THK):
                        if True:
                            P0 = 64 * hh
                            if True:
                                if bk < 2:
                                    src = TH[hh][bk]
                                    skey = THK[hh][bk]
                                    c_a, c_b, N = 0, 512 * bk, 512
                                else:
                                    src = Tsamp
                                    skey = 'TS'
                                    c_a, c_b, N = 4 * hh, OWN, 4
                                T.op('dve', lambda e, src=src, c_a=c_a, N=N: e.reciprocal(out=rden[64:65, 0:N], in_=src[64:65, c_a:c_a + N]),
                                     reads=[skey], writes=['rden'])
                                T.op('pe', lambda e, N=N: e.matmul(pb[7][0:64, 0:N], lhsT=ones_f[64:65, 0:64], rhs=rden[64:65, 0:N],
                                                                   start=True, stop=True), reads=['rden', 'ones_f'], writes=[PBK[7]])
                                T.op('act', lambda e, N=N: e.copy(out=bcs[:, 0:N], in_=pb[7][0:64, 0:N]), reads=[PBK[7]], writes=['bcs'])
                                T.op('dve', lambda e, src=src, c_a=c_a, c_b=c_b, N=N, P0=P0: e.tensor_tensor(
                                    out=mixT[P0:P0 + 64, hg, c_b:c_b + N], in0=src[0:64, c_a:c_a + N], in1=bcs[:, 0:N], op=ALU.mult),
                                    reads=[skey, 'bcs'], writes=[('mixT', hg, hh, bk)])
                    for hh_ in range(2):
                        for bk_ in range(3):
                            pend_norm.append(lambda hh_=hh_, bk_=bk_, fn=normalise: fn(hh_, bk_))

                while pend_norm:
                    pend_norm.pop(0)()

                T.barrier()
                pat.close()
                _stop(3)
                with ExitStack() as pp_:
                    uA = sb(pp_, "uA", [128, 2, 1040], F32)
                    uB = sb(pp_, "uB", [128, 1040], F32)
                    uC = sb(pp_, "uC", [128, 1040], F32)
                    usT = sb(pp_, "usT", [128, 2, 4], F32)
                    diffT = sb(pp_, "diffT", [128, 2, NTOK], BF16)
                    wpl = sb(pp_, "wpl", [128, 2, 256], BF16)
                    psc = sb(pp_, "psc", [128, 8], F32)
                    invc = sb(pp_, "invc", [128, 4, 16], F32)
                    sel = sb(pp_, "sel", [60, 4, 4], F32)
                    stt = sb(pp_, "stt", [60, 1024], F32)
                    d16 = sb(pp_, "d16", [128, 16], F32)
                    ssum = sb(pp_, "ssum", [128, 4], F32)
                    ost = sb(pp_, "ost", [16, 128], F32)
                    ost2 = sb(pp_, "ost2", [4, 128], F32)
                    T.dma('sp', "k0", invc[:], invc_d[:, :, :], writes=['invc'])
                    T.dma('sp', "k1", sel[:], sel_d[:, :, :], writes=['sel'])
                    T.dma('sp', "k2", stt[:], spool.rearrange("b j c -> (b j) c"), writes=['stt'])
                    for j in range(8):
                        T.dma('sp', "k3", psc[:, j:j + 1], pool_scale[128 * j:128 * j + 128, :], writes=['psc'])
                    for gi in range(4):
                        win = POOL_WINDOWS[gi]
                        T.dma('pool', "wpl", wpl[:], w_pool[gi].rearrange("(k q) n -> q k n", q=128), writes=['wpl'])
                        for ct in range(2):
                            jt = 2 * gi + ct
                            s_u = load_slab(9216 + 128 * jt)
                            for (xc, N, dst, dkey) in [(2032, 16, uA[:, ct, 0:16], ('uA', ct, 0)),
                                                       (2048, 512, uA[:, ct, 16:528], ('uA', ct, 1)),
                                                       (2560, 512, uA[:, ct, 528:1040], ('uA', ct, 2)),
                                                       (-1, 4, usT[:, ct, :], ('usT', ct))]:
                                if xc >= 0:
                                    bi = proj_fm(s_u, lambda k, xc=xc, N=N: xT[:, k, xc:xc + N], N)
                                else:
                                    bi = proj_fm(s_u, lambda k: xsT[:, k, :], 4)
                                T.op('act', lambda e, bi=bi, N=N, dst=dst: e.copy(out=dst, in_=pb[bi][:, 0:N]),
                                     reads=[PBK[bi]], writes=[dkey])
                            ukeys = [('uA', ct, i) for i in range(3)]
                            T.op('pe', lambda e, ct=ct: e.transpose(out=pb[7][0:16, 0:128], in_=uA[:, ct, 1024:1040], identity=ident[:]),
                                 reads=ukeys + ['ident'], writes=[PBK[7]])
                            T.op('dve', lambda e: e.tensor_copy(out=ost[:, :], in_=pb[7][0:16, 0:128]), reads=[PBK[7]], writes=['ost'])
                            T.dma('sp', "ost", poolp[:, 128 * jt:128 * jt + 128], ost[:, :], reads=['ost'])
                            T.op('pe', lambda e, ct=ct: e.transpose(out=pb[7][0:4, 128:256], in_=usT[:, ct, :], identity=ident[:]),
                                 reads=[('usT', ct), 'ident'], writes=[PBK[7]])
                            T.op('dve', lambda e: e.tensor_copy(out=ost2[:, :], in_=pb[7][0:4, 128:256]), reads=[PBK[7]], writes=['ost2'])
                            T.dma('sp', "ost2", pools[:, 14, 128 * jt:128 * jt + 128], ost2[:, :], reads=['ost2'])
                            cur, ckey = uA[:, ct, :], ukeys
                            bufs2 = [(uB, 'uB'), (uC, 'uC')]
                            step, n = 1, 0
                            while step < win:
                                dst, dk = bufs2[n % 2]
                                T.op('dve', lambda e, cur=cur, dst=dst, step=step: e.tensor_tensor(
                                    out=dst[:, step:1040], in0=cur[:, step:1040], in1=cur[:, 0:1040 - step], op=ALU.add),
                                    reads=(ckey if isinstance(ckey, list) else [ckey]), writes=[dk])
                                cur, ckey = dst[:, :], dk
                                step *= 2
                                n += 1
                            T.op('dve', lambda e, cur=cur, ct=ct: e.scalar_tensor_tensor(
                                out=diffT[:, ct, 0:OWN], in0=cur[:, 16:1040], scalar=1.0 / win, in1=uA[:, ct, 16:1040],
                                op0=ALU.mult, op1=ALU.subtract), reads=[ckey] + ukeys, writes=[('diffT', ct)])
                            T.op('dve', lambda e, cur=cur: e.tensor_tensor(out=d16[:, :], in0=cur[:, 16:32], in1=invc[:, gi, :],
                                                                           op=ALU.mult), reads=[ckey, 'invc'], writes=['d16'])
                            T.op('dve', lambda e, ct=ct: e.tensor_tensor(out=diffT[:, ct, 0:16], in0=d16[:, :], in1=uA[:, ct, 16:32],
                                                                         op=ALU.subtract), reads=['d16'] + ukeys, writes=[('diffT', ct)])
                            T.op('pe', lambda e, jt=jt: e.matmul(pb[7][:, 256:260], lhsT=stt[:, 128 * jt:128 * jt + 128], rhs=sel[:, gi, :],
                                                                 start=True, stop=True), reads=['stt', 'sel'], writes=[PBK[7]])
                            T.op('dve', lambda e, ct=ct: e.tensor_tensor(out=ssum[:, :], in0=pb[7][:, 256:260], in1=usT[:, ct, :], op=ALU.add),
                                 reads=[PBK[7], ('usT', ct)], writes=['ssum'])
                            T.op('dve', lambda e, ct=ct: e.scalar_tensor_tensor(
                                out=diffT[:, ct, OWN:OWN + 4], in0=ssum[:, :], scalar=1.0 / win, in1=usT[:, ct, :],
                                op0=ALU.mult, op1=ALU.subtract), reads=['ssum', ('usT', ct)], writes=[('diffT', ct)])
                        for dt_ in range(2):
                            for (c0, N) in CHUNKS:
                                bi = mmbank()

                                def f(e, bi=bi, dt_=dt_, c0=c0, N=N):
                                    for cc in range(2):
                                        ins = e.matmul(pb[bi][:, 0:N], lhsT=wpl[:, cc, 128 * dt_:128 * dt_ + 128],
                                                       rhs=diffT[:, cc, c0:c0 + N], start=(cc == 0), stop=(cc == 1))
                                    return ins
                                T.op('pe', f, reads=['wpl', ('diffT', 0), ('diffT', 1)], writes=[PBK[bi]])
                                jd = 2 * gi + dt_
                                T.op('dve', lambda e, bi=bi, jd=jd, c0=c0, N=N: e.tensor_scalar(
                                    out=mixT[:, 8 + jd, c0:c0 + N], in0=pb[bi][:, 0:N], scalar1=psc[:, jd:jd + 1], scalar2=None,
                                    op0=ALU.mult), reads=[PBK[bi], 'psc'], writes=[('mixTp', jd, c0)])
                    T.barrier()
            T.barrier()

            _stop(4)
            stream = sb(es, "stream", [128, 9, 2048], F32)
            xnT = mixT
            comb = sb(es, "comb", [128, 9, 32], F32)
            st1 = sb(es, "st1", [128, 8], F32)
            LB = {}

            def alloc_ln(scope, tag):
                LB['gam'] = sb(scope, "gam" + tag, [128, 2048], F32)
                LB['bet'] = sb(scope, "bet" + tag, [128, 2048], F32)
                LB['sq'] = sb(scope, "sq" + tag, [128, 2048], F32)
                LB['xnb'] = sb(scope, "xnb" + tag, [128, 2048], BF16)

            def layer_norm(tt, nt, eps):
                v = stream[0:nt, tt, :]
                k = ('stream', tt)
                gam, bet, sq = LB['gam'], LB['bet'], LB['sq']
                T.op('dve', lambda e: e.tensor_reduce(out=st1[0:nt, 0:1], in_=v, axis=AX.X, op=ALU.add), reads=[k], writes=['st_a'])
                T.op('pool', lambda e: e.tensor_tensor(out=sq[0:nt, :], in0=v, in1=v, op=ALU.mult), reads=[k], writes=['sq'])
                T.op('dve', lambda e: e.tensor_reduce(out=st1[0:nt, 1:2], in_=sq[0:nt, :], axis=AX.X, op=ALU.add),
                     reads=['sq'], writes=['st_b'])
                T.op('dve', lambda e: e.tensor_scalar(out=st1[0:nt, 2:4], in0=st1[0:nt, 0:2], scalar1=1.0 / 2048, scalar2=None,
                                                      op0=ALU.mult), reads=['st_a', 'st_b'], writes=['st_c'])
                T.op('dve', lambda e: e.tensor_tensor(out=st1[0:nt, 4:5], in0=st1[0:nt, 2:3], in1=st1[0:nt, 2:3], op=ALU.mult),
                     reads=['st_c'], writes=['st_d'])
                T.op('dve', lambda e: e.tensor_tensor(out=st1[0:nt, 5:6], in0=st1[0:nt, 3:4], in1=st1[0:nt, 4:5], op=ALU.subtract),
                     reads=['st_c', 'st_d'], writes=['st_e'])
                T.op('dve', lambda e: e.tensor_scalar(out=st1[0:nt, 7:8], in0=st1[0:nt, 5:6], scalar1=eps, scalar2=None,
                                                      op0=ALU.add), reads=['st_e'], writes=['st_g'])
                T.op('act', lambda e: e.activation(out=st1[0:nt, 7:8], in_=st1[0:nt, 7:8], func=AF.Sqrt), reads=['st_g'], writes=['st_g'])
                T.op('dve', lambda e: e.reciprocal(out=st1[0:nt, 6:7], in_=st1[0:nt, 7:8]), reads=['st_g'], writes=['st_f'])
                T.op('dve', lambda e: e.tensor_scalar(out=v, in0=v, scalar1=st1[0:nt, 2:3], scalar2=st1[0:nt, 6:7],
                                                      op0=ALU.subtract, op1=ALU.mult), reads=[k, 'st_c', 'st_f'], writes=[k])
                T.op('pool', lambda e: e.tensor_tensor(out=v, in0=v, in1=gam[0:nt, :], op=ALU.mult), reads=[k, 'gam'], writes=[k])
                T.op('pool', lambda e: e.tensor_tensor(out=v, in0=v, in1=bet[0:nt, :], op=ALU.add), reads=[k, 'bet'], writes=[k])

            def to_T(tt, t0, nt):
                k = ('stream', tt)
                xnb = LB['xnb']
                T.op('act', lambda e: e.copy(out=xnb[0:nt, :], in_=stream[0:nt, tt, :]), reads=[k], writes=['xnb'])
                for g in range(2):
                    bi = mmbank()
                    bank = pb[bi][:].bitcast(BF16)

                    def f(e, g=g, bank=bank):
                        for j in range(8):
                            kk = 8 * g + j
                            ins = e.transpose(out=bank[:, nt * j:nt * j + nt], in_=xnb[0:nt, 128 * kk:128 * kk + 128],
                                              identity=identb[0:nt, 0:nt])
                        return ins
                    T.op('pe', f, reads=['xnb', 'identb'], writes=[PBK[bi]])
                    T.op('dve', lambda e, g=g, bank=bank: e.tensor_copy(out=xnT[:, 8 * g:8 * g + 8, t0:t0 + nt],
                                                                        in_=bank[:, 0:8 * nt].rearrange("q (j c) -> q j c", c=nt)),
                         reads=[PBK[bi]], writes=[('xnT', tt, g)])

            with ExitStack() as pB:
                wo = [sb(pB, "wo%d" % i, [128, 16, 512], BF16) for i in range(2)]
                xres = [sb(pB, "xres%d" % i, [128, 512], F32) for i in range(3)]
                alloc_ln(pB, "B")
                gam, bet = LB['gam'], LB['bet']
                T.dma('sp', "k0", gam[:], ln_gb[0].partition_broadcast(128), writes=['gam'])
                T.dma('sp', "k1", bet[:], ln_gb[1].partition_broadcast(128), writes=['bet'])
                n_x = 0
                for cg in range(4):
                    s = cg % 2
                    T.dma('pool', "wo%d" % s, wo[s][:], w_out[:, 512 * cg:512 * cg + 512].rearrange("(k q) n -> q k n", q=128),
                          writes=[('wo', s)])
                    for tt, (t0, nt) in enumerate(TT):
                        xs_ = n_x % 3
                        n_x += 1
                        src = xh[2048 + t0:2048 + t0 + nt, 512 * cg:512 * cg + 512] if tt < 8 else xs[:, 512 * cg:512 * cg + 512]
                        T.dma('sp', "xres%d" % xs_, xres[xs_][0:nt, :], src, writes=[('xres', xs_)])
                        bi = mmbank()

                        def f(e, bi=bi, s=s, t0=t0, nt=nt):
                            for k in range(16):
                                ins = e.matmul(pb[bi][0:nt, :], lhsT=mixT[:, k, t0:t0 + nt], rhs=wo[s][:, k, :],
                                               start=(k == 0), stop=(k == 15))
                            return ins
                        T.op('pe', f, reads=[('wo', s)], writes=[PBK[bi]])
                        T.op('dve', lambda e, bi=bi, xs_=xs_, tt=tt, nt=nt, cg=cg: e.scalar_tensor_tensor(
                            out=stream[0:nt, tt, 512 * cg:512 * cg + 512], in0=xres[xs_][0:nt, :], scalar=ALPHA, in1=pb[bi][0:nt, :],
                            op0=ALU.mult, op1=ALU.add), reads=[PBK[bi], ('xres', xs_)], writes=[('stream', tt)])
                        if cg == 3:
                            layer_norm(tt, nt, LN_EPS)
                            to_T(tt, t0, nt)
            T.barrier()

            _stop(5)
            with ExitStack() as pC:
                wr = sb(pC, "wr", [128, 16, 36], BF16)
                brow = sb(pC, "brow", [1, 36], BF16)
                lg = sb(pC, "lg", [128, 36], F32)
                rt = sb(pC, "rt", [128, 160], F32)
                T.dma('pool', "k0", wr[:, :, 0:4], w_gr.rearrange("(k q) n -> q k n", q=128), writes=['wr'])
                T.dma('pool', "k1", wr[:, :, 4:36], w_er.rearrange("(k q) n -> q k n", q=128), writes=['wr'])
                T.dma('pool', "k2", brow[:], b_r[:, :], writes=['brow'])
                BIG = 1.0e9
                for tt, (t0, nt) in enumerate(TT):
                    bi = mmbank()

                    def f(e, bi=bi, t0=t0, nt=nt):
                        for k in range(16):
                            e.matmul(pb[bi][0:nt, 0:36], lhsT=xnT[:, k, t0:t0 + nt], rhs=wr[:, k, :], start=(k == 0), stop=False)
                        return e.matmul(pb[bi][0:nt, 0:36], lhsT=ones_b[0:1, 0:nt], rhs=brow[0:1, :], start=False, stop=True)
                    T.op('pe', f, reads=['wr', 'brow', 'ones_b'], writes=[PBK[bi]])
                    R = lambda a, b_: rt[0:nt, a:b_]
                    ops = []
                    V = lambda fn: ops.append(fn)
                    V(lambda e: e.tensor_copy(out=lg[0:nt, :], in_=pb[bi][0:nt, 0:36]))
                    V(lambda e: e.tensor_reduce(out=R(0, 1), in_=lg[0:nt, 0:4], axis=AX.X, op=ALU.max))
                    V(lambda e: e.tensor_scalar(out=R(4, 8), in0=lg[0:nt, 0:4], scalar1=R(0, 1), scalar2=None, op0=ALU.is_equal))
                    V(lambda e: e.tensor_scalar(out=R(8, 12), in0=lg[0:nt, 0:4], scalar1=R(0, 1), scalar2=None, op0=ALU.subtract))
                    V(('act', lambda e: e.activation(out=R(8, 12), in_=R(8, 12), func=AF.Exp)))
                    V(lambda e: e.tensor_reduce(out=R(1, 2), in_=R(8, 12), axis=AX.X, op=ALU.add))
                    V(lambda e: e.reciprocal(out=R(2, 3), in_=R(1, 2)))
                    V(lambda e: e.tensor_scalar(out=R(12, 16), in0=R(4, 8), scalar1=-1.0, scalar2=BIG, op0=ALU.add, op1=ALU.mult))
                    V(lambda e: e.tensor_tensor(out=R(16, 48).rearrange("q (g c) -> q g c", c=8),
                                                in0=lg[0:nt, 4:36].rearrange("q (g c) -> q g c", c=8),
                                                in1=R(12, 16).unsqueeze(2).to_broadcast([nt, 4, 8]), op=ALU.add))
                    V(lambda e: e.tensor_reduce(out=R(3, 4), in_=R(16, 48), axis=AX.X, op=ALU.max))
                    V(lambda e: e.tensor_scalar(out=R(48, 80), in0=R(16, 48), scalar1=R(3, 4), scalar2=None, op0=ALU.is_equal))
                    V(lambda e: e.scalar_tensor_tensor(out=R(80, 112), in0=R(48, 80), scalar=-BIG, in1=R(16, 48),
                                                       op0=ALU.mult, op1=ALU.add))
                    V(lambda e: e.tensor_reduce(out=R(112, 113), in_=R(80, 112), axis=AX.X, op=ALU.max))
                    V(lambda e: e.tensor_scalar(out=R(116, 148), in0=R(80, 112), scalar1=R(112, 113), scalar2=None, op0=ALU.is_equal))
                    V(lambda e: e.tensor_tensor(out=R(113, 114), in0=R(112, 113), in1=R(3, 4), op=ALU.subtract))
                    V(('act', lambda e: e.activation(out=R(113, 114), in_=R(113, 114), func=AF.Exp)))
                    V(lambda e: e.tensor_scalar(out=R(114, 115), in0=R(113, 114), scalar1=1.0, scalar2=None, op0=ALU.add))
                    V(lambda e: e.reciprocal(out=R(115, 116), in_=R(114, 115)))
                    V(lambda e: e.tensor_scalar(out=R(148, 149), in0=R(2, 3), scalar1=1.0 / ALPHA, scalar2=None, op0=ALU.mult))
                    V(lambda e: e.tensor_tensor(out=R(149, 150), in0=R(148, 149), in1=R(115, 116), op=ALU.mult))
                    V(lambda e: e.tensor_tensor(out=R(150, 151), in0=R(148, 149), in1=R(149, 150), op=ALU.subtract))
                    V(lambda e: e.tensor_scalar(out=R(48, 80), in0=R(48, 80), scalar1=R(149, 150), scalar2=None, op0=ALU.mult))
                    V(lambda e: e.scalar_tensor_tensor(out=comb[0:nt, tt, :], in0=R(116, 148), scalar=R(150, 151), in1=R(48, 80),
                                                       op0=ALU.mult, op1=ALU.add))
                    first = True
                    for o in ops:
                        eng, fn = (o if isinstance(o, tuple) else ('dve', o))
                        T.op(eng, fn, reads=['rt'] + ([PBK[bi]] if first else []), writes=['rt'])
                        first = False
                T.barrier()

                wg = [sb(pC, "wg%d" % i, [128, 16, 256], BF16) for i in range(2)]
                wu = [sb(pC, "wu%d" % i, [128, 16, 256], BF16) for i in range(2)]
                wd = [sb(pC, "wd%d" % i, [128, 2, 2048], BF16) for i in range(2)]
                hT = [sb(pC, "hT%d" % i, [128, 2, NTOK], BF16) for i in range(2)]
                sg = [sb(pC, "sg%d" % i, [128, 512], BF16) for i in range(2)]
                n_sg = 0
                n_gu = 0
                n_dn = 0
                for ex in range(32):
                    s = ex % 2
                    T.dma('pool', "wg%d" % s, wg[s][:], w_gate[ex].rearrange("(k q) n -> q k n", q=128), writes=[('wg', s)])
                    T.dma('pool', "wu%d" % s, wu[s][:], w_up[ex].rearrange("(k q) n -> q k n", q=128), writes=[('wu', s)])
                    T.dma('pool', "wd%d" % s, wd[s][:], w_down[ex].rearrange("(k q) n -> q k n", q=128), writes=[('wd', s)])
                    for (c0, N) in CHUNKS:
                        for fi in range(2):
                            bg = 2 * (n_gu % 2)
                            bu = bg + 1
                            n_gu += 1

                            def f(e, bg=bg, bu=bu, fi=fi, c0=c0, N=N, s=s):
                                for k in range(16):
                                    e.matmul(pb[bg][:, 0:N], lhsT=wg[s][:, k, 128 * fi:128 * fi + 128], rhs=xnT[:, k, c0:c0 + N],
                                             start=(k == 0), stop=(k == 15))
                                for k in range(16):
                                    ins = e.matmul(pb[bu][:, 0:N], lhsT=wu[s][:, k, 128 * fi:128 * fi + 128], rhs=xnT[:, k, c0:c0 + N],
                                                   start=(k == 0), stop=(k == 15))
                                return ins
                            T.op('pe', f, reads=[('wg', s), ('wu', s)], writes=[PBK[bg], PBK[bu]])
                            q = n_sg % 2
                            n_sg += 1
                            T.op('act', lambda e, q=q, bg=bg, N=N: e.activation(out=sg[q][:, 0:N], in_=pb[bg][:, 0:N], func=AF.Silu),
                                 reads=[PBK[bg]], writes=[('sg', q)])
                            T.op('dve', lambda e, q=q, bu=bu, N=N, fi=fi, c0=c0, s=s: e.tensor_tensor(
                                out=hT[s][:, fi, c0:c0 + N], in0=pb[bu][:, 0:N], in1=sg[q][:, 0:N], op=ALU.mult),
                                reads=[PBK[bu], ('sg', q)], writes=[('hT', s, fi, c0)])
                    hkeys = [('hT', s, fi, c0) for fi in range(2) for (c0, _) in CHUNKS]
                    for tt, (t0, nt) in enumerate(TT):
                        for cg in range(4):
                            bi = 4 + (n_dn % 4)
                            n_dn += 1

                            def f(e, bi=bi, t0=t0, nt=nt, cg=cg, s=s):
                                for fi in range(2):
                                    ins = e.matmul(pb[bi][0:nt, :], lhsT=hT[s][:, fi, t0:t0 + nt], rhs=wd[s][:, fi, 512 * cg:512 * cg + 512],
                                                   start=(fi == 0), stop=(fi == 1))
                                return ins
                            T.op('pe', f, reads=hkeys + [('wd', s)], writes=[PBK[bi]])
                            eng = 'dve' if (cg % 2 == 0) else 'pool'
                            if eng == 'dve':
                                T.op('dve', lambda e, bi=bi, tt=tt, nt=nt, cg=cg, ex=ex: e.scalar_tensor_tensor(
                                    out=stream[0:nt, tt, 512 * cg:512 * cg + 512], in0=pb[bi][0:nt, :], scalar=comb[0:nt, tt, ex:ex + 1],
                                    in1=stream[0:nt, tt, 512 * cg:512 * cg + 512], op0=ALU.mult, op1=ALU.add),
                                    reads=[PBK[bi], ('stream', tt, cg)], writes=[('stream', tt, cg)])
                            else:
                                T.op('dve', lambda e, bi=bi, tt=tt, nt=nt, cg=cg, ex=ex: e.scalar_tensor_tensor(
                                    out=stream[0:nt, tt, 512 * cg:512 * cg + 512], in0=pb[bi][0:nt, :], scalar=comb[0:nt, tt, ex:ex + 1],
                                    in1=stream[0:nt, tt, 512 * cg:512 * cg + 512], op0=ALU.mult, op1=ALU.add),
                                    reads=[PBK[bi], ('stream', tt, cg)], writes=[('stream', tt, cg)])
            T.barrier()

            _stop(6)
            with ExitStack() as pD:
                wpg = [sb(pD, "wpg%d" % i, [128, 16, 512], BF16) for i in range(2)]
                wple = sb(pD, "wple", [128, 2, 2048], BF16)
                pTt = sb(pD, "pT", [128, 2, NTOK], BF16)
                pbf = sb(pD, "pbf", [128, 256], BF16)
                sgm = [sb(pD, "sgm%d" % i, [128, 512], F32) for i in range(2)]
                osb = [sb(pD, "osb%d" % i, [128, 512], F32) for i in range(3)]
                alloc_ln(pD, "D")
                gam, bet = LB['gam'], LB['bet']
                T.dma('sp', "k0", gam[:], ln_gb[2].partition_broadcast(128), writes=['gam'])
                T.dma('sp', "k1", bet[:], ln_gb[3].partition_broadcast(128), writes=['bet'])
                T.dma('pool', "k2", wple[:], w_ple.rearrange("(k q) n -> q k n", q=128), writes=['wple'])
                def prep_tile(tt, t0, nt):
                    layer_norm(tt, nt, LN_EPS / (ALPHA * ALPHA))
                    to_T(tt, t0, nt)
                    src = pp[t0:t0 + nt, :] if tt < 8 else pps[:, :]
                    T.dma('pool', "pbf", pbf[0:nt, :], src, writes=['pbf'])
                    bi = mmbank()
                    bank = pb[bi][:].bitcast(BF16)

                    def f(e, bank=bank, nt=nt):
                        for j in range(2):
                            ins = e.transpose(out=bank[:, nt * j:nt * j + nt], in_=pbf[0:nt, 128 * j:128 * j + 128],
                                              identity=identb[0:nt, 0:nt])
                        return ins
                    T.op('pe', f, reads=['pbf', 'identb'], writes=[PBK[bi]])
                    T.op('dve', lambda e, bank=bank, t0=t0, nt=nt: e.tensor_copy(
                        out=pTt[:, :, t0:t0 + nt], in_=bank[:, 0:2 * nt].rearrange("q (j c) -> q j c", c=nt)),
                        reads=[PBK[bi]], writes=[('pT', tt)])

                n_o = 0
                for cg in range(4):
                    s = cg % 2
                    T.dma('pool', "wpg%d" % s, wpg[s][:], w_pg[:, 512 * cg:512 * cg + 512].rearrange("(k q) n -> q k n", q=128),
                          writes=[('wpg', s)])
                    for tt, (t0, nt) in enumerate(TT):
                        if cg == 0:
                            prep_tile(tt, t0, nt)
                        bg = 2 + 2 * (n_o % 2)
                        bp = bg + 1

                        def f(e, bg=bg, bp=bp, s=s, t0=t0, nt=nt, cg=cg):
                            for k in range(16):
                                e.matmul(pb[bg][0:nt, :], lhsT=xnT[:, k, t0:t0 + nt], rhs=wpg[s][:, k, :], start=(k == 0), stop=(k == 15))
                            for j in range(2):
                                ins = e.matmul(pb[bp][0:nt, :], lhsT=pTt[:, j, t0:t0 + nt], rhs=wple[:, j, 512 * cg:512 * cg + 512],
                                               start=(j == 0), stop=(j == 1))
                            return ins
                        T.op('pe', f, reads=[('wpg', s), 'wple', ('pT', tt), ('xnT', tt, 0), ('xnT', tt, 1)], writes=[PBK[bg], PBK[bp]])
                        q = n_o % 2
                        o = n_o % 3
                        n_o += 1
                        T.op('act', lambda e, q=q, bg=bg, nt=nt: e.activation(out=sgm[q][0:nt, :], in_=pb[bg][0:nt, :], func=AF.Sigmoid),
                             reads=[PBK[bg]], writes=[('sgm', q)])
                        T.op('dve', lambda e, q=q, bp=bp, nt=nt: e.tensor_tensor(out=sgm[q][0:nt, :], in0=pb[bp][0:nt, :], in1=sgm[q][0:nt, :],
                                                                                op=ALU.mult), reads=[PBK[bp], ('sgm', q)], writes=[('sgm', q)])
                        T.op('pool', lambda e, q=q, o=o, tt=tt, nt=nt, cg=cg: e.tensor_tensor(
                            out=osb[o][0:nt, :], in0=sgm[q][0:nt, :], in1=stream[0:nt, tt, 512 * cg:512 * cg + 512], op=ALU.add),
                            reads=[('sgm', q), ('stream', tt)], writes=[('osb', o)])
                        T.dma('sp', "osb%d" % o, y[t0:t0 + nt, 512 * cg:512 * cg + 512], osb[o][0:nt, :], reads=[('osb', o)])
        except _Stop:
            pass
        DEAD[0] = False
        T.barrier(final=True)
    return nc


def _consts(core):
    own0 = OWN * core
    inv_freq = (np.float32(500000.0) ** (-(np.arange(0, 16, 2, dtype=np.float32)) / np.float32(16))).astype(np.float32)
    pos = np.concatenate([np.arange(own0 - 2048, own0 + OWN), np.full(4, 8192)]).astype(np.float32)
    ang = (pos[None, :] * inv_freq[:, None]).astype(np.float32).astype(np.float64)
    C = np.ones((128, 3076), np.float32)
    S = np.zeros((128, 3076), np.float32)
    R = np.zeros((128, 128), np.float32)
    for hh in range(2):
        b = 64 * hh
        C[b:b + 8] = np.cos(ang)
        C[b + 8:b + 16] = np.cos(ang)
        S[b:b + 8] = -np.sin(ang)
        S[b + 8:b + 16] = np.sin(ang)
        for i in range(8):
            R[b + 8 + i, b + i] = 1.0
            R[b + i, b + 8 + i] = 1.0
    kk = np.arange(128)[:, None]
    qq = np.arange(128)[None, :]
    gen = np.concatenate([(kk >= qq), (kk <= qq)], axis=1).astype(np.float32)
    mk = np.zeros((128, 4, 256), np.float32)
    mk[:, 0] = gen
    first = gen.copy()
    if core == 0:
        first[:, 0:128] = 0.0
    mk[:, 1] = first
    mk[:, 2] = first
    q64 = np.arange(64)[None, :]
    m_glob = 64 * core - 128 + kk
    b0 = ((kk >= q64) & (m_glob >= 0)).astype(np.float32)
    b1 = (kk <= q64).astype(np.float32)
    mk[:, 3, 0:64] = b0
    mk[:, 3, 64:128] = b1
    invc = np.zeros((128, 4, 16), np.float32)
    for gi, win in enumerate(POOL_WINDOWS):
        p_ = own0 + np.arange(16)
        invc[:, gi, :] = (1.0 / np.minimum(p_ + 1, win)).astype(np.float32)[None, :]
    sel = np.zeros((60, 4, 4), np.float32)
    for b in range(4):
        for gi, win in enumerate(POOL_WINDOWS):
            for j in range(15):
                if j >= 15 - (win - 1):
                    sel[b * 15 + j, gi, b] = 1.0
    return dict(ropeC=C, ropeS=S, rperm=R, mk=mk, invc=invc, sel=sel)


_PROG = None
DEBUG_CORES = [None]


def kernel(**inp):
    global _PROG
    if _PROG is None:
        _PROG = build_program()
    nc = _PROG
    f = lambda a: np.ascontiguousarray(a, dtype=np.float32)
    x = inp["x_prompt"][0]
    xpad = np.concatenate([np.zeros((2048, 2048), np.float32), x], axis=0)
    shared = dict(
        w_in=f(inp["w_in"][0]), w_out=f(inp["w_out"][0]), w_pool=f(inp["w_pool"][0]),
        pool_scale=f(inp["pool_scale"][0].reshape(1024, 1)),
        ln1_g=f(inp["ln1_g"]), ln1_b=f(inp["ln1_b"]), ln2_g=f(inp["ln2_g"]), ln2_b=f(inp["ln2_b"]),
        w_gr=f(inp["w_group_router"][0]), w_er=f(inp["w_expert_router"][0]),
        b_r=f(np.concatenate([inp["b_group_router"][0], inp["b_expert_router"][0]])[None, :]),
        w_gate=f(inp["w_gate"][0]), w_up=f(inp["w_up"][0]), w_down=f(inp["w_down"][0]),
        w_ple=f(inp["w_ple"][0]), w_pg=f(inp["w_ple_gate"][0]))
    caches = [inp["cache_kv_w128_d1"][0], inp["cache_kv_w512_d4"][0], inp["cache_kv_w2048_d16"][0]]
    in_maps = []
    for c in range(NCORE):
        m = dict(shared)
        m["xh"] = f(xpad[OWN * c:OWN * c + 3072])
        m["xs"] = f(inp["x_sample"][4 * c:4 * c + 4, 0])
        for p in range(3):
            m["c%d" % p] = f(caches[p][4 * c:4 * c + 4].reshape(4, CL[p], 2, 1024))
        m["spool"] = f(inp["state_pool"][0, 4 * c:4 * c + 4])
        m["pp"] = f(inp["p_prompt"][0, 0, OWN * c:OWN * c + OWN])
        m["pps"] = f(inp["p_sample"][0, 4 * c:4 * c + 4, 0])
        m.update(_consts(c))
        in_maps.append(m)
    if DEBUG_CORES[0] is not None:
        sub = [in_maps[c] for c in DEBUG_CORES[0]]
        return run_bass_kernel_spmd(nc, sub, core_ids=list(range(len(sub)))).results
    res = run_bass_kernel_spmd(nc, in_maps, core_ids=list(range(NCORE))).results
    y_p = np.concatenate([r["y"][0:OWN] for r in res], 0)[None]
    y_s = np.concatenate([r["y"][OWN:OWN + 4] for r in res], 0)[:, None, :]
    kv0 = res[7]["kvp0"].reshape(1, 1, 128, 2, 16, 64)
    kv1 = res[7]["kvp1"].reshape(1, 1, 512, 2, 16, 64)
    kv2 = np.concatenate([res[6]["kvp2"], res[7]["kvp2"]], 0).reshape(1, 1, 2048, 2, 16, 64)
    pl = res[7]["poolp"][1:16].reshape(1, 1, 15, 1024)
    ks = [np.concatenate([r["kvs%d" % p] for r in res], 0).reshape(1, 32, CL[p], 2, 16, 64) for p in range(3)]
    pls = np.concatenate([r["pools"] for r in res], 0).reshape(1, 32, 15, 1024)
    outs = (y_p, y_s, kv0, kv1, kv2, pl, ks[0], ks[1], ks[2], pls)
    return tuple(np.ascontiguousarray(o, dtype=np.float32) for o in outs)
```
